# Optimizing a Trainium2 kernel written in Bass

```python
import jax
import jax.numpy as jnp
from jax import lax
import numpy as np

D_MODEL = 1024
BATCH = 8
SEQ = 8192
DEPTH = 2

GRID_W = 64
CTX_LEN = 256
CHUNK = 128
NORM_EPS = 1e-6
ROPE_BASE = 10000.0

GLA_HEADS = 4
GLA_K = D_MODEL // 2
GLA_V = D_MODEL
GLA_DK = GLA_K // GLA_HEADS
GLA_DV = GLA_V // GLA_HEADS
GLA_LOW_RANK = 16
GLA_TAU = 16.0

ATT_HEAD_DIM = 128
ATT_Q_HEADS = D_MODEL // ATT_HEAD_DIM
ATT_KV_HEADS = 2
ATT_WINDOW = 128
ATT_QBLOCK = 128

RET_HEADS = 4
RET_K = D_MODEL // 2
RET_V = D_MODEL
RET_DK = RET_K // RET_HEADS
RET_DV = RET_V // RET_HEADS

MOE_GROUPS = 4
MOE_EXPERTS_PER_GROUP = 8
MOE_EXPERTS = MOE_GROUPS * MOE_EXPERTS_PER_GROUP
MOE_TOP_K = 2
MOE_HIDDEN = D_MODEL
MOE_BLOCK = 256

IN_LAYOUT = (
    ('gla_q', GLA_K), ('gla_k', GLA_K), ('gla_v', GLA_V), ('gla_r', GLA_V), ('gla_lr', 2 * GLA_LOW_RANK),
    ('att_q', ATT_Q_HEADS * ATT_HEAD_DIM), ('att_k', ATT_KV_HEADS * ATT_HEAD_DIM), ('att_v', ATT_KV_HEADS * ATT_HEAD_DIM),
    ('ret_q', RET_K), ('ret_k', RET_K), ('ret_v', RET_V), ('ret_g', RET_V),
    ('gates', 3 * D_MODEL),
)
N_IN = (2 * GLA_K + 2 * GLA_V + 2 * GLA_LOW_RANK + ATT_Q_HEADS * ATT_HEAD_DIM + 2 * ATT_KV_HEADS * ATT_HEAD_DIM
        + 2 * RET_K + 2 * RET_V + 3 * D_MODEL)

kernel_name = 'hybrid_gla_swa_retention_hmoe_dit'


def rms_norm(x, g):
    x32 = x.astype(jnp.float32)
    y = x32 * lax.rsqrt(jnp.mean(x32 * x32, axis=-1, keepdims=True) + NORM_EPS)
    return y.astype(x.dtype) * g


def head_group_norm(y, g):
    b, t, h, dv = y.shape
    y32 = y.astype(jnp.float32)
    mu = jnp.mean(y32, axis=-1, keepdims=True)
    var = jnp.mean(jnp.square(y32 - mu), axis=-1, keepdims=True)
    yn = ((y32 - mu) * lax.rsqrt(var + NORM_EPS)).astype(y.dtype)
    return yn.reshape(b, t, h * dv) * g


def rope(x, pos):
    half = x.shape[-1] // 2
    inv_freq = ROPE_BASE ** (-jnp.arange(half, dtype=jnp.float32) / half)
    ang = pos.astype(jnp.float32)[:, None] * inv_freq[None, :]
    cos = jnp.cos(ang)[:, None, :].astype(x.dtype)
    sin = jnp.sin(ang)[:, None, :].astype(x.dtype)
    x1, x2 = x[..., :half], x[..., half:]
    return jnp.concatenate([x1 * cos - x2 * sin, x2 * cos + x1 * sin], axis=-1)


def axial_rope(x, rows, cols):
    h = x.shape[-1] // 2
    return jnp.concatenate([rope(x[..., :h], rows), rope(x[..., h:], cols)], axis=-1)


def chunk_recurrence(q, k, v, log_a, s0, strict):
    b_, t_, h_, dk = q.shape
    dv = v.shape[-1]
    n = t_ // CHUNK
    f32 = jnp.float32
    qc = q.astype(f32).reshape(b_, n, CHUNK, h_, dk)
    kc = k.astype(f32).reshape(b_, n, CHUNK, h_, dk)
    vc = v.astype(f32).reshape(b_, n, CHUNK, h_, dv)
    cum = jnp.cumsum(log_a.astype(f32).reshape(b_, n, CHUNK, h_, dk), axis=2)
    cum_last = cum[:, :, -1:]
    q_in = qc * jnp.exp(cum)
    k_in = kc * jnp.exp(-cum)
    k_out = kc * jnp.exp(cum_last - cum)
    idx = jnp.arange(CHUNK)
    mask = (idx[:, None] > idx[None, :]) if strict else (idx[:, None] >= idx[None, :])
    scores = jnp.where(mask, jnp.einsum('bnihd,bnjhd->bnhij', q_in, k_in), 0.0)
    o_intra = jnp.einsum('bnhij,bnjhe->bnihe', scores, vc)

    def step(s, xs):
        q_i, k_o, v_i, dec = xs
        o = jnp.einsum('bihd,bhde->bihe', q_i, s)
        s = s * dec[..., None] + jnp.einsum('bjhd,bjhe->bhde', k_o, v_i)
        return s, o

    xs = (jnp.moveaxis(q_in, 1, 0), jnp.moveaxis(k_out, 1, 0), jnp.moveaxis(vc, 1, 0),
          jnp.moveaxis(jnp.exp(cum_last[:, :, 0]), 1, 0))
    s_fin, o_inter = lax.scan(step, s0.astype(f32), xs)
    o = o_intra + jnp.moveaxis(o_inter, 0, 1)
    return o.reshape(b_, t_, h_, dv).astype(v.dtype), s_fin


def bidirectional_scan(q, k, v, la_fwd, la_bwd, n_ctx):
    b_, _, h_, dk = q.shape
    dv = v.shape[-1]
    la_fwd = jnp.broadcast_to(la_fwd, q.shape)
    la_bwd = jnp.broadcast_to(la_bwd, q.shape)
    s0 = jnp.zeros((b_, h_, dk, dv), jnp.float32)
    rev = lambda a: jnp.flip(a, axis=1)
    oc_f, sc_f = chunk_recurrence(q[:, :n_ctx], k[:, :n_ctx], v[:, :n_ctx], la_fwd[:, :n_ctx], s0, False)
    ol_f, _ = chunk_recurrence(q[:, n_ctx:], k[:, n_ctx:], v[:, n_ctx:], la_fwd[:, n_ctx:], sc_f, False)
    oc_b, sc_b = chunk_recurrence(rev(q[:, :n_ctx]), rev(k[:, :n_ctx]), rev(v[:, :n_ctx]), rev(la_bwd[:, :n_ctx]), s0, True)
    ol_b, _ = chunk_recurrence(rev(q[:, n_ctx:]), rev(k[:, n_ctx:]), rev(v[:, n_ctx:]), rev(la_bwd[:, n_ctx:]), sc_b, True)
    return jnp.concatenate([oc_f + rev(oc_b), ol_f + rev(ol_b)], axis=1)


def window_attention(q_lat, k_lat, v_lat, q_ctx, k_ctx, v_ctx, sink, with_ctx_queries):
    b_, l_, hq, hd = q_lat.shape
    g_ = hq // ATT_KV_HEADS
    c_ = k_ctx.shape[1]
    nb = l_ // ATT_QBLOCK
    span = ATT_QBLOCK + 2 * ATT_WINDOW
    scale = hd ** -0.5
    pad = ((0, 0), (ATT_WINDOW, ATT_WINDOW), (0, 0), (0, 0))
    k_pad = jnp.pad(k_lat, pad)
    v_pad = jnp.pad(v_lat, pad)
    sink_logit = sink.astype(jnp.float32).reshape(1, ATT_KV_HEADS, g_, 1, 1)
    q_blocks = jnp.moveaxis(q_lat.reshape(b_, nb, ATT_QBLOCK, ATT_KV_HEADS, g_, hd), 1, 0)

    def attend_block(args):
        qb, n = args
        start = n * ATT_QBLOCK
        kw = lax.dynamic_slice_in_dim(k_pad, start, span, axis=1)
        vw = lax.dynamic_slice_in_dim(v_pad, start, span, axis=1)
        q_pos = start + jnp.arange(ATT_QBLOCK)
        k_pos = start - ATT_WINDOW + jnp.arange(span)
        valid = ((jnp.abs(q_pos[:, None] - k_pos[None, :]) <= ATT_WINDOW)
                 & (k_pos >= 0)[None, :] & (k_pos < l_)[None, :])
        s_loc = jnp.where(valid, jnp.einsum('bqhgd,bkhd->bhgqk', qb, kw).astype(jnp.float32) * scale, -jnp.inf)
        s_ctx = jnp.einsum('bqhgd,bkhd->bhgqk', qb, k_ctx).astype(jnp.float32) * scale
        s_sink = jnp.broadcast_to(sink_logit, s_ctx.shape[:-1] + (1,))
        p = jax.nn.softmax(jnp.concatenate([s_sink, s_ctx, s_loc], axis=-1), axis=-1).astype(qb.dtype)
        return (jnp.einsum('bhgqk,bkhd->bqhgd', p[..., 1:1 + c_], v_ctx)
                + jnp.einsum('bhgqk,bkhd->bqhgd', p[..., 1 + c_:], vw))

    o_lat = lax.map(attend_block, (q_blocks, jnp.arange(nb)))
    o_lat = jnp.moveaxis(o_lat, 0, 1).reshape(b_, l_, hq * hd)
    if not with_ctx_queries:
        return o_lat
    qc = q_ctx.reshape(b_, c_, ATT_KV_HEADS, g_, hd)
    s = jnp.einsum('bqhgd,bkhd->bhgqk', qc, k_ctx).astype(jnp.float32) * scale
    s_sink = jnp.broadcast_to(sink_logit, s.shape[:-1] + (1,))
    p = jax.nn.softmax(jnp.concatenate([s_sink, s], axis=-1), axis=-1)[..., 1:].astype(q_ctx.dtype)
    o_ctx = jnp.einsum('bhgqk,bkhd->bqhgd', p, v_ctx).reshape(b_, c_, hq * hd)
    return jnp.concatenate([o_ctx, o_lat], axis=1)


def token_mixers(h, n_ctx, rows, cols, ret_pos, ret_log_decay, w_in, gla_wa2, gla_ba, gla_norm_g, attn_sink,
                 ret_norm_g, w_br_gla, w_br_attn, w_br_ret, w_out, latent_only):
    t0 = n_ctx if latent_only else 0
    offsets = {}
    start = 0
    for name, width in IN_LAYOUT:
        offsets[name] = (start, start + width)
        start += width

    def proj(name, hh):
        lo, hi = offsets[name]
        return hh @ w_in[:, lo:hi]

    def heads(a, n):
        return a.reshape(a.shape[0], a.shape[1], n, -1)

    h_out = h[:, t0:]

    lr_f, lr_b = jnp.split(proj('gla_lr', h), 2, axis=-1)
    la_f = jax.nn.log_sigmoid((lr_f @ gla_wa2[0] + gla_ba[0]).astype(jnp.float32)) / GLA_TAU
    la_b = jax.nn.log_sigmoid((lr_b @ gla_wa2[1] + gla_ba[1]).astype(jnp.float32)) / GLA_TAU
    o_gla = bidirectional_scan(heads(proj('gla_q', h), GLA_HEADS) * GLA_DK ** -0.5, heads(proj('gla_k', h), GLA_HEADS),
                               heads(proj('gla_v', h), GLA_HEADS), heads(la_f, GLA_HEADS), heads(la_b, GLA_HEADS), n_ctx)
    y_gla = head_group_norm(o_gla[:, t0:], gla_norm_g) * jax.nn.silu(proj('gla_r', h_out))

    aq = heads(proj('att_q', h), ATT_Q_HEADS)
    ak = heads(proj('att_k', h), ATT_KV_HEADS)
    av = heads(proj('att_v', h), ATT_KV_HEADS)
    y_att = window_attention(axial_rope(aq[:, n_ctx:], rows, cols), axial_rope(ak[:, n_ctx:], rows, cols), av[:, n_ctx:],
                             aq[:, :n_ctx], ak[:, :n_ctx], av[:, :n_ctx], attn_sink, not latent_only)

    rq = rope(heads(proj('ret_q', h), RET_HEADS), ret_pos)
    rk = rope(heads(proj('ret_k', h), RET_HEADS), ret_pos) * RET_DK ** -0.5
    o_ret = bidirectional_scan(rq, rk, heads(proj('ret_v', h), RET_HEADS), ret_log_decay, ret_log_decay, n_ctx)
    y_ret = head_group_norm(o_ret[:, t0:], ret_norm_g) * jax.nn.silu(proj('ret_g', h_out))

    g_gla, g_att, g_ret = jnp.split(jax.nn.sigmoid(proj('gates', h_out)), 3, axis=-1)
    merged = g_gla * (y_gla @ w_br_gla) + g_att * (y_att @ w_br_attn) + g_ret * (y_ret @ w_br_ret)
    return merged @ w_out


def hier_moe(h, w_grp, b_grp, w_exp, b_exp, w1, w3, w2):
    n_tok, d = h.shape
    grp_logits = (h @ w_grp + b_grp).astype(jnp.float32)
    grp = jnp.argmax(grp_logits, axis=-1)
    grp_prob = jnp.take_along_axis(jax.nn.softmax(grp_logits, axis=-1), grp[:, None], axis=-1)
    exp_logits = (h @ w_exp + b_exp).astype(jnp.float32).reshape(n_tok, MOE_GROUPS, MOE_EXPERTS_PER_GROUP)
    in_grp = jnp.take_along_axis(exp_logits, grp[:, None, None], axis=1)[:, 0]
    top_val, top_idx = lax.top_k(in_grp, MOE_TOP_K)
    gate = grp_prob * jax.nn.softmax(top_val, axis=-1)
    expert = (grp[:, None] * MOE_EXPERTS_PER_GROUP + top_idx).astype(jnp.int32)

    n_assign = n_tok * MOE_TOP_K
    e_flat = expert.reshape(n_assign)
    t_flat = jnp.repeat(jnp.arange(n_tok, dtype=jnp.int32), MOE_TOP_K)
    order = jnp.argsort(e_flat)
    e_s, t_s, w_s = e_flat[order], t_flat[order], gate.reshape(n_assign)[order]
    counts = jnp.zeros((MOE_EXPERTS,), jnp.int32).at[e_s].add(1)
    starts = jnp.cumsum(counts) - counts
    padded = (counts + MOE_BLOCK - 1) // MOE_BLOCK * MOE_BLOCK
    pad_end = jnp.cumsum(padded)
    pad_start = pad_end - padded
    dest = pad_start[e_s] + jnp.arange(n_assign, dtype=jnp.int32) - starts[e_s]
    n_blocks = (n_assign + MOE_EXPERTS * (MOE_BLOCK - 1) + MOE_BLOCK - 1) // MOE_BLOCK
    n_slots = n_blocks * MOE_BLOCK
    slot_tok = jnp.full((n_slots,), n_tok, jnp.int32).at[dest].set(t_s)
    slot_w = jnp.zeros((n_slots,), h.dtype).at[dest].set(w_s.astype(h.dtype))
    blk_exp = jnp.minimum(jnp.searchsorted(pad_end, jnp.arange(n_blocks, dtype=jnp.int32) * MOE_BLOCK, side='right'),
                          MOE_EXPERTS - 1)
    h_pad = jnp.concatenate([h, jnp.zeros((1, d), h.dtype)], axis=0)

    def step(acc, xs):
        tok, wgt, e = xs
        xb = h_pad[tok]
        yb = (jax.nn.silu(xb @ w1[e]) * (xb @ w3[e])) @ w2[e]
        return acc.at[tok].add(yb * wgt[:, None]), None

    acc, _ = lax.scan(step, jnp.zeros((n_tok + 1, d), h.dtype),
                      (slot_tok.reshape(n_blocks, MOE_BLOCK), slot_w.reshape(n_blocks, MOE_BLOCK), blk_exp))
    return acc[:n_tok]


def setup_inputs(seed: int = 0) -> dict:
    key = jax.random.key(seed)
    keys = iter(jax.random.split(key, 32))
    f32 = jnp.float32
    D = D_MODEL

    def nrm(shape, scale):
        return jax.random.normal(next(keys), shape, f32) * scale

    return {
        'x': nrm((BATCH, SEQ, D), 1.0),
        'c': nrm((BATCH, D), 1.0),
        'ctx': nrm((BATCH, CTX_LEN, D), 1.0),
        'c_ctx': nrm((D,), 1.0),
        'w_ada': nrm((DEPTH, D, 6 * D), 0.5 * D ** -0.5),
        'b_ada': nrm((DEPTH, 6 * D), 0.02),
        'norm1_g': 1.0 + nrm((DEPTH, D), 0.05),
        'norm2_g': 1.0 + nrm((DEPTH, D), 0.05),
        'w_in': nrm((DEPTH, D, N_IN), D ** -0.5),
        'gla_wa2': nrm((DEPTH, 2, GLA_LOW_RANK, GLA_K), GLA_LOW_RANK ** -0.5),
        'gla_ba': nrm((DEPTH, 2, GLA_K), 0.1),
        'gla_norm_g': 1.0 + nrm((DEPTH, GLA_V), 0.05),
        'attn_sink': nrm((DEPTH, ATT_Q_HEADS), 0.5),
        'ret_norm_g': 1.0 + nrm((DEPTH, RET_V), 0.05),
        'w_br_gla': nrm((DEPTH, GLA_V, D), GLA_V ** -0.5),
        'w_br_attn': nrm((DEPTH, ATT_Q_HEADS * ATT_HEAD_DIM, D), (ATT_Q_HEADS * ATT_HEAD_DIM) ** -0.5),
        'w_br_ret': nrm((DEPTH, RET_V, D), RET_V ** -0.5),
        'w_out': nrm((DEPTH, D, D), D ** -0.5),
        'moe_w_grp': nrm((DEPTH, D, MOE_GROUPS), D ** -0.5),
        'moe_b_grp': nrm((DEPTH, MOE_GROUPS), 0.01),
        'moe_w_exp': nrm((DEPTH, D, MOE_EXPERTS), D ** -0.5),
        'moe_b_exp': nrm((DEPTH, MOE_EXPERTS), 0.01),
        'moe_w1': nrm((DEPTH, MOE_EXPERTS, D, MOE_HIDDEN), D ** -0.5),
        'moe_w3': nrm((DEPTH, MOE_EXPERTS, D, MOE_HIDDEN), D ** -0.5),
        'moe_w2': nrm((DEPTH, MOE_EXPERTS, MOE_HIDDEN, D), MOE_HIDDEN ** -0.5),
        'final_g': 1.0 + nrm((D,), 0.05),
    }


def reference(x, c, ctx, c_ctx, w_ada, b_ada, norm1_g, norm2_g, w_in, gla_wa2, gla_ba, gla_norm_g, attn_sink,
              ret_norm_g, w_br_gla, w_br_attn, w_br_ret, w_out, moe_w_grp, moe_b_grp, moe_w_exp, moe_b_exp,
              moe_w1, moe_w3, moe_w2, final_g):
    b_, l_, d = x.shape
    n_ctx = ctx.shape[1]
    ROWS = l_ // GRID_W
    rows = jnp.broadcast_to(jnp.arange(ROWS)[:, None], (ROWS, GRID_W)).reshape(l_)
    cols = jnp.broadcast_to(jnp.arange(GRID_W)[None, :], (ROWS, GRID_W)).reshape(l_)
    ret_pos = jnp.arange(n_ctx + l_)
    ret_log_decay = jnp.log(1.0 - jnp.exp2(-5.0 - jnp.arange(RET_HEADS, dtype=jnp.float32))).reshape(1, 1, RET_HEADS, 1)
    silu_c = jax.nn.silu(c)
    silu_cc = jax.nn.silu(c_ctx)

    for layer in range(DEPTH):
        last = layer == DEPTH - 1
        mod = silu_c @ w_ada[layer] + b_ada[layer]
        mod_c = silu_cc @ w_ada[layer] + b_ada[layer]
        sh1, sc1, g1, sh2, sc2, g2 = jnp.split(mod[:, None, :], 6, axis=-1)
        sh1c, sc1c, g1c, sh2c, sc2c, g2c = jnp.split(mod_c, 6, axis=-1)

        h = jnp.concatenate([rms_norm(ctx, norm1_g[layer]) * (1.0 + sc1c) + sh1c,
                             rms_norm(x, norm1_g[layer]) * (1.0 + sc1) + sh1], axis=1)
        mix = token_mixers(h, n_ctx, rows, cols, ret_pos, ret_log_decay, w_in[layer], gla_wa2[layer], gla_ba[layer],
                           gla_norm_g[layer], attn_sink[layer], ret_norm_g[layer], w_br_gla[layer], w_br_attn[layer],
                           w_br_ret[layer], w_out[layer], last)
        x = x + g1 * mix[:, -l_:]
        if not last:
            ctx = ctx + g1c * mix[:, :n_ctx]

        h2 = rms_norm(x, norm2_g[layer]) * (1.0 + sc2) + sh2
        if not last:
            h2 = jnp.concatenate([rms_norm(ctx, norm2_g[layer]) * (1.0 + sc2c) + sh2c, h2], axis=1)
        f = hier_moe(h2.reshape(-1, d), moe_w_grp[layer], moe_b_grp[layer], moe_w_exp[layer], moe_b_exp[layer],
                     moe_w1[layer], moe_w3[layer], moe_w2[layer]).reshape(h2.shape)
        x = x + g2 * f[:, -l_:]
        if not last:
            ctx = ctx + g2c * f[:, :n_ctx]

    return rms_norm(x, final_g)
```

```python
import numpy as np
import concourse.bass as bass
import concourse.mybir as mybir
from concourse.bass_utils import run_bass_kernel_spmd
from contextlib import ExitStack

F32 = mybir.dt.float32
BF16 = mybir.dt.bfloat16
AF = mybir.ActivationFunctionType
ALU = mybir.AluOpType
AX = mybir.AxisListType

D = 1024
CTX = 256
NIN = 10784
EPS = 1e-6
C_GQ, C_GK, C_GV, C_GR, C_LR = 0, 512, 1024, 2048, 3072
C_AQ, C_AK, C_AV = 3104, 4128, 4384
C_RQ, C_RK, C_RV, C_RG = 4640, 5152, 5664, 6688
C_GATES = 7712
BIG = 1.0e4


class Buf:
    __slots__ = ("t", "w", "r", "name", "dsem", "excl")

    def __init__(self, t, name):
        self.t = t
        self.name = name
        self.w = {}
        self.r = {}
        self.dsem = None
        self.excl = False

    def __getitem__(self, idx):
        return self.t[idx]


class Eng:
    def __init__(self, name, eng, sem, is_pe=False):
        self.name = name
        self.eng = eng
        self.sem = sem
        self.cnt = 0
        self.waited = {}
        self.is_pe = is_pe


class K:
    def __init__(self):
        self.nc = bass.Bass("TRN2", target_bir_lowering=False)
        self.es = ExitStack()
        self.scopes = []
        nc = self.nc
        self.sems = {}
        self.dcnt = {}
        self.free_dsems = []
        self.scope_dsems = []
        self.nsem = 0
        self.pe = Eng("pe", nc.tensor, self.newsem("pe"), True)
        self.act = Eng("act", nc.scalar, self.newsem("act"))
        self.dve = Eng("dve", nc.vector, self.newsem("dve"))
        self.pool = Eng("pool", nc.gpsimd, self.newsem("pool"))
        self.sp = Eng("sp", nc.sync, self.newsem("sp"))
        self.engs = [self.pe, self.act, self.dve, self.pool, self.sp]
        self.nbuf = 0

    def newsem(self, name):
        s = self.es.enter_context(self.nc.semaphore(name))
        self.sems[name] = s
        self.nsem += 1
        return name

    def _stack(self):
        return self.scopes[-1] if self.scopes else self.es

    def sb(self, shape, dt, name=None):
        self.nbuf += 1
        name = name or f"sb{self.nbuf}"
        return Buf(self._stack().enter_context(self.nc.sbuf_tensor(name, list(shape), dt)), name)

    def ps(self, shape, dt=F32, name=None):
        self.nbuf += 1
        name = name or f"ps{self.nbuf}"
        b = Buf(self._stack().enter_context(self.nc.psum_tensor(name, list(shape), dt)), name)
        b.excl = True
        return b

    def dram(self, name, shape, dt, kind="Internal"):
        t = self.nc.dram_tensor(name, list(shape), dt, kind=kind)
        return Buf(t.ap(), name)

    def _sync(self, E, reads, writes):
        need = {}
        for b in reads:
            for s, v in b.w.items():
                if need.get(s, 0) < v:
                    need[s] = v
            if b.excl:
                for s, v in b.r.items():
                    if s != E.sem and need.get(s, 0) < v:
                        need[s] = v
        for b in writes:
            for s, v in b.w.items():
                if need.get(s, 0) < v:
                    need[s] = v
            for s, v in b.r.items():
                if need.get(s, 0) < v:
                    need[s] = v
        for s, v in need.items():
            if E.is_pe and s == E.sem:
                continue
            if E.waited.get(s, 0) < v:
                E.eng.wait_ge(self.sems[s], v)
                E.waited[s] = v

    def op(self, E, reads, writes, fn):
        self._sync(E, reads, writes)
        ins = fn(E.eng)
        E.cnt += 1
        ins.then_inc(self.sems[E.sem], 1)
        for b in reads:
            b.r[E.sem] = E.cnt
        for b in writes:
            b.w = {E.sem: E.cnt}
            b.r = {}
        return ins

    def dma(self, Q, out, in_, reads, writes, semb=None, **kw):
        self._sync(Q, reads, writes)
        semb = semb or (writes + reads)[0]
        if semb.dsem is None:
            if self.free_dsems:
                semb.dsem = self.free_dsems.pop()
            else:
                semb.dsem = self.newsem(f"d{self.nsem}")
                self.dcnt[semb.dsem] = 0
            if self.scopes:
                self.scope_dsems[-1].append(semb.dsem)
        ins = Q.eng.dma_start(out=out, in_=in_, **kw)
        self.dcnt[semb.dsem] += 16
        v = self.dcnt[semb.dsem]
        ins.then_inc(self.sems[semb.dsem], 16)
        for b in reads:
            b.r[semb.dsem] = v
        for b in writes:
            b.w = {semb.dsem: v}
            b.r = {}
        return ins

    def barrier(self):
        for E in self.engs:
            for E2 in self.engs:
                if E2 is E or E2.cnt == 0:
                    continue
                if E.waited.get(E2.sem, 0) < E2.cnt:
                    E.eng.wait_ge(self.sems[E2.sem], E2.cnt)
                    E.waited[E2.sem] = E2.cnt
            for s, v in self.dcnt.items():
                if v and E.waited.get(s, 0) < v:
                    E.eng.wait_ge(self.sems[s], v)
                    E.waited[s] = v

    def push(self):
        self.scopes.append(ExitStack())
        self.scope_dsems.append([])

    def pop(self):
        self.barrier()
        self.scopes.pop().close()
        self.free_dsems.extend(self.scope_dsems.pop())


def build(L, n_layers=2, dbg=(), stop=99, NEXP=32):
    T = CTX + L
    NT = T // 128
    NC_ = CTX // 128
    k = K()
    nc = k.nc
    pe, act, dve, pool, sp = k.pe, k.act, k.dve, k.pool, k.sp

    def inp(name, shape, dt=F32):
        return k.dram(name, shape, dt, kind="ExternalInput")

    xin = inp("xin", [T, D])
    ccT = inp("ccT", [128, 16])
    w_ada = inp("w_ada", [2, D, 6 * D])
    b_ada = inp("b_ada", [2, 6 * D])
    norm1_g = inp("norm1_g", [2, D])
    norm2_g = inp("norm2_g", [2, D])
    w_in = inp("w_in", [2, D, NIN])
    wa_aug = inp("wa_aug", [2, 33, 1024])
    gla_norm_g = inp("gla_norm_g", [2, D])
    ret_norm_g = inp("ret_norm_g", [2, D])
    attn_sink = inp("attn_sink", [2, 8])
    w_br = [inp("w_br_gla", [2, D, D]), inp("w_br_attn", [2, D, D]), inp("w_br_ret", [2, D, D])]
    w_out = inp("w_out", [2, D, D])
    w_r = inp("w_r", [2, D, 36])
    b_r = inp("b_r", [2, 36])
    if "nomoe" not in dbg:
        moe_w1 = inp("moe_w1", [2, 32, D, D])
        moe_w3 = inp("moe_w3", [2, 32, D, D])
        moe_w2 = inp("moe_w2", [2, 32, D, D])
    final_g = inp("final_g", [1, D])
    c_masks = inp("c_masks", [3, 128, 512])
    c_tri = inp("c_tri", [4, 128, 128])
    c_spret = inp("c_spret", [128, 512])
    c_rope_a = inp("c_rope_a", [2, L, 128])
    c_rope_r = inp("c_rope_r", [2, T, 128])
    c_ident = inp("c_ident", [128, 128])

    out = k.dram("out", [L, D], F32, kind="ExternalOutput")
    dbg_out = {}

    def scratch(name, shape, dt):
        kind = "ExternalOutput" if name in dbg else "Internal"
        b = k.dram(name, shape, dt, kind=kind)
        if name in dbg:
            dbg_out[name] = b
        return b

    modv = scratch("modv", [2, 6 * D], F32)
    hT = scratch("hT", [NT, 128, 8, 128], BF16)
    proj = scratch("proj", [T, NIN], BF16)
    lrp = scratch("lrp", [T, 32], F32)
    o_part = scratch("o_part", [T, D], F32)
    yT = [scratch("yT_gla", [NT, 128, 8, 128], BF16), scratch("yT_att", [NT, 128, 8, 128], BF16),
          scratch("yT_ret", [NT, 128, 8, 128], BF16)]
    xm = scratch("xm", [T, D], F32)
    xn = scratch("xn", [T, D], F32)

    ident = k.sb([128, 128], F32, "ident")
    identb = k.sb([128, 128], BF16, "identb")
    onesb = k.sb([128, 128], BF16, "onesb")
    k.dma(sp, ident[:], c_ident[:], [c_ident], [ident])
    k.op(dve, [ident], [identb], lambda e: e.tensor_copy(out=identb[:], in_=ident[:]))
    k.op(dve, [], [onesb], lambda e: e.memset(onesb[:], 1.0))

    def bcast_load(dst, src_ap, srcbuf, q=None):
        k.dma(q or sp, dst[:], src_ap.partition_broadcast(128), [srcbuf], [dst])

    def stage_ada(l):
        k.push()
        cc = k.sb([128, 16], F32)
        sc = k.sb([128, 16], F32)
        wbuf = [k.sb([128, 8, 512], F32) for _ in range(2)]
        bb = k.sb([1, 6 * D], F32)
        one1 = k.sb([1, 2], F32)
        res = [k.sb([2, 512], F32) for _ in range(2)]
        pp = [k.ps([2, 512]) for _ in range(2)]
        k.dma(sp, cc[:], ccT[:], [ccT], [cc])
        k.dma(sp, bb[:], b_ada[l:l + 1, :], [b_ada], [bb])
        k.op(dve, [], [one1], lambda e: e.memset(one1[:], 1.0))
        k.op(act, [cc], [sc], lambda e: e.activation(out=sc[:], in_=cc[:], func=AF.Silu))
        scv = sc[:].rearrange("p (k r) -> p k r", r=2)
        for cg in range(12):
            wb = wbuf[cg % 2]
            k.dma(sp, wb[:], w_ada[l, :, cg * 512:(cg + 1) * 512].rearrange("(k p) n -> p k n", p=128), [w_ada], [wb])
            p = pp[cg % 2]
            r = res[cg % 2]
            for kk in range(8):
                k.op(pe, [sc, wb], [p], lambda e: e.matmul(p[:], lhsT=scv[:, kk, :], rhs=wb[:, kk, :], start=(kk == 0), stop=False))
            k.op(pe, [one1, bb], [p], lambda e: e.matmul(p[:], lhsT=one1[:], rhs=bb[:, cg * 512:(cg + 1) * 512], start=False, stop=True))
            k.op(act, [p], [r], lambda e: e.copy(out=r[:], in_=p[:]))
            k.dma(sp, modv[:, cg * 512:(cg + 1) * 512], r[:], [r], [modv])
        k.pop()

    def mod_tiles(l, which, ng):
        base = 0 if which == 1 else 3
        gB = k.sb([128, D], F32)
        bcast_load(gB, ng[l:l + 1, :], ng)
        outt = []
        for r in range(2):
            A = k.sb([128, D], F32)
            sh = k.sb([128, D], F32)
            bcast_load(A, modv[r:r + 1, (base + 1) * D:(base + 2) * D], modv)
            bcast_load(sh, modv[r:r + 1, base * D:(base + 1) * D], modv)
            k.op(dve, [A, gB], [A], lambda e: e.scalar_tensor_tensor(out=A[:], in0=A[:], scalar=1.0, in1=gB[:], op0=ALU.add, op1=ALU.mult))
            outt += [A, sh]
        return outt

    def rms_mod(xt, A, sh, junk, st, hout):
        k.op(act, [xt], [junk, st], lambda e: e.activation(out=junk[:], in_=xt[:], func=AF.Square, accum_out=st[:, 0:1]))
        k.op(dve, [st], [st], lambda e: e.tensor_scalar(out=st[:, 1:2], in0=st[:, 0:1], scalar1=1.0 / D, scalar2=EPS, op0=ALU.mult, op1=ALU.add))
        k.op(act, [st], [st], lambda e: e.activation(out=st[:, 2:3], in_=st[:, 1:2], func=AF.Sqrt))
        k.op(dve, [st], [st], lambda e: e.reciprocal(out=st[:, 3:4], in_=st[:, 2:3]))
        k.op(dve, [xt, st, A], [junk], lambda e: e.scalar_tensor_tensor(out=junk[:], in0=xt[:], scalar=st[:, 3:4], in1=A[:], op0=ALU.mult, op1=ALU.mult))
        k.op(dve, [junk, sh], [hout], lambda e: e.tensor_add(out=hout[:], in0=junk[:], in1=sh[:]))

    def transpose8(src, dstT, ptr, idt, evac):
        for c in range(8):
            k.op(pe, [src, idt], [ptr], lambda e: e.transpose(ptr[:, c * 128:(c + 1) * 128], src[:, c * 128:(c + 1) * 128], idt[:]))
        if evac is act:
            k.op(act, [ptr], [dstT], lambda e: e.copy(out=dstT[:].rearrange("p k t -> p (k t)"), in_=ptr[:]))
        else:
            k.op(dve, [ptr], [dstT], lambda e: e.tensor_copy(out=dstT[:].rearrange("p k t -> p (k t)"), in_=ptr[:]))

    def stage_norm1(l, xcur):
        k.push()
        A_l, sh_l, A_c, sh_c = mod_tiles(l, 1, norm1_g)
        xt = [k.sb([128, D], F32) for _ in range(2)]
        junk = k.sb([128, D], F32)
        st = k.sb([128, 4], F32)
        hb = k.sb([128, D], BF16)
        hTt = [k.sb([128, 8, 128], BF16) for _ in range(2)]
        ptr = k.ps([128, 1024], BF16)
        for t in range(NT):
            x_ = xt[t % 2]
            k.dma(sp, x_[:], xcur[t * 128:(t + 1) * 128, :], [xcur], [x_])
            A, sh = (A_c, sh_c) if t < NC_ else (A_l, sh_l)
            rms_mod(x_, A, sh, junk, st, hb)
            h_ = hTt[t % 2]
            transpose8(hb, h_, ptr, identb, act)
            k.dma(pool, hT[t], h_[:], [h_], [hT])
        k.pop()

    def stage_proj(l):
        k.push()
        PW = 2048
        passes = [(c0, min(c0 + PW, NIN)) for c0 in range(0, NIN, PW)]
        wt = [k.sb([128, 8, PW], BF16) for _ in range(2)]
        ht = [k.sb([128, 8, 128], BF16) for _ in range(2)]
        ot = [k.sb([128, PW], BF16) for _ in range(2)]
        lrt = k.sb([128, 32], F32)
        pb = [k.ps([128, 512]) for _ in range(6)]
        ev = 0
        it = 0
        for pi, (c0, c1) in enumerate(passes):
            w_ = wt[pi % 2]
            ncol = c1 - c0
            for kk in range(8):
                k.dma(pool, w_[:, kk, 0:ncol], w_in[l, kk * 128:(kk + 1) * 128, c0:c1], [w_in], [w_])
            for t in range(NT):
                h_ = ht[it % 2]
                o_ = ot[it % 2]
                it += 1
                k.dma(sp, h_[:], hT[t], [hT], [h_])
                for n0 in range(0, ncol, 512):
                    n1 = min(n0 + 512, ncol)
                    p = pb[ev % 6]
                    for kk in range(8):
                        k.op(pe, [h_, w_], [p], lambda e: e.matmul(p[:, 0:n1 - n0], lhsT=h_[:, kk, :], rhs=w_[:, kk, n0:n1], start=(kk == 0), stop=(kk == 7)))
                    if ev % 2 == 0:
                        k.op(act, [p], [o_], lambda e: e.copy(out=o_[:, n0:n1], in_=p[:, 0:n1 - n0]))
                    else:
                        k.op(dve, [p], [o_], lambda e: e.tensor_copy(out=o_[:, n0:n1], in_=p[:, 0:n1 - n0]))
                    if c0 + n0 <= C_LR < c0 + n1:
                        off = C_LR - c0 - n0
                        k.op(dve, [p], [lrt, p], lambda e: e.tensor_copy(out=lrt[:], in_=p[:, off:off + 32]))
                        k.dma(sp, lrp[t * 128:(t + 1) * 128, :], lrt[:], [lrt], [lrp])
                    ev += 1
                k.dma(pool, proj[t * 128:(t + 1) * 128, c0:c1], o_[:, 0:ncol], [o_], [proj])
        k.pop()


    def rope_apply(dst, src, rp, nh, blk, tmp):
        G = 128 // (2 * blk)
        sv = src[:].rearrange("p (h g t d) -> p (h g) t d", h=nh, g=G, t=2)
        tv = tmp[:].rearrange("p (h g t d) -> p (h g) t d", h=nh, g=G, t=2)
        cosb = rp[:, 0, :].rearrange("p (o d) -> p o d", o=1).to_broadcast([128, nh, 128])
        sinv = rp[:, 1, :].rearrange("p (o g t d) -> p (o g) t d", o=1, g=G, t=2)
        k.op(dve, [src, rp], [dst], lambda e: e.tensor_mul(out=dst[:].rearrange("p (h d) -> p h d", h=nh), in0=src[:].rearrange("p (h d) -> p h d", h=nh), in1=cosb))
        if G == 1:
            for tt in range(2):
                k.op(dve, [src, rp], [tmp], lambda e: e.tensor_mul(out=tv[:, :, tt, :], in0=sv[:, :, 1 - tt, :], in1=sinv[:, :, tt, :].to_broadcast([128, nh, blk])))
        else:
            s4 = src[:].rearrange("p (h g t d) -> p h g t d", h=nh, g=G, t=2)
            t4 = tmp[:].rearrange("p (h g t d) -> p h g t d", h=nh, g=G, t=2)
            r4 = rp[:, 1, :].rearrange("p (g t d) -> p g t d", g=G, t=2)
            for g in range(G):
                for tt in range(2):
                    k.op(dve, [src, rp], [tmp], lambda e: e.tensor_mul(out=t4[:, :, g, tt, :], in0=s4[:, :, g, 1 - tt, :], in1=r4[:, g, tt, :].rearrange("p (o d) -> p o d", o=1).to_broadcast([128, nh, blk])))
        k.op(dve, [dst, tmp], [dst], lambda e: e.tensor_add(out=dst[:], in0=dst[:], in1=tmp[:]))

    def stage_recur(l, kind):
        k.push()
        is_gla = kind == 0
        cq, cv, cg = (C_GQ, C_GV, C_GR) if is_gla else (C_RQ, C_RV, C_RG)
        ng = gla_norm_g if is_gla else ret_norm_g
        scale = 128.0 ** -0.5
        masks = k.sb([128, 2, 512], F32)
        tri = k.sb([128, 4, 128], F32)
        n16 = k.sb([128, 1], F32)
        gainB = k.sb([128, D], F32)
        k.dma(sp, masks[:], c_masks[0:2].rearrange("m j c -> j m c"), [c_masks], [masks])
        k.dma(sp, tri[:], c_tri[:].rearrange("m j c -> j m c"), [c_tri], [tri])
        k.op(dve, [], [n16], lambda e: e.memset(n16[:], -1.0 / 16.0))
        bcast_load(gainB, ng[l:l + 1, :], ng)
        sp_t = k.sb([128, 512], F32)
        if is_gla:
            WA = k.sb([33, 1024], F32)
            lrT = k.sb([33, 128], F32)
            k.dma(sp, WA[:], wa_aug[l], [wa_aug], [WA])
            k.op(dve, [], [lrT], lambda e: e.memset(lrT[:], 1.0))
        else:
            k.dma(sp, sp_t[:], c_spret[:], [c_spret], [sp_t])
        S = k.sb([128, D], F32)
        Sbf = k.sb([128, D], BF16)
        qk = [k.sb([128, 1024], BF16) for _ in range(2)]
        vt = [k.sb([128, 1024], BF16) for _ in range(2)]
        lrt = [k.sb([128, 32], F32) for _ in range(2)]
        gt = [k.sb([128, 1024], BF16) for _ in range(2)]
        oft = [k.sb([128, 1024], F32) for _ in range(2)]
        rpt = [k.sb([128, 2, 128], F32) for _ in range(2)]
        eq = k.sb([128, 512], F32)
        ekin = k.sb([128, 512], F32)
        ekout = k.sb([128, 512], F32)
        dec = k.sb([128, 4], F32)
        qkr = k.sb([128, 1024], F32)
        t2 = k.sb([128, 1024], F32)
        qkin = k.sb([128, 1024], BF16)
        kout = k.sb([128, 512], BF16)
        qkT = k.sb([128, 8, 128], BF16)
        PT = k.sb([128, 512], BF16)
        osb = k.sb([128, 1024], F32)
        junk = k.sb([128, 1024], F32)
        yn = k.sb([128, 1024], F32)
        sg = k.sb([128, 1024], F32)
        yb = k.sb([128, 1024], BF16)
        yTt = k.sb([128, 8, 128], BF16)
        st = k.sb([128, 32], F32)
        bA = k.ps([128, 512])
        bB = k.ps([128, 512])
        bC = k.ps([128, 512])
        ptr = k.ps([128, 1024], BF16)
        ob = k.ps([128, 1024])
        dsb = k.ps([128, 1024])

        it = 0
        for dirn in range(2):
            order = list(range(NT)) if dirn == 0 else (list(range(NC_ - 1, -1, -1)) + list(range(NT - 1, NC_ - 1, -1)))
            k.op(dve, [], [S], lambda e: e.memset(S[:], 0.0))
            k.op(dve, [], [Sbf], lambda e: e.memset(Sbf[:], 0.0))
            decay_done = False
            for t in order:
                pr = it % 2
                it += 1
                rows = slice(t * 128, (t + 1) * 128)
                qk_, v_, lr_, g_, of_, rp_ = qk[pr], vt[pr], lrt[pr], gt[pr], oft[pr], rpt[pr]
                k.dma(sp, qk_[:], proj[rows, cq:cq + 1024], [proj], [qk_])
                k.dma(sp, v_[:], proj[rows, cv:cv + 1024], [proj], [v_])
                if is_gla:
                    k.dma(sp, lr_[:], lrp[rows, :], [lrp], [lr_])
                else:
                    k.dma(sp, rp_[:], c_rope_r[:, rows, :].rearrange("c t d -> t c d"), [c_rope_r], [rp_])
                if dirn == 1:
                    k.dma(sp, g_[:], proj[rows, cg:cg + 1024], [proj], [g_])
                    k.dma(sp, of_[:], o_part[rows, :], [o_part], [of_])
                if is_gla:
                    k.op(pe, [lr_, ident], [bC], lambda e: e.transpose(bC[0:32, 0:128], lr_[:], ident[:]))
                    k.op(act, [bC], [lrT], lambda e: e.copy(out=lrT[0:32, :], in_=bC[0:32, 0:128]))
                    k.op(pe, [lrT, WA], [bA], lambda e: e.matmul(bA[:], lhsT=lrT[:], rhs=WA[:, dirn * 512:(dirn + 1) * 512], start=True, stop=True))
                    k.op(act, [bA], [sp_t], lambda e: e.activation(out=sp_t[:], in_=bA[:], func=AF.Exp, scale=-1.0))
                    k.op(act, [sp_t], [sp_t], lambda e: e.activation(out=sp_t[:], in_=sp_t[:], func=AF.Ln, bias=1.0))
                if is_gla or not decay_done:
                    decay_done = True
                    k.op(pe, [tri, sp_t], [bB], lambda e: e.matmul(bB[:], lhsT=tri[:, 2 * dirn, :], rhs=sp_t[:], start=True, stop=True))
                    k.op(pe, [tri, sp_t], [bA], lambda e: e.matmul(bA[:], lhsT=tri[:, 2 * dirn + 1, :], rhs=sp_t[:], start=True, stop=True))
                    for h in range(4):
                        k.op(pe, [sp_t, n16], [bC], lambda e: e.matmul(bC[:, 128 + h:129 + h], lhsT=sp_t[:, h * 128:(h + 1) * 128], rhs=n16[:], start=True, stop=True))
                    k.op(act, [bB], [eq], lambda e: e.activation(out=eq[:], in_=bB[:], func=AF.Exp))
                    k.op(act, [bB], [ekin], lambda e: e.activation(out=ekin[:], in_=bB[:], func=AF.Exp, scale=-1.0))
                    k.op(act, [bA], [ekout], lambda e: e.activation(out=ekout[:], in_=bA[:], func=AF.Exp))
                    k.op(act, [bC], [dec], lambda e: e.activation(out=dec[:], in_=bC[:, 128:132], func=AF.Exp))
                if is_gla:
                    src = qk_
                else:
                    rope_apply(qkr, qk_, rp_, 8, 64, t2)
                    src = qkr
                k.op(dve, [src, eq], [qkin], lambda e: e.scalar_tensor_tensor(out=qkin[:, 0:512], in0=src[:, 0:512], scalar=scale, in1=eq[:], op0=ALU.mult, op1=ALU.mult))
                k.op(dve, [src, ekin], [qkin], lambda e: e.tensor_mul(out=qkin[:, 512:1024], in0=src[:, 512:1024], in1=ekin[:]))
                k.op(dve, [src, ekout], [kout], lambda e: e.tensor_mul(out=kout[:], in0=src[:, 512:1024], in1=ekout[:]))
                transpose8(qkin, qkT, ptr, identb, act)
                for h in range(4):
                    k.op(pe, [qkT], [bC], lambda e: e.matmul(bC[:, h * 128:(h + 1) * 128], lhsT=qkT[:, 4 + h, :], rhs=qkT[:, h, :], start=True, stop=True))
                k.op(dve, [bC, masks], [PT], lambda e: e.tensor_mul(out=PT[:], in0=bC[:], in1=masks[:, dirn, :]))
                for h in range(4):
                    hs = slice(h * 256, (h + 1) * 256)
                    k.op(pe, [PT, v_], [ob], lambda e: e.matmul(ob[:, hs], lhsT=PT[:, h * 128:(h + 1) * 128], rhs=v_[:, hs], start=True, stop=False))
                    k.op(pe, [qkT, Sbf], [ob], lambda e: e.matmul(ob[:, hs], lhsT=qkT[:, h, :], rhs=Sbf[:, hs], start=False, stop=True))
                for h in range(4):
                    hs = slice(h * 256, (h + 1) * 256)
                    k.op(pe, [kout, v_], [dsb], lambda e: e.matmul(dsb[:, hs], lhsT=kout[:, h * 128:(h + 1) * 128], rhs=v_[:, hs], start=True, stop=True))
                for h in range(4):
                    hs = slice(h * 256, (h + 1) * 256)
                    k.op(dve, [S, dec, dsb], [S], lambda e: e.scalar_tensor_tensor(out=S[:, hs], in0=S[:, hs], scalar=dec[:, h:h + 1], in1=dsb[:, hs], op0=ALU.mult, op1=ALU.add))
                k.op(act, [S], [Sbf], lambda e: e.copy(out=Sbf[:], in_=S[:]))
                if dirn == 0:
                    k.op(act, [ob], [osb], lambda e: e.copy(out=osb[:], in_=ob[:]))
                    k.dma(pool, o_part[rows, :], osb[:], [osb], [o_part])
                else:
                    k.op(dve, [ob, of_], [osb], lambda e: e.tensor_add(out=osb[:], in0=ob[:], in1=of_[:]))
                    k.op(dve, [osb], [st], lambda e: e.reduce_sum(out=st[:, 0:4], in_=osb[:].rearrange("p (h d) -> p h d", h=4), axis=AX.X))
                    k.op(act, [osb], [junk], lambda e: e.activation(out=junk[:], in_=osb[:], func=AF.Square))
                    k.op(dve, [junk], [st], lambda e: e.reduce_sum(out=st[:, 4:8], in_=junk[:].rearrange("p (h d) -> p h d", h=4), axis=AX.X))
                    k.op(dve, [st], [st], lambda e: e.tensor_scalar(out=st[:, 8:16], in0=st[:, 0:8], scalar1=1.0 / 256.0, scalar2=None, op0=ALU.mult))
                    k.op(dve, [st], [st], lambda e: e.tensor_mul(out=st[:, 16:20], in0=st[:, 8:12], in1=st[:, 8:12]))
                    k.op(dve, [st], [st], lambda e: e.tensor_sub(out=st[:, 20:24], in0=st[:, 12:16], in1=st[:, 16:20]))
                    k.op(dve, [st], [st], lambda e: e.tensor_scalar(out=st[:, 20:24], in0=st[:, 20:24], scalar1=EPS, scalar2=None, op0=ALU.add))
                    k.op(act, [st], [st], lambda e: e.activation(out=st[:, 24:28], in_=st[:, 20:24], func=AF.Sqrt))
                    k.op(dve, [st], [st], lambda e: e.reciprocal(out=st[:, 28:32], in_=st[:, 24:28]))
                    for h in range(4):
                        hs = slice(h * 256, (h + 1) * 256)
                        k.op(dve, [osb, st], [yn], lambda e: e.tensor_scalar(out=yn[:, hs], in0=osb[:, hs], scalar1=st[:, 8 + h:9 + h], scalar2=st[:, 28 + h:29 + h], op0=ALU.subtract, op1=ALU.mult))
                    k.op(act, [g_], [sg], lambda e: e.activation(out=sg[:], in_=g_[:], func=AF.Silu))
                    k.op(dve, [yn, gainB], [yn], lambda e: e.tensor_mul(out=yn[:], in0=yn[:], in1=gainB[:]))
                    k.op(dve, [yn, sg], [yb], lambda e: e.tensor_mul(out=yb[:], in0=yn[:], in1=sg[:]))
                    transpose8(yb, yTt, ptr, identb, act)
                    k.dma(pool, yT[kind][t], yTt[:], [yTt], [yT[kind]])
        k.pop()


    def stage_attn(l, with_ctx):
        k.push()
        scale = 128.0 ** -0.5
        kT_all = k.sb([128, 2, T], BF16)
        v_all = k.sb([128, NT, 256], BF16)
        masks = k.sb([128, 3, 512], F32)
        k.dma(sp, masks[:], c_masks[:].rearrange("m j c -> j m c"), [c_masks], [masks])
        sE = k.sb([128, 8], F32)
        onesf = k.sb([128, 128], F32)
        sinkE = k.sb([128, 8, 128], F32)
        bcast_load(sE, attn_sink[l:l + 1, :], attn_sink)
        k.op(act, [sE], [sE], lambda e: e.activation(out=sE[:], in_=sE[:], func=AF.Exp))
        k.op(dve, [], [onesf], lambda e: e.memset(onesf[:], 1.0))
        for h in range(8):
            k.op(dve, [onesf, sE], [sinkE], lambda e: e.tensor_scalar(out=sinkE[:, h, :], in0=onesf[:], scalar1=sE[:, h:h + 1], scalar2=None, op0=ALU.mult))
        for t0 in range(0, NT, 16):
            t1 = min(NT, t0 + 16)
            k.dma(sp, v_all[:, t0:t1, :], proj[t0 * 128:t1 * 128, C_AV:C_AV + 256].rearrange("(t p) c -> p t c", p=128), [proj], [v_all])
        kt = [k.sb([128, 256], BF16) for _ in range(2)]
        rpt = [k.sb([128, 2, 128], F32) for _ in range(2)]
        kr = k.sb([128, 256], F32)
        tmpk = k.sb([128, 256], F32)
        kb16 = k.sb([128, 256], BF16)
        ptr = k.ps([128, 1024], BF16)
        for t in range(NT):
            k_ = kt[t % 2]
            rp_ = rpt[t % 2]
            rows = slice(t * 128, (t + 1) * 128)
            k.dma(sp, k_[:], proj[rows, C_AK:C_AK + 256], [proj], [k_])
            if t >= NC_:
                n = t - NC_
                k.dma(sp, rp_[:], c_rope_a[:, n * 128:(n + 1) * 128, :].rearrange("c t d -> t c d"), [c_rope_a], [rp_])
                rope_apply(kr, k_, rp_, 2, 32, tmpk)
                k.op(act, [kr], [kb16], lambda e: e.copy(out=kb16[:], in_=kr[:]))
                src = kb16
            else:
                src = k_
            for h in range(2):
                k.op(pe, [src, identb], [ptr], lambda e: e.transpose(ptr[:, h * 128:(h + 1) * 128], src[:, h * 128:(h + 1) * 128], identb[:]))
            k.op(act, [ptr], [kT_all], lambda e: e.copy(out=kT_all[:, :, rows], in_=ptr[:, 0:256].rearrange("p (h t) -> p h t", h=2)))
        qt = [k.sb([128, 1024], BF16) for _ in range(2)]
        qr = k.sb([128, 1024], F32)
        tmpq = k.sb([128, 1024], F32)
        qs = k.sb([128, 1024], BF16)
        qT = k.sb([128, 8, 128], BF16)
        Pt = [k.sb([128, 512], BF16) for _ in range(3)]
        rden = k.sb([128, 512], F32)
        yTt = [k.sb([128, 8, 128], BF16) for _ in range(2)]
        scb = [k.ps([128, 512]) for _ in range(3)]
        outb = [k.ps([128, 512]) for _ in range(2)]
        denb = [k.ps([128, 512]) for _ in range(2)]
        qtiles = list(range(0 if with_ctx else NC_, NT))
        for qi, t in enumerate(qtiles):
            q_ = qt[qi % 2]
            rp_ = rpt[qi % 2]
            y_ = yTt[qi % 2]
            rows = slice(t * 128, (t + 1) * 128)
            k.dma(sp, q_[:], proj[rows, C_AQ:C_AQ + 1024], [proj], [q_])
            if t >= NC_:
                n = t - NC_
                k.dma(sp, rp_[:], c_rope_a[:, n * 128:(n + 1) * 128, :].rearrange("c t d -> t c d"), [c_rope_a], [rp_])
                rope_apply(qr, q_, rp_, 8, 32, tmpq)
                k.op(dve, [qr], [qs], lambda e: e.tensor_scalar(out=qs[:], in0=qr[:], scalar1=scale, scalar2=None, op0=ALU.mult))
            else:
                k.op(dve, [q_], [qs], lambda e: e.tensor_scalar(out=qs[:], in0=q_[:], scalar1=scale, scalar2=None, op0=ALU.mult))
            transpose8(qs, qT, ptr, identb, act)
            kbs = [(c, None) for c in range(NC_)]
            if t >= NC_:
                n = t - NC_
                if n >= 1:
                    kbs.append((t - 1, 2))
                kbs.append((t, None))
                if t + 1 < NT:
                    kbs.append((t + 1, 0))
            items = [(g, kb, mi, j == 0, j == len(kbs) - 1) for g in range(2) for j, (kb, mi) in enumerate(kbs)]

            def emit_sc(ii):
                g, kb, mi, first, last = items[ii]
                sc = scb[ii % 3]
                k.op(pe, [kT_all, qT], [sc], lambda e: e.matmul(sc[:], lhsT=kT_all[:, g, kb * 128:(kb + 1) * 128], rhs=qT[:, 4 * g:4 * g + 4, :].rearrange("p h t -> p (h t)"), start=True, stop=True))

            def emit_pv(ii):
                g, kb, mi, first, last = items[ii]
                sc = scb[ii % 3]
                P = Pt[ii % 3]
                k.op(act, [sc], [P], lambda e: e.activation(out=P[:], in_=sc[:], func=AF.Exp))
                if mi is not None:
                    k.op(dve, [P, masks], [P], lambda e: e.tensor_mul(out=P[:], in0=P[:], in1=masks[:, mi, :]))
                k.op(pe, [v_all, P], [outb[g]], lambda e: e.matmul(outb[g][:], lhsT=v_all[:, kb, g * 128:(g + 1) * 128], rhs=P[:], start=first, stop=last))
                k.op(pe, [onesb, P], [denb[g]], lambda e: e.matmul(denb[g][:], lhsT=onesb[:], rhs=P[:], start=first, stop=last))
                if last:
                    k.op(dve, [denb[g], sinkE], [rden], lambda e: e.tensor_add(out=rden[:], in0=denb[g][:], in1=sinkE[:, 4 * g:4 * g + 4, :].rearrange("p h t -> p (h t)")))
                    k.op(dve, [rden], [rden], lambda e: e.reciprocal(out=rden[:], in_=rden[:]))
                    k.op(dve, [outb[g], rden], [y_], lambda e: e.tensor_mul(out=y_[:, 4 * g:4 * g + 4, :].rearrange("p h t -> p (h t)"), in0=outb[g][:], in1=rden[:]))

            emit_sc(0)
            for ii in range(len(items)):
                if ii + 1 < len(items):
                    emit_sc(ii + 1)
                emit_pv(ii)
            k.dma(pool, yT[1][t], y_[:], [y_], [yT[1]])
        k.pop()


    def load_w_bf16(dst, src2d):
        for kk in range(8):
            k.dma(pool, dst[:, kk, :], src2d[kk * 128:(kk + 1) * 128, :], [w_out], [dst])

    def stage_merge(l, xcur, with_ctx):
        k.push()
        wbr = [k.sb([128, 8, 1024], BF16) for _ in range(3)]
        wo = k.sb([128, 8, 1024], BF16)
        for b in range(3):
            load_w_bf16(wbr[b], w_br[b][l])
        load_w_bf16(wo, w_out[l])
        g1B = [k.sb([128, D], F32) for _ in range(2)]
        for r in range(2):
            bcast_load(g1B[r], modv[r:r + 1, 2 * D:3 * D], modv)
        yTb = [[k.sb([128, 8, 128], BF16) for _ in range(3)] for _ in range(2)]
        gat = [k.sb([128, 3072], BF16) for _ in range(2)]
        xt = [k.sb([128, D], F32) for _ in range(2)]
        sig = k.sb([128, 3072], F32)
        merged = k.sb([128, D], F32)
        tmp = k.sb([128, D], F32)
        mb = k.sb([128, D], BF16)
        mT = k.sb([128, 8, 128], BF16)
        xo = [k.sb([128, D], F32) for _ in range(2)]
        brb = [k.ps([128, 1024]) for _ in range(2)]
        ob = k.ps([128, 1024])
        ptr = k.ps([128, 1024], BF16)
        tiles = list(range(0 if with_ctx else NC_, NT))
        bi = 0
        for i, t in enumerate(tiles):
            pr = i % 2
            rows = slice(t * 128, (t + 1) * 128)
            for b in range(3):
                k.dma(sp, yTb[pr][b][:], yT[b][t], [yT[b]], [yTb[pr][b]])
            k.dma(sp, gat[pr][:], proj[rows, C_GATES:C_GATES + 3072], [proj], [gat[pr]])
            k.dma(sp, xt[pr][:], xcur[rows, :], [xcur], [xt[pr]])
            k.op(act, [gat[pr]], [sig], lambda e: e.activation(out=sig[:], in_=gat[pr][:], func=AF.Sigmoid))
            for b in range(3):
                pb_ = brb[bi % 2]
                bi += 1
                y_ = yTb[pr][b]
                for half in range(2):
                    for kk in range(8):
                        k.op(pe, [y_, wbr[b]], [pb_], lambda e: e.matmul(pb_[:, half * 512:(half + 1) * 512], lhsT=y_[:, kk, :], rhs=wbr[b][:, kk, half * 512:(half + 1) * 512], start=(kk == 0), stop=(kk == 7)))
                if b == 0:
                    k.op(dve, [pb_, sig], [merged], lambda e: e.tensor_mul(out=merged[:], in0=pb_[:], in1=sig[:, 0:1024]))
                else:
                    k.op(dve, [pb_, sig], [tmp], lambda e: e.tensor_mul(out=tmp[:], in0=pb_[:], in1=sig[:, b * 1024:(b + 1) * 1024]))
                    k.op(dve, [merged, tmp], [merged], lambda e: e.tensor_add(out=merged[:], in0=merged[:], in1=tmp[:]))
            k.op(act, [merged], [mb], lambda e: e.copy(out=mb[:], in_=merged[:]))
            transpose8(mb, mT, ptr, identb, act)
            for half in range(2):
                for kk in range(8):
                    k.op(pe, [mT, wo], [ob], lambda e: e.matmul(ob[:, half * 512:(half + 1) * 512], lhsT=mT[:, kk, :], rhs=wo[:, kk, half * 512:(half + 1) * 512], start=(kk == 0), stop=(kk == 7)))
            gB = g1B[1] if t < NC_ else g1B[0]
            k.op(dve, [ob, gB], [tmp], lambda e: e.tensor_mul(out=tmp[:], in0=ob[:], in1=gB[:]))
            k.op(dve, [tmp, xt[pr]], [xo[pr]], lambda e: e.tensor_add(out=xo[pr][:], in0=tmp[:], in1=xt[pr][:]))
            k.dma(pool, xm[rows, :], xo[pr][:], [xo[pr]], [xm])
        k.pop()


    def stage_moe(l, xnext, last):
        k.push()
        GS = 12
        A_l, sh_l, A_c, sh_c = mod_tiles(l, 2, norm2_g)
        g2B = [k.sb([128, D], F32) for _ in range(2)]
        for r in range(2):
            bcast_load(g2B[r], modv[r:r + 1, 5 * D:6 * D], modv)
        if last:
            fgB = k.sb([128, D], F32)
            bcast_load(fgB, final_g[0:1, :], final_g)
        wr = k.sb([128, 8, 36], F32)
        brB = k.sb([128, 36], F32)
        k.dma(sp, wr[:], w_r[l].rearrange("(k p) n -> p k n", p=128), [w_r], [wr])
        bcast_load(brB, b_r[l:l + 1, :], b_r)
        acc = k.sb([128, GS, D], F32)
        hTb = k.sb([128, 8, GS * 128], BF16)
        Gd = k.sb([128, GS, 32], F32)
        W = [k.sb([128, 8, 1024], BF16) for _ in range(3)]
        uT = k.sb([128, 8, 512], BF16)
        sgl = [k.sb([128, 512], F32) for _ in range(2)]
        xt = [k.sb([128, D], F32) for _ in range(2)]
        junk = k.sb([128, D], F32)
        h2 = k.sb([128, D], F32)
        h2T = k.sb([128, 8, 128], F32)
        st = k.sb([128, 4], F32)
        rt = k.sb([128, 64], F32)
        lgs = k.sb([128, 36], F32)
        em = k.sb([128, 32], F32)
        em2 = k.sb([128, 32], F32)
        oh1 = k.sb([128, 32], F32)
        oh2 = k.sb([128, 32], F32)
        ptrf = k.ps([128, 1024])
        gvb = [k.ps([128, 512]) for _ in range(4)]
        yb = k.ps([128, 1024])
        tiles = list(range(NC_ if last else 0, NT))
        groups = [tiles[i:i + GS] for i in range(0, len(tiles), GS)]
        for grp in groups:
            for sl, t in enumerate(grp):
                x_ = xt[sl % 2]
                rows = slice(t * 128, (t + 1) * 128)
                k.dma(sp, x_[:], xm[rows, :], [xm], [x_])
                A, sh = (A_c, sh_c) if t < NC_ else (A_l, sh_l)
                rms_mod(x_, A, sh, junk, st, h2)
                transpose8(h2, h2T, ptrf, ident, act)
                k.op(dve, [h2T], [hTb], lambda e: e.tensor_copy(out=hTb[:, :, sl * 128:(sl + 1) * 128], in_=h2T[:]))
                for kk in range(8):
                    k.op(pe, [h2T, wr], [ptrf], lambda e: e.matmul(ptrf[:, 0:36], lhsT=h2T[:, kk, :], rhs=wr[:, kk, :], start=(kk == 0), stop=(kk == 7)))
                k.op(dve, [ptrf, brB], [lgs], lambda e: e.tensor_add(out=lgs[:], in0=ptrf[:, 0:36], in1=brB[:]))
                k.op(dve, [lgs], [rt], lambda e: e.reduce_max(out=rt[:, 0:1], in_=lgs[:, 0:4], axis=AX.X))
                k.op(dve, [lgs, rt], [rt], lambda e: e.tensor_scalar(out=rt[:, 4:8], in0=lgs[:, 0:4], scalar1=rt[:, 0:1], scalar2=None, op0=ALU.is_equal))
                k.op(dve, [rt], [rt], lambda e: e.tensor_scalar(out=rt[:, 1:2], in0=rt[:, 0:1], scalar1=-1.0, scalar2=None, op0=ALU.mult))
                k.op(act, [lgs, rt], [rt], lambda e: e.activation(out=rt[:, 8:12], in_=lgs[:, 0:4], func=AF.Exp, bias=rt[:, 1:2], scale=1.0, accum_out=rt[:, 2:3]))
                k.op(dve, [rt], [rt], lambda e: e.reciprocal(out=rt[:, 3:4], in_=rt[:, 2:3]))
                k.op(dve, [rt], [rt], lambda e: e.tensor_scalar(out=rt[:, 12:16], in0=rt[:, 4:8], scalar1=1.0, scalar2=BIG, op0=ALU.subtract, op1=ALU.mult))
                k.op(dve, [lgs, rt], [em], lambda e: e.tensor_tensor(out=em[:].rearrange("p (g e) -> p g e", g=4), in0=lgs[:, 4:36].rearrange("p (g e) -> p g e", g=4), in1=rt[:, 12:16].rearrange("p (g o) -> p g o", o=1).to_broadcast([128, 4, 8]), op=ALU.add))
                k.op(dve, [em], [rt], lambda e: e.reduce_max(out=rt[:, 16:17], in_=em[:], axis=AX.X))
                k.op(dve, [em, rt], [oh1], lambda e: e.tensor_scalar(out=oh1[:], in0=em[:], scalar1=rt[:, 16:17], scalar2=None, op0=ALU.is_equal))
                k.op(dve, [oh1, em], [em2], lambda e: e.scalar_tensor_tensor(out=em2[:], in0=oh1[:], scalar=-BIG, in1=em[:], op0=ALU.mult, op1=ALU.add))
                k.op(dve, [em2], [rt], lambda e: e.reduce_max(out=rt[:, 17:18], in_=em2[:], axis=AX.X))
                k.op(dve, [em2, rt], [oh2], lambda e: e.tensor_scalar(out=oh2[:], in0=em2[:], scalar1=rt[:, 17:18], scalar2=None, op0=ALU.is_equal))
                k.op(dve, [rt], [rt], lambda e: e.tensor_sub(out=rt[:, 18:19], in0=rt[:, 17:18], in1=rt[:, 16:17]))
                k.op(act, [rt], [rt], lambda e: e.activation(out=rt[:, 19:20], in_=rt[:, 18:19], func=AF.Exp))
                k.op(dve, [rt], [rt], lambda e: e.tensor_scalar(out=rt[:, 20:21], in0=rt[:, 19:20], scalar1=1.0, scalar2=None, op0=ALU.add))
                k.op(dve, [rt], [rt], lambda e: e.reciprocal(out=rt[:, 21:22], in_=rt[:, 20:21]))
                k.op(dve, [rt], [rt], lambda e: e.tensor_mul(out=rt[:, 22:23], in0=rt[:, 19:20], in1=rt[:, 21:22]))
                k.op(dve, [rt], [rt], lambda e: e.tensor_mul(out=rt[:, 23:24], in0=rt[:, 21:22], in1=rt[:, 3:4]))
                k.op(dve, [rt], [rt], lambda e: e.tensor_mul(out=rt[:, 24:25], in0=rt[:, 22:23], in1=rt[:, 3:4]))
                k.op(dve, [oh1, rt], [Gd], lambda e: e.tensor_scalar(out=Gd[:, sl, :], in0=oh1[:], scalar1=rt[:, 23:24], scalar2=None, op0=ALU.mult))
                k.op(dve, [oh2, rt, Gd], [Gd], lambda e: e.scalar_tensor_tensor(out=Gd[:, sl, :], in0=oh2[:], scalar=rt[:, 24:25], in1=Gd[:, sl, :], op0=ALU.mult, op1=ALU.add))
            ng_ = len(grp)
            blocks = [(b0, min(b0 + 4, ng_)) for b0 in range(0, ng_, 4)]
            gi = 0
            for ex in range(NEXP):
                load_w_bf16(W[0], moe_w1[l, ex])
                load_w_bf16(W[1], moe_w3[l, ex])
                load_w_bf16(W[2], moe_w2[l, ex])
                for (b0, b1) in blocks:
                    N = (b1 - b0) * 128
                    cols = slice(b0 * 128, b1 * 128)
                    for hc in range(8):
                        gb = gvb[gi % 4]
                        vb = gvb[(gi + 1) % 4]
                        sg_ = sgl[(gi // 2) % 2]
                        gi += 2
                        for kk in range(8):
                            k.op(pe, [W[0], hTb], [gb], lambda e: e.matmul(gb[:, 0:N], lhsT=W[0][:, kk, hc * 128:(hc + 1) * 128], rhs=hTb[:, kk, cols], start=(kk == 0), stop=(kk == 7)))
                        for kk in range(8):
                            k.op(pe, [W[1], hTb], [vb], lambda e: e.matmul(vb[:, 0:N], lhsT=W[1][:, kk, hc * 128:(hc + 1) * 128], rhs=hTb[:, kk, cols], start=(kk == 0), stop=(kk == 7)))
                        k.op(act, [gb], [sg_], lambda e: e.activation(out=sg_[:, 0:N], in_=gb[:, 0:N], func=AF.Silu))
                        k.op(dve, [sg_, vb], [uT], lambda e: e.tensor_mul(out=uT[:, hc, 0:N], in0=sg_[:, 0:N], in1=vb[:, 0:N]))
                    for sl in range(b0, b1):
                        tc_ = slice((sl - b0) * 128, (sl - b0 + 1) * 128)
                        for half in range(2):
                            for hc in range(8):
                                k.op(pe, [uT, W[2]], [yb], lambda e: e.matmul(yb[:, half * 512:(half + 1) * 512], lhsT=uT[:, hc, tc_], rhs=W[2][:, hc, half * 512:(half + 1) * 512], start=(hc == 0), stop=(hc == 7)))
                        if ex == 0:
                            k.op(dve, [yb, Gd], [acc], lambda e: e.tensor_scalar(out=acc[:, sl, :], in0=yb[:], scalar1=Gd[:, sl, ex:ex + 1], scalar2=None, op0=ALU.mult))
                        else:
                            k.op(dve, [yb, Gd, acc], [acc], lambda e: e.scalar_tensor_tensor(out=acc[:, sl, :], in0=yb[:], scalar=Gd[:, sl, ex:ex + 1], in1=acc[:, sl, :], op0=ALU.mult, op1=ALU.add))
            for sl, t in enumerate(grp):
                x_ = xt[sl % 2]
                rows = slice(t * 128, (t + 1) * 128)
                k.dma(sp, x_[:], xm[rows, :], [xm], [x_])
                gB = g2B[1] if t < NC_ else g2B[0]
                k.op(dve, [acc, gB], [junk], lambda e: e.tensor_mul(out=junk[:], in0=acc[:, sl, :], in1=gB[:]))
                k.op(dve, [junk, x_], [h2], lambda e: e.tensor_add(out=h2[:], in0=junk[:], in1=x_[:]))
                if not last:
                    k.dma(pool, xnext[rows, :], h2[:], [h2], [xnext])
                else:
                    k.op(act, [h2], [junk, st], lambda e: e.activation(out=junk[:], in_=h2[:], func=AF.Square, accum_out=st[:, 0:1]))
                    k.op(dve, [st], [st], lambda e: e.tensor_scalar(out=st[:, 1:2], in0=st[:, 0:1], scalar1=1.0 / D, scalar2=EPS, op0=ALU.mult, op1=ALU.add))
                    k.op(act, [st], [st], lambda e: e.activation(out=st[:, 2:3], in_=st[:, 1:2], func=AF.Sqrt))
                    k.op(dve, [st], [st], lambda e: e.reciprocal(out=st[:, 3:4], in_=st[:, 2:3]))
                    k.op(dve, [h2, st, fgB], [junk], lambda e: e.scalar_tensor_tensor(out=junk[:], in0=h2[:], scalar=st[:, 3:4], in1=fgB[:], op0=ALU.mult, op1=ALU.mult))
                    k.dma(pool, out[(t - NC_) * 128:(t - NC_ + 1) * 128, :], junk[:], [junk], [out])
        k.pop()

    layers = list(range(n_layers))
    xcur = xin
    for l in layers:
        last = l == n_layers - 1
        stage_ada(l)
        if stop >= 2:
            stage_norm1(l, xcur)
        if stop >= 3:
            stage_proj(l)
        if stop >= 4:
            stage_recur(l, 0)
        if stop >= 5:
            stage_recur(l, 2)
        if stop >= 6:
            stage_attn(l, not last)
        if stop >= 7:
            stage_merge(l, xcur, not last)
        if stop >= 8:
            stage_moe(l, xn, last)
        xcur = xn
        if stop < 99:
            break
    if stop < 99:
        k.push()
        tmp = k.sb([128, D], F32)
        for t in range(L // 128):
            k.dma(sp, tmp[:], xin[CTX + t * 128:CTX + (t + 1) * 128, :], [xin], [tmp])
            k.dma(sp, out[t * 128:(t + 1) * 128, :], tmp[:], [tmp], [out])
        k.pop()
    k.barrier()
    return k, dbg_out


def _const_tables(L):
    T = CTX + L
    j = np.arange(128)[:, None]
    i = np.arange(128)[None, :]
    le = (j <= i).astype(np.float32)
    gt = (j > i).astype(np.float32)
    ge = (j >= i).astype(np.float32)
    lt = (j < i).astype(np.float32)
    masks = np.stack([np.tile(m, (1, 4)) for m in (le, gt, ge)]).astype(np.float32)
    tri = (np.stack([le, gt, ge, lt]) * (-1.0 / 16.0)).astype(np.float32)
    ld = np.log(1.0 - np.exp2(-5.0 - np.arange(4, dtype=np.float32))).astype(np.float32)
    spret = np.repeat((-16.0 * ld)[None, :], 128, axis=1).reshape(1, 512)
    spret = np.broadcast_to(np.repeat(-16.0 * ld, 128)[None, :], (128, 512)).astype(np.float32)

    def tab(pos, half):
        inv = (10000.0 ** (-np.arange(half, dtype=np.float32) / half)).astype(np.float32)
        ang = pos.astype(np.float32)[:, None] * inv[None, :]
        return np.cos(ang).astype(np.float32), np.sin(ang).astype(np.float32)

    rows = np.arange(L) // 64
    cols = np.arange(L) % 64
    cr, sr = tab(rows, 32)
    cc, sc = tab(cols, 32)
    rope_a = np.stack([np.concatenate([cr, cr, cc, cc], 1), np.concatenate([-sr, sr, -sc, sc], 1)]).astype(np.float32)
    c2, s2 = tab(np.arange(T), 64)
    rope_r = np.stack([np.concatenate([c2, c2], 1), np.concatenate([-s2, s2], 1)]).astype(np.float32)
    return dict(c_ident=np.eye(128, dtype=np.float32), c_masks=masks, c_tri=tri, c_spret=spret,
                c_rope_a=rope_a, c_rope_r=rope_r)


def prep_inputs(inp, b, L):
    f = lambda a: np.ascontiguousarray(np.asarray(a, dtype=np.float32))
    m = {}
    m["xin"] = f(np.concatenate([inp["ctx"][b], inp["x"][b][:L]], axis=0))
    cc = np.stack([np.asarray(inp["c"][b]), np.asarray(inp["c_ctx"])])
    m["ccT"] = f(cc.reshape(2, 8, 128).transpose(2, 1, 0).reshape(128, 16))
    for n in ("w_ada", "b_ada", "norm1_g", "norm2_g", "w_in", "gla_norm_g", "ret_norm_g", "attn_sink",
              "w_br_gla", "w_br_attn", "w_br_ret", "w_out", "moe_w1", "moe_w3", "moe_w2"):
        if n in inp:
            m[n] = f(inp[n])
    wa = np.zeros((2, 33, 1024), np.float32)
    wa[:, 0:16, 0:512] = inp["gla_wa2"][:, 0]
    wa[:, 16:32, 512:1024] = inp["gla_wa2"][:, 1]
    wa[:, 32, 0:512] = inp["gla_ba"][:, 0]
    wa[:, 32, 512:1024] = inp["gla_ba"][:, 1]
    m["wa_aug"] = wa
    m["w_r"] = f(np.concatenate([inp["moe_w_grp"], inp["moe_w_exp"]], axis=-1))
    m["b_r"] = f(np.concatenate([inp["moe_b_grp"], inp["moe_b_exp"]], axis=-1))
    m["final_g"] = f(np.asarray(inp["final_g"]).reshape(1, D))
    return m


_CACHE = {}


def kernel(**inputs):
    L = 8192
    B = 8
    if "prog" not in _CACHE:
        _CACHE["prog"] = build(L)
        _CACHE["const"] = _const_tables(L)
    k, _ = _CACHE["prog"]
    inp = {n: np.asarray(v) for n, v in inputs.items()}
    shared = None
    in_maps = []
    for b in range(B):
        m = prep_inputs(inp, b, L)
        if shared is None:
            shared = {n: v for n, v in m.items() if n not in ("xin", "ccT")}
        else:
            for n in shared:
                m[n] = shared[n]
        m.update(_CACHE["const"])
        in_maps.append(m)
    res = run_bass_kernel_spmd(k.nc, in_maps, core_ids=list(range(B)))
    return np.stack([np.asarray(r["out"]) for r in res.results]).astype(np.float32)
```

```python
import numpy as np
import concourse.bass as bass
import concourse.mybir as mybir
from concourse.bass_utils import run_bass_kernel_spmd
from contextlib import ExitStack

F32 = mybir.dt.float32
BF16 = mybir.dt.bfloat16
I32 = mybir.dt.int32
AF = mybir.ActivationFunctionType
ALU = mybir.AluOpType
AX = mybir.AxisListType

D = 1024
CTX = 256
NIN = 10784
EPS = 1e-6
C_GQ, C_GK, C_GV, C_GR, C_LR = 0, 512, 1024, 2048, 3072
C_AQ, C_AK, C_AV = 3104, 4128, 4384
C_RQ, C_RK, C_RV, C_RG = 4640, 5152, 5664, 6688
C_GATES = 7712
BIG = 1.0e4


class Buf:
    __slots__ = ("t", "w", "r", "name", "dsem", "excl", "isdram")

    def __init__(self, t, name):
        self.t = t
        self.name = name
        self.w = {}
        self.r = {}
        self.dsem = None
        self.isdram = False
        self.excl = False

    def __getitem__(self, idx):
        return self.t[idx]


class Eng:
    def __init__(self, name, eng, sem, is_pe=False):
        self.name = name
        self.eng = eng
        self.sem = sem
        self.cnt = 0
        self.waited = {}
        self.is_pe = is_pe


class K:
    def __init__(self):
        self.nc = bass.Bass("TRN2", target_bir_lowering=False)
        self.es = ExitStack()
        self.scopes = []
        nc = self.nc
        self.sems = {}
        self.dcnt = {}
        self.free_dsems = []
        self.scope_dsems = []
        self.nsem = 0
        self.pe = Eng("pe", nc.tensor, self.newsem("pe"), True)
        self.act = Eng("act", nc.scalar, self.newsem("act"))
        self.dve = Eng("dve", nc.vector, self.newsem("dve"))
        self.pool = Eng("pool", nc.gpsimd, self.newsem("pool"))
        self.sp = Eng("sp", nc.sync, self.newsem("sp"))
        self.engs = [self.pe, self.act, self.dve, self.pool, self.sp]
        self.nbuf = 0

    def newsem(self, name):
        s = self.es.enter_context(self.nc.semaphore(name))
        self.sems[name] = s
        self.nsem += 1
        return name

    def _stack(self):
        return self.scopes[-1] if self.scopes else self.es

    def sb(self, shape, dt, name=None):
        self.nbuf += 1
        name = name or f"sb{self.nbuf}"
        return Buf(self._stack().enter_context(self.nc.sbuf_tensor(name, list(shape), dt)), name)

    def ps(self, shape, dt=F32, name=None):
        self.nbuf += 1
        name = name or f"ps{self.nbuf}"
        b = Buf(self._stack().enter_context(self.nc.psum_tensor(name, list(shape), dt)), name)
        b.excl = True
        return b

    def dram(self, name, shape, dt, kind="Internal"):
        t = self.nc.dram_tensor(name, list(shape), dt, kind=kind)
        b = Buf(t.ap(), name)
        b.isdram = True
        return b

    def _sync(self, E, reads, writes):
        need = {}
        for b in reads:
            for s, v in b.w.items():
                if need.get(s, 0) < v:
                    need[s] = v
            if b.excl:
                for s, v in b.r.items():
                    if s != E.sem and need.get(s, 0) < v:
                        need[s] = v
        for b in writes:
            for s, v in b.w.items():
                if need.get(s, 0) < v:
                    need[s] = v
            for s, v in b.r.items():
                if need.get(s, 0) < v:
                    need[s] = v
        for s, v in need.items():
            if E.is_pe and s == E.sem:
                continue
            if E.waited.get(s, 0) < v:
                E.eng.wait_ge(self.sems[s], v)
                E.waited[s] = v

    def op(self, E, reads, writes, fn):
        self._sync(E, reads, writes)
        ins = fn(E.eng)
        E.cnt += 1
        ins.then_inc(self.sems[E.sem], 1)
        for b in reads:
            b.r[E.sem] = E.cnt
        for b in writes:
            b.w = {E.sem: E.cnt}
            b.r = {}
        return ins

    def dma(self, Q, out, in_, reads, writes, semb=None, **kw):
        self._sync(Q, reads, writes)
        semb = semb or (writes + reads)[0]
        if semb.dsem is None:
            if self.free_dsems:
                semb.dsem = self.free_dsems.pop()
            else:
                semb.dsem = self.newsem(f"d{self.nsem}")
                self.dcnt[semb.dsem] = 0
            if self.scopes:
                self.scope_dsems[-1].append(semb.dsem)
        ins = Q.eng.dma_start(out=out, in_=in_, **kw)
        self.dcnt[semb.dsem] += 16
        v = self.dcnt[semb.dsem]
        ins.then_inc(self.sems[semb.dsem], 16)
        for b in reads:
            b.r[semb.dsem] = v
        for b in writes:
            if b.isdram:
                b.w[semb.dsem] = v
            else:
                b.w = {semb.dsem: v}
                b.r = {}
        return ins

    def idma(self, out, in_, reads, writes, semb, out_off=None, in_off=None, bound=0):
        Q = self.pool
        self._sync(Q, reads, writes)
        if semb.dsem is None:
            if self.free_dsems:
                semb.dsem = self.free_dsems.pop()
            else:
                semb.dsem = self.newsem(f"d{self.nsem}")
                self.dcnt[semb.dsem] = 0
            if self.scopes:
                self.scope_dsems[-1].append(semb.dsem)
        if not hasattr(self, "bregs"):
            self.bregs = {}
        if bound not in self.bregs:
            self.bregs[bound] = Q.eng.to_reg(bound)
        ins = Q.eng.indirect_dma_start(out=out, out_offset=out_off, in_=in_, in_offset=in_off,
                                       bounds_check=self.bregs[bound], oob_is_err=False)
        self.dcnt[semb.dsem] += 16
        v = self.dcnt[semb.dsem]
        ins.then_inc(self.sems[semb.dsem], 16)
        for b in reads:
            b.r[semb.dsem] = v
        for b in writes:
            if b.isdram:
                b.w[semb.dsem] = v
            else:
                b.w = {semb.dsem: v}
                b.r = {}
        return ins

    def barrier(self):
        for E in self.engs:
            for E2 in self.engs:
                if E2 is E or E2.cnt == 0:
                    continue
                if E.waited.get(E2.sem, 0) < E2.cnt:
                    E.eng.wait_ge(self.sems[E2.sem], E2.cnt)
                    E.waited[E2.sem] = E2.cnt
            for s, v in self.dcnt.items():
                if v and E.waited.get(s, 0) < v:
                    E.eng.wait_ge(self.sems[s], v)
                    E.waited[s] = v

    def push(self):
        self.scopes.append(ExitStack())
        self.scope_dsems.append([])

    def pop(self):
        self.barrier()
        self.scopes.pop().close()
        self.free_dsems.extend(self.scope_dsems.pop())


def build(L, n_layers=2, dbg=(), stop=99, NEXP=32, dense_moe=False):
    T = CTX + L
    NT = T // 128
    NC_ = CTX // 128
    k = K()
    nc = k.nc
    pe, act, dve, pool, sp = k.pe, k.act, k.dve, k.pool, k.sp

    def inp(name, shape, dt=F32):
        return k.dram(name, shape, dt, kind="ExternalInput")

    xin = inp("xin", [T, D])
    ccT = inp("ccT", [128, 16])
    w_ada = inp("w_ada", [2, D, 6 * D])
    b_ada = inp("b_ada", [2, 6 * D])
    norm1_g = inp("norm1_g", [2, D])
    norm2_g = inp("norm2_g", [2, D])
    w_in = inp("w_in", [2, D, NIN])
    wa_aug = inp("wa_aug", [2, 33, 1024])
    gla_norm_g = inp("gla_norm_g", [2, D])
    ret_norm_g = inp("ret_norm_g", [2, D])
    attn_sink = inp("attn_sink", [2, 8])
    w_br = [inp("w_br_gla", [2, D, D]), inp("w_br_attn", [2, D, D]), inp("w_br_ret", [2, D, D])]
    w_out = inp("w_out", [2, D, D])
    w_r = inp("w_r", [2, D, 36])
    b_r = inp("b_r", [2, 36])
    if "nomoe" not in dbg:
        moe_w1 = inp("moe_w1", [2 * 32 * D, D])
        moe_w3 = inp("moe_w3", [2 * 32 * D, D])
        moe_w2 = inp("moe_w2", [2 * 32 * D, D])
    final_g = inp("final_g", [1, D])
    c_masks = inp("c_masks", [3, 128, 512])
    c_tri = inp("c_tri", [4, 128, 128])
    c_spret = inp("c_spret", [128, 512])
    c_rope_a = inp("c_rope_a", [2, L, 128])
    c_rope_r = inp("c_rope_r", [2, T, 128])
    c_bstart = inp("c_bstart", [1, 80])
    c_wbase = inp("c_wbase", [128, 8])
    c_ident = inp("c_ident", [128, 128])

    out = k.dram("out", [L, D], F32, kind="ExternalOutput")
    dbg_out = {}

    def scratch(name, shape, dt):
        kind = "ExternalOutput" if name in dbg else "Internal"
        b = k.dram(name, shape, dt, kind=kind)
        if name in dbg:
            dbg_out[name] = b
        return b

    modv = scratch("modv", [2, 6 * D], F32)
    hT = scratch("hT", [NT, 128, 8, 128], BF16)
    proj = scratch("proj", [T, NIN], BF16)
    lrp = scratch("lrp", [T, 32], F32)
    o_part = scratch("o_part", [T, D], F32)
    yT = [scratch("yT_gla", [NT, 128, 8, 128], BF16), scratch("yT_att", [NT, 128, 8, 128], BF16),
          scratch("yT_ret", [NT, 128, 8, 128], BF16)]
    xm = scratch("xm", [T, D], F32)
    BLK = 512
    NBLKMAX = (2 * T + 32 * (BLK - 1) + BLK - 1) // BLK
    h2d = scratch("h2d", [T, D], BF16)
    xs = scratch("xs", [NBLKMAX * BLK, D], BF16)
    ys = scratch("ys", [NBLKMAX * BLK, D], F32)
    xn = scratch("xn", [T, D], F32)

    ident = k.sb([128, 128], F32, "ident")
    identb = k.sb([128, 128], BF16, "identb")
    onesb = k.sb([128, 128], BF16, "onesb")
    k.dma(sp, ident[:], c_ident[:], [c_ident], [ident])
    k.op(dve, [ident], [identb], lambda e: e.tensor_copy(out=identb[:], in_=ident[:]))
    k.op(dve, [], [onesb], lambda e: e.memset(onesb[:], 1.0))

    def bcast_load(dst, src_ap, srcbuf, q=None):
        k.dma(q or sp, dst[:], src_ap.partition_broadcast(128), [srcbuf], [dst])

    def stage_ada(l):
        k.push()
        cc = k.sb([128, 16], F32)
        sc = k.sb([128, 16], F32)
        wbuf = [k.sb([128, 8, 512], F32) for _ in range(2)]
        bb = k.sb([1, 6 * D], F32)
        one1 = k.sb([1, 2], F32)
        res = [k.sb([2, 512], F32) for _ in range(2)]
        pp = [k.ps([2, 512]) for _ in range(2)]
        k.dma(sp, cc[:], ccT[:], [ccT], [cc])
        k.dma(sp, bb[:], b_ada[l:l + 1, :], [b_ada], [bb])
        k.op(dve, [], [one1], lambda e: e.memset(one1[:], 1.0))
        k.op(act, [cc], [sc], lambda e: e.activation(out=sc[:], in_=cc[:], func=AF.Silu))
        scv = sc[:].rearrange("p (k r) -> p k r", r=2)
        for cg in range(12):
            wb = wbuf[cg % 2]
            k.dma(sp, wb[:], w_ada[l, :, cg * 512:(cg + 1) * 512].rearrange("(k p) n -> p k n", p=128), [w_ada], [wb])
            p = pp[cg % 2]
            r = res[cg % 2]
            for kk in range(8):
                k.op(pe, [sc, wb], [p], lambda e: e.matmul(p[:], lhsT=scv[:, kk, :], rhs=wb[:, kk, :], start=(kk == 0), stop=False))
            k.op(pe, [one1, bb], [p], lambda e: e.matmul(p[:], lhsT=one1[:], rhs=bb[:, cg * 512:(cg + 1) * 512], start=False, stop=True))
            k.op(act, [p], [r], lambda e: e.copy(out=r[:], in_=p[:]))
            k.dma(sp, modv[:, cg * 512:(cg + 1) * 512], r[:], [r], [modv])
        k.pop()

    def mod_tiles(l, which, ng):
        base = 0 if which == 1 else 3
        gB = k.sb([128, D], F32)
        bcast_load(gB, ng[l:l + 1, :], ng)
        outt = []
        for r in range(2):
            A = k.sb([128, D], F32)
            sh = k.sb([128, D], F32)
            bcast_load(A, modv[r:r + 1, (base + 1) * D:(base + 2) * D], modv)
            bcast_load(sh, modv[r:r + 1, base * D:(base + 1) * D], modv)
            k.op(dve, [A, gB], [A], lambda e: e.scalar_tensor_tensor(out=A[:], in0=A[:], scalar=1.0, in1=gB[:], op0=ALU.add, op1=ALU.mult))
            outt += [A, sh]
        return outt

    def rms_mod(xt, A, sh, junk, st, hout):
        k.op(act, [xt], [junk, st], lambda e: e.activation(out=junk[:], in_=xt[:], func=AF.Square, accum_out=st[:, 0:1]))
        k.op(dve, [st], [st], lambda e: e.tensor_scalar(out=st[:, 1:2], in0=st[:, 0:1], scalar1=1.0 / D, scalar2=EPS, op0=ALU.mult, op1=ALU.add))
        k.op(act, [st], [st], lambda e: e.activation(out=st[:, 2:3], in_=st[:, 1:2], func=AF.Sqrt))
        k.op(dve, [st], [st], lambda e: e.reciprocal(out=st[:, 3:4], in_=st[:, 2:3]))
        k.op(dve, [xt, st, A], [junk], lambda e: e.scalar_tensor_tensor(out=junk[:], in0=xt[:], scalar=st[:, 3:4], in1=A[:], op0=ALU.mult, op1=ALU.mult))
        k.op(dve, [junk, sh], [hout], lambda e: e.tensor_add(out=hout[:], in0=junk[:], in1=sh[:]))

    def transpose8(src, dstT, ptr, idt, evac):
        for c in range(8):
            k.op(pe, [src, idt], [ptr], lambda e: e.transpose(ptr[:, c * 128:(c + 1) * 128], src[:, c * 128:(c + 1) * 128], idt[:]))
        if evac is act:
            k.op(act, [ptr], [dstT], lambda e: e.copy(out=dstT[:].rearrange("p k t -> p (k t)"), in_=ptr[:]))
        else:
            k.op(dve, [ptr], [dstT], lambda e: e.tensor_copy(out=dstT[:].rearrange("p k t -> p (k t)"), in_=ptr[:]))

    def stage_norm1(l, xcur):
        k.push()
        A_l, sh_l, A_c, sh_c = mod_tiles(l, 1, norm1_g)
        xt = [k.sb([128, D], F32) for _ in range(2)]
        junk = k.sb([128, D], F32)
        st = k.sb([128, 4], F32)
        hb = k.sb([128, D], BF16)
        hTt = [k.sb([128, 8, 128], BF16) for _ in range(2)]
        ptr = k.ps([128, 1024], BF16)
        for t in range(NT):
            x_ = xt[t % 2]
            k.dma(sp, x_[:], xcur[t * 128:(t + 1) * 128, :], [xcur], [x_])
            A, sh = (A_c, sh_c) if t < NC_ else (A_l, sh_l)
            rms_mod(x_, A, sh, junk, st, hb)
            h_ = hTt[t % 2]
            transpose8(hb, h_, ptr, identb, act)
            k.dma(pool, hT[t], h_[:], [h_], [hT])
        k.pop()

    def stage_proj(l):
        k.push()
        PW = 2048
        passes = [(c0, min(c0 + PW, NIN)) for c0 in range(0, NIN, PW)]
        wt = [k.sb([128, 8, PW], BF16) for _ in range(2)]
        ht = [k.sb([128, 8, 128], BF16) for _ in range(2)]
        ot = [k.sb([128, PW], BF16) for _ in range(2)]
        lrt = k.sb([128, 32], F32)
        pb = [k.ps([128, 512]) for _ in range(6)]
        ev = 0
        it = 0
        for pi, (c0, c1) in enumerate(passes):
            w_ = wt[pi % 2]
            ncol = c1 - c0
            for kk in range(8):
                k.dma(pool, w_[:, kk, 0:ncol], w_in[l, kk * 128:(kk + 1) * 128, c0:c1], [w_in], [w_])
            for t in range(NT):
                h_ = ht[it % 2]
                o_ = ot[it % 2]
                it += 1
                k.dma(sp, h_[:], hT[t], [hT], [h_])
                for n0 in range(0, ncol, 512):
                    n1 = min(n0 + 512, ncol)
                    p = pb[ev % 6]
                    for kk in range(8):
                        k.op(pe, [h_, w_], [p], lambda e: e.matmul(p[:, 0:n1 - n0], lhsT=h_[:, kk, :], rhs=w_[:, kk, n0:n1], start=(kk == 0), stop=(kk == 7)))
                    if ev % 2 == 0:
                        k.op(act, [p], [o_], lambda e: e.copy(out=o_[:, n0:n1], in_=p[:, 0:n1 - n0]))
                    else:
                        k.op(dve, [p], [o_], lambda e: e.tensor_copy(out=o_[:, n0:n1], in_=p[:, 0:n1 - n0]))
                    if c0 + n0 <= C_LR < c0 + n1:
                        off = C_LR - c0 - n0
                        k.op(dve, [p], [lrt, p], lambda e: e.tensor_copy(out=lrt[:], in_=p[:, off:off + 32]))
                        k.dma(sp, lrp[t * 128:(t + 1) * 128, :], lrt[:], [lrt], [lrp])
                    ev += 1
                k.dma(pool, proj[t * 128:(t + 1) * 128, c0:c1], o_[:, 0:ncol], [o_], [proj])
        k.pop()


    def rope_apply(dst, src, rp, nh, blk, tmp):
        G = 128 // (2 * blk)
        sv = src[:].rearrange("p (h g t d) -> p (h g) t d", h=nh, g=G, t=2)
        tv = tmp[:].rearrange("p (h g t d) -> p (h g) t d", h=nh, g=G, t=2)
        cosb = rp[:, 0, :].rearrange("p (o d) -> p o d", o=1).to_broadcast([128, nh, 128])
        sinv = rp[:, 1, :].rearrange("p (o g t d) -> p (o g) t d", o=1, g=G, t=2)
        k.op(dve, [src, rp], [dst], lambda e: e.tensor_mul(out=dst[:].rearrange("p (h d) -> p h d", h=nh), in0=src[:].rearrange("p (h d) -> p h d", h=nh), in1=cosb))
        if G == 1:
            for tt in range(2):
                k.op(dve, [src, rp], [tmp], lambda e: e.tensor_mul(out=tv[:, :, tt, :], in0=sv[:, :, 1 - tt, :], in1=sinv[:, :, tt, :].to_broadcast([128, nh, blk])))
        else:
            s4 = src[:].rearrange("p (h g t d) -> p h g t d", h=nh, g=G, t=2)
            t4 = tmp[:].rearrange("p (h g t d) -> p h g t d", h=nh, g=G, t=2)
            r4 = rp[:, 1, :].rearrange("p (g t d) -> p g t d", g=G, t=2)
            for g in range(G):
                for tt in range(2):
                    k.op(dve, [src, rp], [tmp], lambda e: e.tensor_mul(out=t4[:, :, g, tt, :], in0=s4[:, :, g, 1 - tt, :], in1=r4[:, g, tt, :].rearrange("p (o d) -> p o d", o=1).to_broadcast([128, nh, blk])))
        k.op(dve, [dst, tmp], [dst], lambda e: e.tensor_add(out=dst[:], in0=dst[:], in1=tmp[:]))

    def stage_recur(l, kind):
        k.push()
        is_gla = kind == 0
        cq, cv, cg = (C_GQ, C_GV, C_GR) if is_gla else (C_RQ, C_RV, C_RG)
        ng = gla_norm_g if is_gla else ret_norm_g
        scale = 128.0 ** -0.5
        masks = k.sb([128, 2, 512], F32)
        tri = k.sb([128, 4, 128], F32)
        n16 = k.sb([128, 1], F32)
        gainB = k.sb([128, D], F32)
        k.dma(sp, masks[:], c_masks[0:2].rearrange("m j c -> j m c"), [c_masks], [masks])
        k.dma(sp, tri[:], c_tri[:].rearrange("m j c -> j m c"), [c_tri], [tri])
        k.op(dve, [], [n16], lambda e: e.memset(n16[:], -1.0 / 16.0))
        bcast_load(gainB, ng[l:l + 1, :], ng)
        sp_t = k.sb([128, 512], F32)
        if is_gla:
            WA = k.sb([33, 1024], F32)
            lrT = k.sb([33, 128], F32)
            k.dma(sp, WA[:], wa_aug[l], [wa_aug], [WA])
            k.op(dve, [], [lrT], lambda e: e.memset(lrT[:], 1.0))
        else:
            k.dma(sp, sp_t[:], c_spret[:], [c_spret], [sp_t])
        S = k.sb([128, D], F32)
        Sbf = k.sb([128, D], BF16)
        qk = [k.sb([128, 1024], BF16) for _ in range(2)]
        vt = [k.sb([128, 1024], BF16) for _ in range(2)]
        lrt = [k.sb([128, 32], F32) for _ in range(2)]
        gt = [k.sb([128, 1024], BF16) for _ in range(2)]
        oft = [k.sb([128, 1024], F32) for _ in range(2)]
        rpt = [k.sb([128, 2, 128], F32) for _ in range(2)]
        eq = k.sb([128, 512], F32)
        ekin = k.sb([128, 512], F32)
        ekout = k.sb([128, 512], F32)
        dec = k.sb([128, 4], F32)
        qkr = k.sb([128, 1024], F32)
        t2 = k.sb([128, 1024], F32)
        qkin = k.sb([128, 1024], BF16)
        kout = k.sb([128, 512], BF16)
        qkT = k.sb([128, 8, 128], BF16)
        PT = k.sb([128, 512], BF16)
        osb = k.sb([128, 1024], F32)
        junk = k.sb([128, 1024], F32)
        yn = k.sb([128, 1024], F32)
        sg = k.sb([128, 1024], F32)
        yb = k.sb([128, 1024], BF16)
        yTt = k.sb([128, 8, 128], BF16)
        st = k.sb([128, 32], F32)
        bA = k.ps([128, 512])
        bB = k.ps([128, 512])
        bC = k.ps([128, 512])
        ptr = k.ps([128, 1024], BF16)
        ob = k.ps([128, 1024])
        dsb = k.ps([128, 1024])

        it = 0
        for dirn in range(2):
            order = list(range(NT)) if dirn == 0 else (list(range(NC_ - 1, -1, -1)) + list(range(NT - 1, NC_ - 1, -1)))
            k.op(dve, [], [S], lambda e: e.memset(S[:], 0.0))
            k.op(dve, [], [Sbf], lambda e: e.memset(Sbf[:], 0.0))
            decay_done = False
            for t in order:
                pr = it % 2
                it += 1
                rows = slice(t * 128, (t + 1) * 128)
                qk_, v_, lr_, g_, of_, rp_ = qk[pr], vt[pr], lrt[pr], gt[pr], oft[pr], rpt[pr]
                k.dma(sp, qk_[:], proj[rows, cq:cq + 1024], [proj], [qk_])
                k.dma(sp, v_[:], proj[rows, cv:cv + 1024], [proj], [v_])
                if is_gla:
                    k.dma(sp, lr_[:], lrp[rows, :], [lrp], [lr_])
                else:
                    k.dma(sp, rp_[:], c_rope_r[:, rows, :].rearrange("c t d -> t c d"), [c_rope_r], [rp_])
                if dirn == 1:
                    k.dma(sp, g_[:], proj[rows, cg:cg + 1024], [proj], [g_])
                    k.dma(sp, of_[:], o_part[rows, :], [o_part], [of_])
                if is_gla:
                    k.op(pe, [lr_, ident], [bC], lambda e: e.transpose(bC[0:32, 0:128], lr_[:], ident[:]))
                    k.op(act, [bC], [lrT], lambda e: e.copy(out=lrT[0:32, :], in_=bC[0:32, 0:128]))
                    k.op(pe, [lrT, WA], [bA], lambda e: e.matmul(bA[:], lhsT=lrT[:], rhs=WA[:, dirn * 512:(dirn + 1) * 512], start=True, stop=True))
                    k.op(act, [bA], [sp_t], lambda e: e.activation(out=sp_t[:], in_=bA[:], func=AF.Exp, scale=-1.0))
                    k.op(act, [sp_t], [sp_t], lambda e: e.activation(out=sp_t[:], in_=sp_t[:], func=AF.Ln, bias=1.0))
                if is_gla or not decay_done:
                    decay_done = True
                    k.op(pe, [tri, sp_t], [bB], lambda e: e.matmul(bB[:], lhsT=tri[:, 2 * dirn, :], rhs=sp_t[:], start=True, stop=True))
                    k.op(pe, [tri, sp_t], [bA], lambda e: e.matmul(bA[:], lhsT=tri[:, 2 * dirn + 1, :], rhs=sp_t[:], start=True, stop=True))
                    for h in range(4):
                        k.op(pe, [sp_t, n16], [bC], lambda e: e.matmul(bC[:, 128 + h:129 + h], lhsT=sp_t[:, h * 128:(h + 1) * 128], rhs=n16[:], start=True, stop=True))
                    k.op(act, [bB], [eq], lambda e: e.activation(out=eq[:], in_=bB[:], func=AF.Exp))
                    k.op(act, [bB], [ekin], lambda e: e.activation(out=ekin[:], in_=bB[:], func=AF.Exp, scale=-1.0))
                    k.op(act, [bA], [ekout], lambda e: e.activation(out=ekout[:], in_=bA[:], func=AF.Exp))
                    k.op(act, [bC], [dec], lambda e: e.activation(out=dec[:], in_=bC[:, 128:132], func=AF.Exp))
                if is_gla:
                    src = qk_
                else:
                    rope_apply(qkr, qk_, rp_, 8, 64, t2)
                    src = qkr
                k.op(dve, [src, eq], [qkin], lambda e: e.scalar_tensor_tensor(out=qkin[:, 0:512], in0=src[:, 0:512], scalar=scale, in1=eq[:], op0=ALU.mult, op1=ALU.mult))
                k.op(dve, [src, ekin], [qkin], lambda e: e.tensor_mul(out=qkin[:, 512:1024], in0=src[:, 512:1024], in1=ekin[:]))
                k.op(dve, [src, ekout], [kout], lambda e: e.tensor_mul(out=kout[:], in0=src[:, 512:1024], in1=ekout[:]))
                transpose8(qkin, qkT, ptr, identb, act)
                for h in range(4):
                    k.op(pe, [qkT], [bC], lambda e: e.matmul(bC[:, h * 128:(h + 1) * 128], lhsT=qkT[:, 4 + h, :], rhs=qkT[:, h, :], start=True, stop=True))
                k.op(dve, [bC, masks], [PT], lambda e: e.tensor_mul(out=PT[:], in0=bC[:], in1=masks[:, dirn, :]))
                for h in range(4):
                    hs = slice(h * 256, (h + 1) * 256)
                    k.op(pe, [PT, v_], [ob], lambda e: e.matmul(ob[:, hs], lhsT=PT[:, h * 128:(h + 1) * 128], rhs=v_[:, hs], start=True, stop=False))
                    k.op(pe, [qkT, Sbf], [ob], lambda e: e.matmul(ob[:, hs], lhsT=qkT[:, h, :], rhs=Sbf[:, hs], start=False, stop=True))
                for h in range(4):
                    hs = slice(h * 256, (h + 1) * 256)
                    k.op(pe, [kout, v_], [dsb], lambda e: e.matmul(dsb[:, hs], lhsT=kout[:, h * 128:(h + 1) * 128], rhs=v_[:, hs], start=True, stop=True))
                for h in range(4):
                    hs = slice(h * 256, (h + 1) * 256)
                    k.op(dve, [S, dec, dsb], [S], lambda e: e.scalar_tensor_tensor(out=S[:, hs], in0=S[:, hs], scalar=dec[:, h:h + 1], in1=dsb[:, hs], op0=ALU.mult, op1=ALU.add))
                k.op(act, [S], [Sbf], lambda e: e.copy(out=Sbf[:], in_=S[:]))
                if dirn == 0:
                    k.op(act, [ob], [osb], lambda e: e.copy(out=osb[:], in_=ob[:]))
                    k.dma(pool, o_part[rows, :], osb[:], [osb], [o_part])
                else:
                    k.op(dve, [ob, of_], [osb], lambda e: e.tensor_add(out=osb[:], in0=ob[:], in1=of_[:]))
                    k.op(dve, [osb], [st], lambda e: e.reduce_sum(out=st[:, 0:4], in_=osb[:].rearrange("p (h d) -> p h d", h=4), axis=AX.X))
                    k.op(act, [osb], [junk], lambda e: e.activation(out=junk[:], in_=osb[:], func=AF.Square))
                    k.op(dve, [junk], [st], lambda e: e.reduce_sum(out=st[:, 4:8], in_=junk[:].rearrange("p (h d) -> p h d", h=4), axis=AX.X))
                    k.op(dve, [st], [st], lambda e: e.tensor_scalar(out=st[:, 8:16], in0=st[:, 0:8], scalar1=1.0 / 256.0, scalar2=None, op0=ALU.mult))
                    k.op(dve, [st], [st], lambda e: e.tensor_mul(out=st[:, 16:20], in0=st[:, 8:12], in1=st[:, 8:12]))
                    k.op(dve, [st], [st], lambda e: e.tensor_sub(out=st[:, 20:24], in0=st[:, 12:16], in1=st[:, 16:20]))
                    k.op(dve, [st], [st], lambda e: e.tensor_scalar(out=st[:, 20:24], in0=st[:, 20:24], scalar1=EPS, scalar2=None, op0=ALU.add))
                    k.op(act, [st], [st], lambda e: e.activation(out=st[:, 24:28], in_=st[:, 20:24], func=AF.Sqrt))
                    k.op(dve, [st], [st], lambda e: e.reciprocal(out=st[:, 28:32], in_=st[:, 24:28]))
                    for h in range(4):
                        hs = slice(h * 256, (h + 1) * 256)
                        k.op(dve, [osb, st], [yn], lambda e: e.tensor_scalar(out=yn[:, hs], in0=osb[:, hs], scalar1=st[:, 8 + h:9 + h], scalar2=st[:, 28 + h:29 + h], op0=ALU.subtract, op1=ALU.mult))
                    k.op(act, [g_], [sg], lambda e: e.activation(out=sg[:], in_=g_[:], func=AF.Silu))
                    k.op(dve, [yn, gainB], [yn], lambda e: e.tensor_mul(out=yn[:], in0=yn[:], in1=gainB[:]))
                    k.op(dve, [yn, sg], [yb], lambda e: e.tensor_mul(out=yb[:], in0=yn[:], in1=sg[:]))
                    transpose8(yb, yTt, ptr, identb, act)
                    k.dma(pool, yT[kind][t], yTt[:], [yTt], [yT[kind]])
        k.pop()


    def stage_attn(l, with_ctx):
        k.push()
        scale = 128.0 ** -0.5
        kT_all = k.sb([128, 2, T], BF16)
        v_all = k.sb([128, NT, 256], BF16)
        masks = k.sb([128, 3, 512], F32)
        k.dma(sp, masks[:], c_masks[:].rearrange("m j c -> j m c"), [c_masks], [masks])
        sE = k.sb([128, 8], F32)
        onesf = k.sb([128, 128], F32)
        sinkE = k.sb([128, 8, 128], F32)
        bcast_load(sE, attn_sink[l:l + 1, :], attn_sink)
        k.op(act, [sE], [sE], lambda e: e.activation(out=sE[:], in_=sE[:], func=AF.Exp))
        k.op(dve, [], [onesf], lambda e: e.memset(onesf[:], 1.0))
        for h in range(8):
            k.op(dve, [onesf, sE], [sinkE], lambda e: e.tensor_scalar(out=sinkE[:, h, :], in0=onesf[:], scalar1=sE[:, h:h + 1], scalar2=None, op0=ALU.mult))
        for t0 in range(0, NT, 16):
            t1 = min(NT, t0 + 16)
            k.dma(sp, v_all[:, t0:t1, :], proj[t0 * 128:t1 * 128, C_AV:C_AV + 256].rearrange("(t p) c -> p t c", p=128), [proj], [v_all])
        kt = [k.sb([128, 256], BF16) for _ in range(2)]
        rpt = [k.sb([128, 2, 128], F32) for _ in range(2)]
        kr = k.sb([128, 256], F32)
        tmpk = k.sb([128, 256], F32)
        kb16 = k.sb([128, 256], BF16)
        ptr = k.ps([128, 1024], BF16)
        for t in range(NT):
            k_ = kt[t % 2]
            rp_ = rpt[t % 2]
            rows = slice(t * 128, (t + 1) * 128)
            k.dma(sp, k_[:], proj[rows, C_AK:C_AK + 256], [proj], [k_])
            if t >= NC_:
                n = t - NC_
                k.dma(sp, rp_[:], c_rope_a[:, n * 128:(n + 1) * 128, :].rearrange("c t d -> t c d"), [c_rope_a], [rp_])
                rope_apply(kr, k_, rp_, 2, 32, tmpk)
                k.op(act, [kr], [kb16], lambda e: e.copy(out=kb16[:], in_=kr[:]))
                src = kb16
            else:
                src = k_
            for h in range(2):
                k.op(pe, [src, identb], [ptr], lambda e: e.transpose(ptr[:, h * 128:(h + 1) * 128], src[:, h * 128:(h + 1) * 128], identb[:]))
            k.op(act, [ptr], [kT_all], lambda e: e.copy(out=kT_all[:, :, rows], in_=ptr[:, 0:256].rearrange("p (h t) -> p h t", h=2)))
        qt = [k.sb([128, 1024], BF16) for _ in range(2)]
        qr = k.sb([128, 1024], F32)
        tmpq = k.sb([128, 1024], F32)
        qs = k.sb([128, 1024], BF16)
        qT = k.sb([128, 8, 128], BF16)
        Pt = [k.sb([128, 512], BF16) for _ in range(3)]
        rden = k.sb([128, 512], F32)
        yTt = [k.sb([128, 8, 128], BF16) for _ in range(2)]
        scb = [k.ps([128, 512]) for _ in range(3)]
        outb = [k.ps([128, 512]) for _ in range(2)]
        denb = [k.ps([128, 512]) for _ in range(2)]
        qtiles = list(range(0 if with_ctx else NC_, NT))
        for qi, t in enumerate(qtiles):
            q_ = qt[qi % 2]
            rp_ = rpt[qi % 2]
            y_ = yTt[qi % 2]
            rows = slice(t * 128, (t + 1) * 128)
            k.dma(sp, q_[:], proj[rows, C_AQ:C_AQ + 1024], [proj], [q_])
            if t >= NC_:
                n = t - NC_
                k.dma(sp, rp_[:], c_rope_a[:, n * 128:(n + 1) * 128, :].rearrange("c t d -> t c d"), [c_rope_a], [rp_])
                rope_apply(qr, q_, rp_, 8, 32, tmpq)
                k.op(dve, [qr], [qs], lambda e: e.tensor_scalar(out=qs[:], in0=qr[:], scalar1=scale, scalar2=None, op0=ALU.mult))
            else:
                k.op(dve, [q_], [qs], lambda e: e.tensor_scalar(out=qs[:], in0=q_[:], scalar1=scale, scalar2=None, op0=ALU.mult))
            transpose8(qs, qT, ptr, identb, act)
            kbs = [(c, None) for c in range(NC_)]
            if t >= NC_:
                n = t - NC_
                if n >= 1:
                    kbs.append((t - 1, 2))
                kbs.append((t, None))
                if t + 1 < NT:
                    kbs.append((t + 1, 0))
            items = [(g, kb, mi, j == 0, j == len(kbs) - 1) for g in range(2) for j, (kb, mi) in enumerate(kbs)]

            def emit_sc(ii):
                g, kb, mi, first, last = items[ii]
                sc = scb[ii % 3]
                k.op(pe, [kT_all, qT], [sc], lambda e: e.matmul(sc[:], lhsT=kT_all[:, g, kb * 128:(kb + 1) * 128], rhs=qT[:, 4 * g:4 * g + 4, :].rearrange("p h t -> p (h t)"), start=True, stop=True))

            def emit_pv(ii):
                g, kb, mi, first, last = items[ii]
                sc = scb[ii % 3]
                P = Pt[ii % 3]
                k.op(act, [sc], [P], lambda e: e.activation(out=P[:], in_=sc[:], func=AF.Exp))
                if mi is not None:
                    k.op(dve, [P, masks], [P], lambda e: e.tensor_mul(out=P[:], in0=P[:], in1=masks[:, mi, :]))
                k.op(pe, [v_all, P], [outb[g]], lambda e: e.matmul(outb[g][:], lhsT=v_all[:, kb, g * 128:(g + 1) * 128], rhs=P[:], start=first, stop=last))
                k.op(pe, [onesb, P], [denb[g]], lambda e: e.matmul(denb[g][:], lhsT=onesb[:], rhs=P[:], start=first, stop=last))
                if last:
                    k.op(dve, [denb[g], sinkE], [rden], lambda e: e.tensor_add(out=rden[:], in0=denb[g][:], in1=sinkE[:, 4 * g:4 * g + 4, :].rearrange("p h t -> p (h t)")))
                    k.op(dve, [rden], [rden], lambda e: e.reciprocal(out=rden[:], in_=rden[:]))
                    k.op(dve, [outb[g], rden], [y_], lambda e: e.tensor_mul(out=y_[:, 4 * g:4 * g + 4, :].rearrange("p h t -> p (h t)"), in0=outb[g][:], in1=rden[:]))

            emit_sc(0)
            for ii in range(len(items)):
                if ii + 1 < len(items):
                    emit_sc(ii + 1)
                emit_pv(ii)
            k.dma(pool, yT[1][t], y_[:], [y_], [yT[1]])
        k.pop()


    def load_w_bf16(dst, src2d):
        for kk in range(8):
            k.dma(pool, dst[:, kk, :], src2d[kk * 128:(kk + 1) * 128, :], [w_out], [dst])

    def stage_merge(l, xcur, with_ctx):
        k.push()
        wbr = [k.sb([128, 8, 1024], BF16) for _ in range(3)]
        wo = k.sb([128, 8, 1024], BF16)
        for b in range(3):
            load_w_bf16(wbr[b], w_br[b][l])
        load_w_bf16(wo, w_out[l])
        g1B = [k.sb([128, D], F32) for _ in range(2)]
        for r in range(2):
            bcast_load(g1B[r], modv[r:r + 1, 2 * D:3 * D], modv)
        yTb = [[k.sb([128, 8, 128], BF16) for _ in range(3)] for _ in range(2)]
        gat = [k.sb([128, 3072], BF16) for _ in range(2)]
        xt = [k.sb([128, D], F32) for _ in range(2)]
        sig = k.sb([128, 3072], F32)
        merged = k.sb([128, D], F32)
        tmp = k.sb([128, D], F32)
        mb = k.sb([128, D], BF16)
        mT = k.sb([128, 8, 128], BF16)
        xo = [k.sb([128, D], F32) for _ in range(2)]
        brb = [k.ps([128, 1024]) for _ in range(2)]
        ob = k.ps([128, 1024])
        ptr = k.ps([128, 1024], BF16)
        tiles = list(range(0 if with_ctx else NC_, NT))
        bi = 0
        for i, t in enumerate(tiles):
            pr = i % 2
            rows = slice(t * 128, (t + 1) * 128)
            for b in range(3):
                k.dma(sp, yTb[pr][b][:], yT[b][t], [yT[b]], [yTb[pr][b]])
            k.dma(sp, gat[pr][:], proj[rows, C_GATES:C_GATES + 3072], [proj], [gat[pr]])
            k.dma(sp, xt[pr][:], xcur[rows, :], [xcur], [xt[pr]])
            k.op(act, [gat[pr]], [sig], lambda e: e.activation(out=sig[:], in_=gat[pr][:], func=AF.Sigmoid))
            for b in range(3):
                pb_ = brb[bi % 2]
                bi += 1
                y_ = yTb[pr][b]
                for half in range(2):
                    for kk in range(8):
                        k.op(pe, [y_, wbr[b]], [pb_], lambda e: e.matmul(pb_[:, half * 512:(half + 1) * 512], lhsT=y_[:, kk, :], rhs=wbr[b][:, kk, half * 512:(half + 1) * 512], start=(kk == 0), stop=(kk == 7)))
                if b == 0:
                    k.op(dve, [pb_, sig], [merged], lambda e: e.tensor_mul(out=merged[:], in0=pb_[:], in1=sig[:, 0:1024]))
                else:
                    k.op(dve, [pb_, sig], [tmp], lambda e: e.tensor_mul(out=tmp[:], in0=pb_[:], in1=sig[:, b * 1024:(b + 1) * 1024]))
                    k.op(dve, [merged, tmp], [merged], lambda e: e.tensor_add(out=merged[:], in0=merged[:], in1=tmp[:]))
            k.op(act, [merged], [mb], lambda e: e.copy(out=mb[:], in_=merged[:]))
            transpose8(mb, mT, ptr, identb, act)
            for half in range(2):
                for kk in range(8):
                    k.op(pe, [mT, wo], [ob], lambda e: e.matmul(ob[:, half * 512:(half + 1) * 512], lhsT=mT[:, kk, :], rhs=wo[:, kk, half * 512:(half + 1) * 512], start=(kk == 0), stop=(kk == 7)))
            gB = g1B[1] if t < NC_ else g1B[0]
            k.op(dve, [ob, gB], [tmp], lambda e: e.tensor_mul(out=tmp[:], in0=ob[:], in1=gB[:]))
            k.op(dve, [tmp, xt[pr]], [xo[pr]], lambda e: e.tensor_add(out=xo[pr][:], in0=tmp[:], in1=xt[pr][:]))
            k.dma(pool, xm[rows, :], xo[pr][:], [xo[pr]], [xm])
        k.pop()


    def stage_moe(l, xnext, last):
        k.push()
        GS = 12
        A_l, sh_l, A_c, sh_c = mod_tiles(l, 2, norm2_g)
        g2B = [k.sb([128, D], F32) for _ in range(2)]
        for r in range(2):
            bcast_load(g2B[r], modv[r:r + 1, 5 * D:6 * D], modv)
        if last:
            fgB = k.sb([128, D], F32)
            bcast_load(fgB, final_g[0:1, :], final_g)
        wr = k.sb([128, 8, 36], F32)
        brB = k.sb([128, 36], F32)
        k.dma(sp, wr[:], w_r[l].rearrange("(k p) n -> p k n", p=128), [w_r], [wr])
        bcast_load(brB, b_r[l:l + 1, :], b_r)
        acc = k.sb([128, GS, D], F32)
        hTb = k.sb([128, 8, GS * 128], BF16)
        Gd = k.sb([128, GS, 32], F32)
        W = [k.sb([128, 8, 1024], BF16) for _ in range(3)]
        uT = k.sb([128, 8, 512], BF16)
        sgl = [k.sb([128, 512], F32) for _ in range(2)]
        xt = [k.sb([128, D], F32) for _ in range(2)]
        junk = k.sb([128, D], F32)
        h2 = k.sb([128, D], F32)
        h2T = k.sb([128, 8, 128], F32)
        st = k.sb([128, 4], F32)
        rt = k.sb([128, 64], F32)
        lgs = k.sb([128, 36], F32)
        em = k.sb([128, 32], F32)
        em2 = k.sb([128, 32], F32)
        oh1 = k.sb([128, 32], F32)
        oh2 = k.sb([128, 32], F32)
        ptrf = k.ps([128, 1024])
        gvb = [k.ps([128, 512]) for _ in range(4)]
        yb = k.ps([128, 1024])
        tiles = list(range(NC_ if last else 0, NT))
        groups = [tiles[i:i + GS] for i in range(0, len(tiles), GS)]
        for grp in groups:
            for sl, t in enumerate(grp):
                x_ = xt[sl % 2]
                rows = slice(t * 128, (t + 1) * 128)
                k.dma(sp, x_[:], xm[rows, :], [xm], [x_])
                A, sh = (A_c, sh_c) if t < NC_ else (A_l, sh_l)
                rms_mod(x_, A, sh, junk, st, h2)
                transpose8(h2, h2T, ptrf, ident, act)
                k.op(dve, [h2T], [hTb], lambda e: e.tensor_copy(out=hTb[:, :, sl * 128:(sl + 1) * 128], in_=h2T[:]))
                for kk in range(8):
                    k.op(pe, [h2T, wr], [ptrf], lambda e: e.matmul(ptrf[:, 0:36], lhsT=h2T[:, kk, :], rhs=wr[:, kk, :], start=(kk == 0), stop=(kk == 7)))
                k.op(dve, [ptrf, brB], [lgs], lambda e: e.tensor_add(out=lgs[:], in0=ptrf[:, 0:36], in1=brB[:]))
                k.op(dve, [lgs], [rt], lambda e: e.reduce_max(out=rt[:, 0:1], in_=lgs[:, 0:4], axis=AX.X))
                k.op(dve, [lgs, rt], [rt], lambda e: e.tensor_scalar(out=rt[:, 4:8], in0=lgs[:, 0:4], scalar1=rt[:, 0:1], scalar2=None, op0=ALU.is_equal))
                k.op(dve, [rt], [rt], lambda e: e.tensor_scalar(out=rt[:, 1:2], in0=rt[:, 0:1], scalar1=-1.0, scalar2=None, op0=ALU.mult))
                k.op(act, [lgs, rt], [rt], lambda e: e.activation(out=rt[:, 8:12], in_=lgs[:, 0:4], func=AF.Exp, bias=rt[:, 1:2], scale=1.0, accum_out=rt[:, 2:3]))
                k.op(dve, [rt], [rt], lambda e: e.reciprocal(out=rt[:, 3:4], in_=rt[:, 2:3]))
                k.op(dve, [rt], [rt], lambda e: e.tensor_scalar(out=rt[:, 12:16], in0=rt[:, 4:8], scalar1=1.0, scalar2=BIG, op0=ALU.subtract, op1=ALU.mult))
                k.op(dve, [lgs, rt], [em], lambda e: e.tensor_tensor(out=em[:].rearrange("p (g e) -> p g e", g=4), in0=lgs[:, 4:36].rearrange("p (g e) -> p g e", g=4), in1=rt[:, 12:16].rearrange("p (g o) -> p g o", o=1).to_broadcast([128, 4, 8]), op=ALU.add))
                k.op(dve, [em], [rt], lambda e: e.reduce_max(out=rt[:, 16:17], in_=em[:], axis=AX.X))
                k.op(dve, [em, rt], [oh1], lambda e: e.tensor_scalar(out=oh1[:], in0=em[:], scalar1=rt[:, 16:17], scalar2=None, op0=ALU.is_equal))
                k.op(dve, [oh1, em], [em2], lambda e: e.scalar_tensor_tensor(out=em2[:], in0=oh1[:], scalar=-BIG, in1=em[:], op0=ALU.mult, op1=ALU.add))
                k.op(dve, [em2], [rt], lambda e: e.reduce_max(out=rt[:, 17:18], in_=em2[:], axis=AX.X))
                k.op(dve, [em2, rt], [oh2], lambda e: e.tensor_scalar(out=oh2[:], in0=em2[:], scalar1=rt[:, 17:18], scalar2=None, op0=ALU.is_equal))
                k.op(dve, [rt], [rt], lambda e: e.tensor_sub(out=rt[:, 18:19], in0=rt[:, 17:18], in1=rt[:, 16:17]))
                k.op(act, [rt], [rt], lambda e: e.activation(out=rt[:, 19:20], in_=rt[:, 18:19], func=AF.Exp))
                k.op(dve, [rt], [rt], lambda e: e.tensor_scalar(out=rt[:, 20:21], in0=rt[:, 19:20], scalar1=1.0, scalar2=None, op0=ALU.add))
                k.op(dve, [rt], [rt], lambda e: e.reciprocal(out=rt[:, 21:22], in_=rt[:, 20:21]))
                k.op(dve, [rt], [rt], lambda e: e.tensor_mul(out=rt[:, 22:23], in0=rt[:, 19:20], in1=rt[:, 21:22]))
                k.op(dve, [rt], [rt], lambda e: e.tensor_mul(out=rt[:, 23:24], in0=rt[:, 21:22], in1=rt[:, 3:4]))
                k.op(dve, [rt], [rt], lambda e: e.tensor_mul(out=rt[:, 24:25], in0=rt[:, 22:23], in1=rt[:, 3:4]))
                k.op(dve, [oh1, rt], [Gd], lambda e: e.tensor_scalar(out=Gd[:, sl, :], in0=oh1[:], scalar1=rt[:, 23:24], scalar2=None, op0=ALU.mult))
                k.op(dve, [oh2, rt, Gd], [Gd], lambda e: e.scalar_tensor_tensor(out=Gd[:, sl, :], in0=oh2[:], scalar=rt[:, 24:25], in1=Gd[:, sl, :], op0=ALU.mult, op1=ALU.add))
            ng_ = len(grp)
            blocks = [(b0, min(b0 + 4, ng_)) for b0 in range(0, ng_, 4)]
            gi = 0
            for ex in range(NEXP):
                load_w_bf16(W[0], moe_w1[(l * 32 + ex) * D:(l * 32 + ex + 1) * D, :])
                load_w_bf16(W[1], moe_w3[(l * 32 + ex) * D:(l * 32 + ex + 1) * D, :])
                load_w_bf16(W[2], moe_w2[(l * 32 + ex) * D:(l * 32 + ex + 1) * D, :])
                for (b0, b1) in blocks:
                    N = (b1 - b0) * 128
                    cols = slice(b0 * 128, b1 * 128)
                    for hc in range(8):
                        gb = gvb[gi % 4]
                        vb = gvb[(gi + 1) % 4]
                        sg_ = sgl[(gi // 2) % 2]
                        gi += 2
                        for kk in range(8):
                            k.op(pe, [W[0], hTb], [gb], lambda e: e.matmul(gb[:, 0:N], lhsT=W[0][:, kk, hc * 128:(hc + 1) * 128], rhs=hTb[:, kk, cols], start=(kk == 0), stop=(kk == 7)))
                        for kk in range(8):
                            k.op(pe, [W[1], hTb], [vb], lambda e: e.matmul(vb[:, 0:N], lhsT=W[1][:, kk, hc * 128:(hc + 1) * 128], rhs=hTb[:, kk, cols], start=(kk == 0), stop=(kk == 7)))
                        k.op(act, [gb], [sg_], lambda e: e.activation(out=sg_[:, 0:N], in_=gb[:, 0:N], func=AF.Silu))
                        k.op(dve, [sg_, vb], [uT], lambda e: e.tensor_mul(out=uT[:, hc, 0:N], in0=sg_[:, 0:N], in1=vb[:, 0:N]))
                    for sl in range(b0, b1):
                        tc_ = slice((sl - b0) * 128, (sl - b0 + 1) * 128)
                        for half in range(2):
                            for hc in range(8):
                                k.op(pe, [uT, W[2]], [yb], lambda e: e.matmul(yb[:, half * 512:(half + 1) * 512], lhsT=uT[:, hc, tc_], rhs=W[2][:, hc, half * 512:(half + 1) * 512], start=(hc == 0), stop=(hc == 7)))
                        if ex == 0:
                            k.op(dve, [yb, Gd], [acc], lambda e: e.tensor_scalar(out=acc[:, sl, :], in0=yb[:], scalar1=Gd[:, sl, ex:ex + 1], scalar2=None, op0=ALU.mult))
                        else:
                            k.op(dve, [yb, Gd, acc], [acc], lambda e: e.scalar_tensor_tensor(out=acc[:, sl, :], in0=yb[:], scalar=Gd[:, sl, ex:ex + 1], in1=acc[:, sl, :], op0=ALU.mult, op1=ALU.add))
            for sl, t in enumerate(grp):
                x_ = xt[sl % 2]
                rows = slice(t * 128, (t + 1) * 128)
                k.dma(sp, x_[:], xm[rows, :], [xm], [x_])
                gB = g2B[1] if t < NC_ else g2B[0]
                k.op(dve, [acc, gB], [junk], lambda e: e.tensor_mul(out=junk[:], in0=acc[:, sl, :], in1=gB[:]))
                k.op(dve, [junk, x_], [h2], lambda e: e.tensor_add(out=h2[:], in0=junk[:], in1=x_[:]))
                if not last:
                    k.dma(pool, xnext[rows, :], h2[:], [h2], [xnext])
                else:
                    k.op(act, [h2], [junk, st], lambda e: e.activation(out=junk[:], in_=h2[:], func=AF.Square, accum_out=st[:, 0:1]))
                    k.op(dve, [st], [st], lambda e: e.tensor_scalar(out=st[:, 1:2], in0=st[:, 0:1], scalar1=1.0 / D, scalar2=EPS, op0=ALU.mult, op1=ALU.add))
                    k.op(act, [st], [st], lambda e: e.activation(out=st[:, 2:3], in_=st[:, 1:2], func=AF.Sqrt))
                    k.op(dve, [st], [st], lambda e: e.reciprocal(out=st[:, 3:4], in_=st[:, 2:3]))
                    k.op(dve, [h2, st, fgB], [junk], lambda e: e.scalar_tensor_tensor(out=junk[:], in0=h2[:], scalar=st[:, 3:4], in1=fgB[:], op0=ALU.mult, op1=ALU.mult))
                    k.dma(pool, out[(t - NC_) * 128:(t - NC_ + 1) * 128, :], junk[:], [junk], [out])
        k.pop()


    def stage_moe2(l, xnext, last):
        tiles = list(range(NC_ if last else 0, NT))
        ntl = len(tiles)
        NBLK = (2 * ntl * 128 + 32 * (BLK - 1) + BLK - 1) // BLK
        NSLOT = NBLK * BLK
        k.push()
        OH = [k.sb([128, ntl, 32], F32) for _ in range(2)]
        RANK = k.sb([128, ntl, 32], F32)
        GW = k.sb([128, ntl, 2], F32)
        DESTI = k.sb([128, ntl, 2], I32)
        PS = k.sb([128, 32], F32)
        BE = k.sb([128, 80], F32)
        wbase = k.sb([128, 8], F32)
        k.dma(sp, wbase[:], c_wbase[:], [c_wbase], [wbase])
        k.push()
        A_l, sh_l, A_c, sh_c = mod_tiles(l, 2, norm2_g)
        wr = k.sb([128, 8, 36], F32)
        brB = k.sb([128, 36], F32)
        k.dma(sp, wr[:], w_r[l].rearrange("(k p) n -> p k n", p=128), [w_r], [wr])
        bcast_load(brB, b_r[l:l + 1, :], b_r)
        LT = k.sb([128, 128], F32)
        onesf = k.sb([128, 128], F32)
        k.dma(sp, LT[:], c_tri[3], [c_tri], [LT])
        k.op(dve, [LT], [LT], lambda e: e.tensor_scalar(out=LT[:], in0=LT[:], scalar1=-16.0, scalar2=None, op0=ALU.mult))
        k.op(dve, [], [onesf], lambda e: e.memset(onesf[:], 1.0))
        Rsum = k.sb([128, 32], F32)
        Mt = k.sb([128, 32], F32)
        k.op(dve, [], [Rsum], lambda e: e.memset(Rsum[:], 0.0))
        xt = [k.sb([128, D], F32) for _ in range(2)]
        junk = k.sb([128, D], F32)
        h2 = k.sb([128, D], F32)
        h2b = [k.sb([128, D], BF16) for _ in range(2)]
        h2T = k.sb([128, 8, 128], F32)
        st = k.sb([128, 4], F32)
        rt = k.sb([128, 64], F32)
        lgs = k.sb([128, 36], F32)
        em = k.sb([128, 32], F32)
        em2 = k.sb([128, 32], F32)
        ptrf = k.ps([128, 1024])
        rkp = k.ps([128, 32])
        for sl, t in enumerate(tiles):
            x_ = xt[sl % 2]
            hb_ = h2b[sl % 2]
            rows = slice(t * 128, (t + 1) * 128)
            oh1 = OH[0][:, sl, :]
            oh2 = OH[1][:, sl, :]
            k.dma(sp, x_[:], xm[rows, :], [xm], [x_])
            A, sh = (A_c, sh_c) if t < NC_ else (A_l, sh_l)
            rms_mod(x_, A, sh, junk, st, h2)
            k.op(act, [h2], [hb_], lambda e: e.copy(out=hb_[:], in_=h2[:]))
            k.dma(sp, h2d[rows, :], hb_[:], [hb_], [h2d])
            transpose8(h2, h2T, ptrf, ident, act)
            for kk in range(8):
                k.op(pe, [h2T, wr], [ptrf], lambda e: e.matmul(ptrf[:, 0:36], lhsT=h2T[:, kk, :], rhs=wr[:, kk, :], start=(kk == 0), stop=(kk == 7)))
            k.op(dve, [ptrf, brB], [lgs], lambda e: e.tensor_add(out=lgs[:], in0=ptrf[:, 0:36], in1=brB[:]))
            k.op(dve, [lgs], [rt], lambda e: e.reduce_max(out=rt[:, 0:1], in_=lgs[:, 0:4], axis=AX.X))
            k.op(dve, [lgs, rt], [rt], lambda e: e.tensor_scalar(out=rt[:, 4:8], in0=lgs[:, 0:4], scalar1=rt[:, 0:1], scalar2=None, op0=ALU.is_equal))
            k.op(dve, [rt], [rt], lambda e: e.tensor_scalar(out=rt[:, 1:2], in0=rt[:, 0:1], scalar1=-1.0, scalar2=None, op0=ALU.mult))
            k.op(act, [lgs, rt], [rt], lambda e: e.activation(out=rt[:, 8:12], in_=lgs[:, 0:4], func=AF.Exp, bias=rt[:, 1:2], scale=1.0, accum_out=rt[:, 2:3]))
            k.op(dve, [rt], [rt], lambda e: e.reciprocal(out=rt[:, 3:4], in_=rt[:, 2:3]))
            k.op(dve, [rt], [rt], lambda e: e.tensor_scalar(out=rt[:, 12:16], in0=rt[:, 4:8], scalar1=1.0, scalar2=BIG, op0=ALU.subtract, op1=ALU.mult))
            k.op(dve, [lgs, rt], [em], lambda e: e.tensor_tensor(out=em[:].rearrange("p (g e) -> p g e", g=4), in0=lgs[:, 4:36].rearrange("p (g e) -> p g e", g=4), in1=rt[:, 12:16].rearrange("p (g o) -> p g o", o=1).to_broadcast([128, 4, 8]), op=ALU.add))
            k.op(dve, [em], [rt], lambda e: e.reduce_max(out=rt[:, 16:17], in_=em[:], axis=AX.X))
            k.op(dve, [em, rt], [OH[0]], lambda e: e.tensor_scalar(out=oh1, in0=em[:], scalar1=rt[:, 16:17], scalar2=None, op0=ALU.is_equal))
            k.op(dve, [OH[0], em], [em2], lambda e: e.scalar_tensor_tensor(out=em2[:], in0=oh1, scalar=-BIG, in1=em[:], op0=ALU.mult, op1=ALU.add))
            k.op(dve, [em2], [rt], lambda e: e.reduce_max(out=rt[:, 17:18], in_=em2[:], axis=AX.X))
            k.op(dve, [em2, rt], [OH[1]], lambda e: e.tensor_scalar(out=oh2, in0=em2[:], scalar1=rt[:, 17:18], scalar2=None, op0=ALU.is_equal))
            k.op(dve, [rt], [rt], lambda e: e.tensor_sub(out=rt[:, 18:19], in0=rt[:, 17:18], in1=rt[:, 16:17]))
            k.op(act, [rt], [rt], lambda e: e.activation(out=rt[:, 19:20], in_=rt[:, 18:19], func=AF.Exp))
            k.op(dve, [rt], [rt], lambda e: e.tensor_scalar(out=rt[:, 20:21], in0=rt[:, 19:20], scalar1=1.0, scalar2=None, op0=ALU.add))
            k.op(dve, [rt], [rt], lambda e: e.reciprocal(out=rt[:, 21:22], in_=rt[:, 20:21]))
            k.op(dve, [rt], [rt], lambda e: e.tensor_mul(out=rt[:, 22:23], in0=rt[:, 19:20], in1=rt[:, 21:22]))
            k.op(dve, [rt], [GW], lambda e: e.tensor_mul(out=GW[:, sl, 0:1], in0=rt[:, 21:22], in1=rt[:, 3:4]))
            k.op(dve, [rt], [GW], lambda e: e.tensor_mul(out=GW[:, sl, 1:2], in0=rt[:, 22:23], in1=rt[:, 3:4]))
            k.op(dve, [OH[0], OH[1]], [Mt], lambda e: e.tensor_add(out=Mt[:], in0=oh1, in1=oh2))
            k.op(pe, [LT, Mt], [rkp], lambda e: e.matmul(rkp[:], lhsT=LT[:], rhs=Mt[:], start=True, stop=False))
            k.op(pe, [onesf, Rsum], [rkp], lambda e: e.matmul(rkp[:], lhsT=onesf[:], rhs=Rsum[:], start=False, stop=True))
            k.op(act, [rkp], [RANK], lambda e: e.copy(out=RANK[:, sl, :], in_=rkp[:]))
            k.op(dve, [Rsum, Mt], [Rsum], lambda e: e.tensor_add(out=Rsum[:], in0=Rsum[:], in1=Mt[:]))
        cnt = k.sb([128, 32], F32)
        pad = k.sb([128, 32], F32)
        pend = k.sb([128, 32], F32)
        bst = k.sb([128, 80], F32)
        tmp32 = k.sb([128, 32], F32)
        dstf = k.sb([128, 2], F32)
        bcast_load(bst, c_bstart[0:1, :], c_bstart)
        k.op(pe, [onesf, Rsum], [rkp], lambda e: e.matmul(rkp[:], lhsT=onesf[:], rhs=Rsum[:], start=True, stop=True))
        k.op(dve, [rkp], [cnt], lambda e: e.tensor_copy(out=cnt[:], in_=rkp[:]))
        k.op(dve, [], [pad], lambda e: e.memset(pad[:], 0.0))
        for j in range((2 * ntl * 128) // BLK + 1):
            k.op(dve, [cnt, pad], [pad], lambda e: e.scalar_tensor_tensor(out=pad[:], in0=cnt[:], scalar=float(j * BLK), in1=pad[:], op0=ALU.is_gt, op1=ALU.add))
        k.op(dve, [pad], [pad], lambda e: e.tensor_scalar(out=pad[:], in0=pad[:], scalar1=float(BLK), scalar2=None, op0=ALU.mult))
        k.op(dve, [], [PS], lambda e: e.memset(PS[:], 0.0))
        for ex in range(1, 32):
            k.op(dve, [PS, pad], [PS], lambda e: e.tensor_add(out=PS[:, ex:ex + 1], in0=PS[:, ex - 1:ex], in1=pad[:, ex - 1:ex]))
        k.op(dve, [PS, pad], [pend], lambda e: e.tensor_add(out=pend[:], in0=PS[:], in1=pad[:]))
        k.op(dve, [], [BE], lambda e: e.memset(BE[:], 0.0))
        for ex in range(32):
            k.op(dve, [bst, pend, BE], [BE], lambda e: e.scalar_tensor_tensor(out=BE[:], in0=bst[:], scalar=pend[:, ex:ex + 1], in1=BE[:], op0=ALU.is_ge, op1=ALU.add))
        k.op(dve, [BE], [BE], lambda e: e.tensor_scalar(out=BE[:], in0=BE[:], scalar1=31.0, scalar2=1024.0, op0=ALU.min, op1=ALU.mult))
        for sl in range(ntl):
            k.op(dve, [RANK, PS], [tmp32], lambda e: e.tensor_add(out=tmp32[:], in0=RANK[:, sl, :], in1=PS[:]))
            for kq in range(2):
                k.op(dve, [tmp32, OH[kq]], [junk], lambda e: e.tensor_mul(out=junk[:, 0:32], in0=tmp32[:], in1=OH[kq][:, sl, :]))
                k.op(dve, [junk], [dstf], lambda e: e.reduce_sum(out=dstf[:, kq:kq + 1], in_=junk[:, 0:32], axis=AX.X))
            k.op(dve, [dstf], [DESTI], lambda e: e.tensor_copy(out=DESTI[:, sl, :], in_=dstf[:]))
        k.pop()
        k.push()
        zt = k.sb([128, 4, D], BF16)
        k.op(dve, [], [zt], lambda e: e.memset(zt[:], 0.0))
        xsv = xs[:].rearrange("(n p) d -> p n d", p=128)
        for b in range(NBLK):
            k.dma(sp, xsv[:, 4 * b:4 * b + 4, :], zt[:], [zt], [xs])
        hb2 = [k.sb([128, D], BF16) for _ in range(2)]
        for sl, t in enumerate(tiles):
            hb_ = hb2[sl % 2]
            k.dma(sp, hb_[:], h2d[t * 128:(t + 1) * 128, :], [h2d], [hb_])
            for kq in range(2):
                k.idma(xs[:, :], hb_[:], [hb_, DESTI], [xs], hb_, out_off=bass.IndirectOffsetOnAxis(ap=DESTI[:, sl, kq:kq + 1], axis=0), bound=NSLOT - 1)
        k.pop()
        k.push()
        stg = [k.sb([128, 8 * D], F32) for _ in range(2)]
        W = [k.sb([128, 8, D], BF16) for _ in range(3)]
        idxf = k.sb([128, 8], F32)
        idxi = [k.sb([128, 8], I32) for _ in range(2)]
        xst = [k.sb([128, D], BF16) for _ in range(4)]
        xT = k.sb([128, 8, 512], BF16)
        uT = k.sb([128, 8, 512], BF16)
        sgl = [k.sb([128, 512], F32) for _ in range(2)]
        ysb = [k.sb([128, D], F32) for _ in range(2)]
        ptr = k.ps([128, 1024], BF16)
        gvb = [k.ps([128, 512]) for _ in range(4)]
        yb = k.ps([128, 1024])
        wsrc = [w[:, :] for w in (moe_w1, moe_w3, moe_w2)]
        si = 0
        gi = 0
        yi = 0
        for b in range(NBLK):
            ix = idxi[b % 2]
            k.op(dve, [wbase, BE], [idxf], lambda e: e.tensor_scalar(out=idxf[:], in0=wbase[:], scalar1=BE[:, b:b + 1], scalar2=float(l * 32 * 1024), op0=ALU.add, op1=ALU.add))
            k.op(dve, [idxf], [ix], lambda e: e.tensor_copy(out=ix[:], in_=idxf[:]))
            for i in range(4):
                k.dma(sp, xst[i][:], xs[b * BLK + i * 128:b * BLK + (i + 1) * 128, :], [xs], [xst[i]])
            for m in range(3):
                sg_ = stg[si % 2]
                si += 1
                for kk in range(8):
                    k.idma(sg_[:, kk * D:(kk + 1) * D], wsrc[m], [moe_w1, ix], [sg_], sg_, in_off=bass.IndirectOffsetOnAxis(ap=ix[:, kk:kk + 1], axis=0), bound=2 * 32 * 1024 - 1)
                k.op(act, [sg_], [W[m]], lambda e: e.copy(out=W[m][:].rearrange("p k n -> p (k n)"), in_=sg_[:]))
            for i in range(4):
                for c in range(8):
                    k.op(pe, [xst[i], identb], [ptr], lambda e: e.transpose(ptr[:, c * 128:(c + 1) * 128], xst[i][:, c * 128:(c + 1) * 128], identb[:]))
                k.op(dve, [ptr], [xT], lambda e: e.tensor_copy(out=xT[:, :, i * 128:(i + 1) * 128], in_=ptr[:].rearrange("p (k t) -> p k t", k=8)))
            for hc in range(8):
                gb = gvb[gi % 4]
                vb = gvb[(gi + 1) % 4]
                sl_ = sgl[(gi // 2) % 2]
                gi += 2
                for kk in range(8):
                    k.op(pe, [W[0], xT], [gb], lambda e: e.matmul(gb[:], lhsT=W[0][:, kk, hc * 128:(hc + 1) * 128], rhs=xT[:, kk, :], start=(kk == 0), stop=(kk == 7)))
                for kk in range(8):
                    k.op(pe, [W[1], xT], [vb], lambda e: e.matmul(vb[:], lhsT=W[1][:, kk, hc * 128:(hc + 1) * 128], rhs=xT[:, kk, :], start=(kk == 0), stop=(kk == 7)))
                k.op(act, [gb], [sl_], lambda e: e.activation(out=sl_[:], in_=gb[:], func=AF.Silu))
                k.op(dve, [sl_, vb], [uT], lambda e: e.tensor_mul(out=uT[:, hc, :], in0=sl_[:], in1=vb[:]))
            for i in range(4):
                tc_ = slice(i * 128, (i + 1) * 128)
                for half in range(2):
                    for hc in range(8):
                        k.op(pe, [uT, W[2]], [yb], lambda e: e.matmul(yb[:, half * 512:(half + 1) * 512], lhsT=uT[:, hc, tc_], rhs=W[2][:, hc, half * 512:(half + 1) * 512], start=(hc == 0), stop=(hc == 7)))
                y_ = ysb[yi % 2]
                yi += 1
                k.op(dve, [yb], [y_], lambda e: e.tensor_copy(out=y_[:], in_=yb[:]))
                k.dma(sp, ys[b * BLK + i * 128:b * BLK + (i + 1) * 128, :], y_[:], [y_], [ys])
        k.pop()
        k.push()
        g2B = [k.sb([128, D], F32) for _ in range(2)]
        for r in range(2):
            bcast_load(g2B[r], modv[r:r + 1, 5 * D:6 * D], modv)
        if last:
            fgB = k.sb([128, D], F32)
            bcast_load(fgB, final_g[0:1, :], final_g)
        xt = [k.sb([128, D], F32) for _ in range(2)]
        y0 = [k.sb([128, D], F32) for _ in range(2)]
        y1 = [k.sb([128, D], F32) for _ in range(2)]
        f_ = k.sb([128, D], F32)
        xo = [k.sb([128, D], F32) for _ in range(2)]
        junk = k.sb([128, D], F32)
        st = k.sb([128, 4], F32)
        for sl, t in enumerate(tiles):
            pr = sl % 2
            rows = slice(t * 128, (t + 1) * 128)
            k.dma(sp, xt[pr][:], xm[rows, :], [xm], [xt[pr]])
            k.idma(y0[pr][:], ys[:, :], [ys, DESTI], [y0[pr]], y0[pr], in_off=bass.IndirectOffsetOnAxis(ap=DESTI[:, sl, 0:1], axis=0), bound=NSLOT - 1)
            k.idma(y1[pr][:], ys[:, :], [ys, DESTI], [y1[pr]], y1[pr], in_off=bass.IndirectOffsetOnAxis(ap=DESTI[:, sl, 1:2], axis=0), bound=NSLOT - 1)
            gB = g2B[1] if t < NC_ else g2B[0]
            k.op(dve, [y0[pr], GW], [f_], lambda e: e.tensor_scalar(out=f_[:], in0=y0[pr][:], scalar1=GW[:, sl, 0:1], scalar2=None, op0=ALU.mult))
            k.op(dve, [y1[pr], GW, f_], [f_], lambda e: e.scalar_tensor_tensor(out=f_[:], in0=y1[pr][:], scalar=GW[:, sl, 1:2], in1=f_[:], op0=ALU.mult, op1=ALU.add))
            k.op(dve, [f_, gB], [f_], lambda e: e.tensor_mul(out=f_[:], in0=f_[:], in1=gB[:]))
            k.op(dve, [f_, xt[pr]], [xo[pr]], lambda e: e.tensor_add(out=xo[pr][:], in0=f_[:], in1=xt[pr][:]))
            if not last:
                k.dma(sp, xnext[rows, :], xo[pr][:], [xo[pr]], [xnext])
            else:
                k.op(act, [xo[pr]], [junk, st], lambda e: e.activation(out=junk[:], in_=xo[pr][:], func=AF.Square, accum_out=st[:, 0:1]))
                k.op(dve, [st], [st], lambda e: e.tensor_scalar(out=st[:, 1:2], in0=st[:, 0:1], scalar1=1.0 / D, scalar2=EPS, op0=ALU.mult, op1=ALU.add))
                k.op(act, [st], [st], lambda e: e.activation(out=st[:, 2:3], in_=st[:, 1:2], func=AF.Sqrt))
                k.op(dve, [st], [st], lambda e: e.reciprocal(out=st[:, 3:4], in_=st[:, 2:3]))
                k.op(dve, [xo[pr], st, fgB], [junk], lambda e: e.scalar_tensor_tensor(out=junk[:], in0=xo[pr][:], scalar=st[:, 3:4], in1=fgB[:], op0=ALU.mult, op1=ALU.mult))
                k.dma(sp, out[(t - NC_) * 128:(t - NC_ + 1) * 128, :], junk[:], [junk], [out])
        k.pop()
        k.pop()

    layers = list(range(n_layers))
    xcur = xin
    for l in layers:
        last = l == n_layers - 1
        stage_ada(l)
        if stop >= 2:
            stage_norm1(l, xcur)
        if stop >= 3:
            stage_proj(l)
        if stop >= 4:
            stage_recur(l, 0)
        if stop >= 5:
            stage_recur(l, 2)
        if stop >= 6:
            stage_attn(l, not last)
        if stop >= 7:
            stage_merge(l, xcur, not last)
        if stop >= 8:
            (stage_moe if dense_moe else stage_moe2)(l, xn, last)
        xcur = xn
        if stop < 99:
            break
    if stop < 99:
        k.push()
        tmp = k.sb([128, D], F32)
        for t in range(L // 128):
            k.dma(sp, tmp[:], xin[CTX + t * 128:CTX + (t + 1) * 128, :], [xin], [tmp])
            k.dma(sp, out[t * 128:(t + 1) * 128, :], tmp[:], [tmp], [out])
        k.pop()
    k.barrier()
    return k, dbg_out


def _const_tables(L):
    T = CTX + L
    j = np.arange(128)[:, None]
    i = np.arange(128)[None, :]
    le = (j <= i).astype(np.float32)
    gt = (j > i).astype(np.float32)
    ge = (j >= i).astype(np.float32)
    lt = (j < i).astype(np.float32)
    masks = np.stack([np.tile(m, (1, 4)) for m in (le, gt, ge)]).astype(np.float32)
    tri = (np.stack([le, gt, ge, lt]) * (-1.0 / 16.0)).astype(np.float32)
    ld = np.log(1.0 - np.exp2(-5.0 - np.arange(4, dtype=np.float32))).astype(np.float32)
    spret = np.repeat((-16.0 * ld)[None, :], 128, axis=1).reshape(1, 512)
    spret = np.broadcast_to(np.repeat(-16.0 * ld, 128)[None, :], (128, 512)).astype(np.float32)

    def tab(pos, half):
        inv = (10000.0 ** (-np.arange(half, dtype=np.float32) / half)).astype(np.float32)
        ang = pos.astype(np.float32)[:, None] * inv[None, :]
        return np.cos(ang).astype(np.float32), np.sin(ang).astype(np.float32)

    rows = np.arange(L) // 64
    cols = np.arange(L) % 64
    cr, sr = tab(rows, 32)
    cc, sc = tab(cols, 32)
    rope_a = np.stack([np.concatenate([cr, cr, cc, cc], 1), np.concatenate([-sr, sr, -sc, sc], 1)]).astype(np.float32)
    c2, s2 = tab(np.arange(T), 64)
    rope_r = np.stack([np.concatenate([c2, c2], 1), np.concatenate([-s2, s2], 1)]).astype(np.float32)
    return dict(c_ident=np.eye(128, dtype=np.float32), c_masks=masks, c_tri=tri, c_spret=spret,
                c_rope_a=rope_a, c_rope_r=rope_r,
                c_bstart=(np.arange(80, dtype=np.float32) * 512.0).reshape(1, 80),
                c_wbase=(np.arange(8, dtype=np.float32)[None, :] * 128.0 + np.arange(128, dtype=np.float32)[:, None]))


def prep_inputs(inp, b, L):
    f = lambda a: np.ascontiguousarray(np.asarray(a, dtype=np.float32))
    m = {}
    m["xin"] = f(np.concatenate([inp["ctx"][b], inp["x"][b][:L]], axis=0))
    cc = np.stack([np.asarray(inp["c"][b]), np.asarray(inp["c_ctx"])])
    m["ccT"] = f(cc.reshape(2, 8, 128).transpose(2, 1, 0).reshape(128, 16))
    for n in ("w_ada", "b_ada", "norm1_g", "norm2_g", "w_in", "gla_norm_g", "ret_norm_g", "attn_sink",
              "w_br_gla", "w_br_attn", "w_br_ret", "w_out", "moe_w1", "moe_w3", "moe_w2"):
        if n in inp:
            m[n] = f(inp[n])
            if n.startswith("moe_w"):
                m[n] = m[n].reshape(2 * 32 * D, D)
    wa = np.zeros((2, 33, 1024), np.float32)
    wa[:, 0:16, 0:512] = inp["gla_wa2"][:, 0]
    wa[:, 16:32, 512:1024] = inp["gla_wa2"][:, 1]
    wa[:, 32, 0:512] = inp["gla_ba"][:, 0]
    wa[:, 32, 512:1024] = inp["gla_ba"][:, 1]
    m["wa_aug"] = wa
    m["w_r"] = f(np.concatenate([inp["moe_w_grp"], inp["moe_w_exp"]], axis=-1))
    m["b_r"] = f(np.concatenate([inp["moe_b_grp"], inp["moe_b_exp"]], axis=-1))
    m["final_g"] = f(np.asarray(inp["final_g"]).reshape(1, D))
    return m


_CACHE = {}


def kernel(**inputs):
    L = 8192
    B = 8
    if "prog" not in _CACHE:
        _CACHE["prog"] = build(L)
        _CACHE["const"] = _const_tables(L)
    k, _ = _CACHE["prog"]
    inp = {n: np.asarray(v) for n, v in inputs.items()}
    shared = None
    in_maps = []
    for b in range(B):
        m = prep_inputs(inp, b, L)
        if shared is None:
            shared = {n: v for n, v in m.items() if n not in ("xin", "ccT")}
        else:
            for n in shared:
                m[n] = shared[n]
        m.update(_CACHE["const"])
        in_maps.append(m)
    res = run_bass_kernel_spmd(k.nc, in_maps, core_ids=list(range(B)))
    return np.stack([np.asarray(r["out"]) for r in res.results]).astype(np.float32)
```

```python
import numpy as np
import concourse.bass as bass
import concourse.mybir as mybir
from concourse.bass_utils import run_bass_kernel_spmd
from contextlib import ExitStack

F32 = mybir.dt.float32
BF16 = mybir.dt.bfloat16
I32 = mybir.dt.int32
AF = mybir.ActivationFunctionType
ALU = mybir.AluOpType
AX = mybir.AxisListType

D = 1024
CTX = 256
NIN = 10784
EPS = 1e-6
C_GQ, C_GK, C_GV, C_GR, C_LR = 0, 512, 1024, 2048, 3072
C_AQ, C_AK, C_AV = 3104, 4128, 4384
C_RQ, C_RK, C_RV, C_RG = 4640, 5152, 5664, 6688
C_GATES = 7712
BIG = 1.0e4


class Buf:
    __slots__ = ("t", "w", "r", "name", "dsem", "excl", "isdram")

    def __init__(self, t, name):
        self.t = t
        self.name = name
        self.w = {}
        self.r = {}
        self.dsem = None
        self.isdram = False
        self.excl = False

    def __getitem__(self, idx):
        return self.t[idx]


class Eng:
    def __init__(self, name, eng, sem, is_pe=False):
        self.name = name
        self.eng = eng
        self.sem = sem
        self.cnt = 0
        self.waited = {}
        self.is_pe = is_pe


class K:
    def __init__(self):
        self.nc = bass.Bass("TRN2", target_bir_lowering=False)
        self.es = ExitStack()
        self.scopes = []
        nc = self.nc
        self.sems = {}
        self.dcnt = {}
        self.free_dsems = []
        self.scope_dsems = []
        self.nsem = 0
        self.pe = Eng("pe", nc.tensor, self.newsem("pe"), True)
        self.act = Eng("act", nc.scalar, self.newsem("act"))
        self.dve = Eng("dve", nc.vector, self.newsem("dve"))
        self.pool = Eng("pool", nc.gpsimd, self.newsem("pool"))
        self.sp = Eng("sp", nc.sync, self.newsem("sp"))
        self.engs = [self.pe, self.act, self.dve, self.pool, self.sp]
        self.nbuf = 0

    def newsem(self, name):
        s = self.es.enter_context(self.nc.semaphore(name))
        self.sems[name] = s
        self.nsem += 1
        return name

    def _stack(self):
        return self.scopes[-1] if self.scopes else self.es

    def sb(self, shape, dt, name=None):
        self.nbuf += 1
        name = name or f"sb{self.nbuf}"
        return Buf(self._stack().enter_context(self.nc.sbuf_tensor(name, list(shape), dt)), name)

    def ps(self, shape, dt=F32, name=None):
        self.nbuf += 1
        name = name or f"ps{self.nbuf}"
        b = Buf(self._stack().enter_context(self.nc.psum_tensor(name, list(shape), dt)), name)
        b.excl = True
        return b

    def dram(self, name, shape, dt, kind="Internal"):
        t = self.nc.dram_tensor(name, list(shape), dt, kind=kind)
        b = Buf(t.ap(), name)
        b.isdram = True
        return b

    def _sync(self, E, reads, writes):
        need = {}
        for b in reads:
            for s, v in b.w.items():
                if need.get(s, 0) < v:
                    need[s] = v
            if b.excl:
                for s, v in b.r.items():
                    if s != E.sem and need.get(s, 0) < v:
                        need[s] = v
        for b in writes:
            for s, v in b.w.items():
                if need.get(s, 0) < v:
                    need[s] = v
            for s, v in b.r.items():
                if need.get(s, 0) < v:
                    need[s] = v
        for s, v in need.items():
            if E.is_pe and s == E.sem:
                continue
            if E.waited.get(s, 0) < v:
                E.eng.wait_ge(self.sems[s], v)
                E.waited[s] = v

    def op(self, E, reads, writes, fn):
        self._sync(E, reads, writes)
        ins = fn(E.eng)
        E.cnt += 1
        ins.then_inc(self.sems[E.sem], 1)
        for b in reads:
            b.r[E.sem] = E.cnt
        for b in writes:
            b.w = {E.sem: E.cnt}
            b.r = {}
        return ins

    def dma(self, Q, out, in_, reads, writes, semb=None, **kw):
        self._sync(Q, reads, writes)
        semb = semb or (writes + reads)[0]
        if semb.dsem is None:
            if self.free_dsems:
                semb.dsem = self.free_dsems.pop()
            else:
                semb.dsem = self.newsem(f"d{self.nsem}")
                self.dcnt[semb.dsem] = 0
            if self.scopes:
                self.scope_dsems[-1].append(semb.dsem)
        ins = Q.eng.dma_start(out=out, in_=in_, **kw)
        self.dcnt[semb.dsem] += 16
        v = self.dcnt[semb.dsem]
        ins.then_inc(self.sems[semb.dsem], 16)
        for b in reads:
            b.r[semb.dsem] = v
        for b in writes:
            if b.isdram:
                b.w[semb.dsem] = v
            else:
                b.w = {semb.dsem: v}
                b.r = {}
        return ins

    def idma(self, out, in_, reads, writes, semb, out_off=None, in_off=None, bound=0):
        Q = self.pool
        self._sync(Q, reads, writes)
        if semb.dsem is None:
            if self.free_dsems:
                semb.dsem = self.free_dsems.pop()
            else:
                semb.dsem = self.newsem(f"d{self.nsem}")
                self.dcnt[semb.dsem] = 0
            if self.scopes:
                self.scope_dsems[-1].append(semb.dsem)
        if not hasattr(self, "bregs"):
            self.bregs = {}
        if bound not in self.bregs:
            self.bregs[bound] = Q.eng.to_reg(bound)
        ins = Q.eng.indirect_dma_start(out=out, out_offset=out_off, in_=in_, in_offset=in_off,
                                       bounds_check=self.bregs[bound], oob_is_err=False)
        self.dcnt[semb.dsem] += 16
        v = self.dcnt[semb.dsem]
        ins.then_inc(self.sems[semb.dsem], 16)
        for b in reads:
            b.r[semb.dsem] = v
        for b in writes:
            if b.isdram:
                b.w[semb.dsem] = v
            else:
                b.w = {semb.dsem: v}
                b.r = {}
        return ins

    def barrier(self):
        for E in self.engs:
            for E2 in self.engs:
                if E2 is E or E2.cnt == 0:
                    continue
                if E.waited.get(E2.sem, 0) < E2.cnt:
                    E.eng.wait_ge(self.sems[E2.sem], E2.cnt)
                    E.waited[E2.sem] = E2.cnt
            for s, v in self.dcnt.items():
                if v and E.waited.get(s, 0) < v:
                    E.eng.wait_ge(self.sems[s], v)
                    E.waited[s] = v

    def push(self):
        self.scopes.append(ExitStack())
        self.scope_dsems.append([])

    def pop(self):
        self.barrier()
        self.scopes.pop().close()
        self.free_dsems.extend(self.scope_dsems.pop())


def build(L, n_layers=2, dbg=(), stop=99, NEXP=32, dense_moe=False):
    T = CTX + L
    NT = T // 128
    NC_ = CTX // 128
    k = K()
    nc = k.nc
    pe, act, dve, pool, sp = k.pe, k.act, k.dve, k.pool, k.sp

    def inp(name, shape, dt=F32):
        return k.dram(name, shape, dt, kind="ExternalInput")

    xin = inp("xin", [T, D])
    ccT = inp("ccT", [128, 16])
    w_ada = inp("w_ada", [2, D, 6 * D])
    b_ada = inp("b_ada", [2, 6 * D])
    norm1_g = inp("norm1_g", [2, D])
    norm2_g = inp("norm2_g", [2, D])
    w_in = inp("w_in", [2, D, NIN])
    wa_aug = inp("wa_aug", [2, 33, 1024])
    gla_norm_g = inp("gla_norm_g", [2, D])
    ret_norm_g = inp("ret_norm_g", [2, D])
    attn_sink = inp("attn_sink", [2, 8])
    w_br = [inp("w_br_gla", [2, D, D]), inp("w_br_attn", [2, D, D]), inp("w_br_ret", [2, D, D])]
    w_out = inp("w_out", [2, D, D])
    w_r = inp("w_r", [2, D, 36])
    b_r = inp("b_r", [2, 36])
    if "nomoe" not in dbg:
        moe_w1 = inp("moe_w1", [2 * 32 * 128, 8 * D])
        moe_w3 = inp("moe_w3", [2 * 32 * 128, 8 * D])
        moe_w2 = inp("moe_w2", [2 * 32 * 128, 8 * D])
    final_g = inp("final_g", [1, D])
    c_masks = inp("c_masks", [3, 128, 512])
    c_tri = inp("c_tri", [4, 128, 128])
    c_spret = inp("c_spret", [128, 512])
    c_rope_a = inp("c_rope_a", [2, L, 128])
    c_rope_r = inp("c_rope_r", [2, T, 128])
    c_bstart = inp("c_bstart", [1, 80])
    c_wbase = inp("c_wbase", [128, 8])
    c_ident = inp("c_ident", [128, 128])

    out = k.dram("out", [L, D], F32, kind="ExternalOutput")
    dbg_out = {}

    def scratch(name, shape, dt):
        kind = "ExternalOutput" if name in dbg else "Internal"
        b = k.dram(name, shape, dt, kind=kind)
        if name in dbg:
            dbg_out[name] = b
        return b

    modv = scratch("modv", [2, 6 * D], F32)
    hT = scratch("hT", [NT, 128, 8, 128], BF16)
    proj = scratch("proj", [T, NIN], BF16)
    lrp = scratch("lrp", [T, 32], F32)
    o_part = scratch("o_part", [T, D], F32)
    yT = [scratch("yT_gla", [NT, 128, 8, 128], BF16), scratch("yT_att", [NT, 128, 8, 128], BF16),
          scratch("yT_ret", [NT, 128, 8, 128], BF16)]
    xm = scratch("xm", [T, D], F32)
    BLK = 512
    NBLKMAX = (2 * T + 32 * (BLK - 1) + BLK - 1) // BLK
    h2d = scratch("h2d", [T, D], BF16)
    xs = scratch("xs", [NBLKMAX * BLK, D], BF16)
    ys = scratch("ys", [NBLKMAX * BLK, D], F32)
    xn = scratch("xn", [T, D], F32)

    ident = k.sb([128, 128], F32, "ident")
    identb = k.sb([128, 128], BF16, "identb")
    onesb = k.sb([128, 128], BF16, "onesb")
    k.dma(sp, ident[:], c_ident[:], [c_ident], [ident])
    k.op(dve, [ident], [identb], lambda e: e.tensor_copy(out=identb[:], in_=ident[:]))
    k.op(dve, [], [onesb], lambda e: e.memset(onesb[:], 1.0))

    def bcast_load(dst, src_ap, srcbuf, q=None):
        k.dma(q or sp, dst[:], src_ap.partition_broadcast(128), [srcbuf], [dst])

    def stage_ada(l):
        k.push()
        cc = k.sb([128, 16], F32)
        sc = k.sb([128, 16], F32)
        wbuf = [k.sb([128, 8, 512], F32) for _ in range(2)]
        bb = k.sb([1, 6 * D], F32)
        one1 = k.sb([1, 2], F32)
        res = [k.sb([2, 512], F32) for _ in range(2)]
        pp = [k.ps([2, 512]) for _ in range(2)]
        k.dma(sp, cc[:], ccT[:], [ccT], [cc])
        k.dma(sp, bb[:], b_ada[l:l + 1, :], [b_ada], [bb])
        k.op(dve, [], [one1], lambda e: e.memset(one1[:], 1.0))
        k.op(act, [cc], [sc], lambda e: e.activation(out=sc[:], in_=cc[:], func=AF.Silu))
        scv = sc[:].rearrange("p (k r) -> p k r", r=2)
        for cg in range(12):
            wb = wbuf[cg % 2]
            k.dma(sp, wb[:], w_ada[l, :, cg * 512:(cg + 1) * 512].rearrange("(k p) n -> p k n", p=128), [w_ada], [wb])
            p = pp[cg % 2]
            r = res[cg % 2]
            for kk in range(8):
                k.op(pe, [sc, wb], [p], lambda e: e.matmul(p[:], lhsT=scv[:, kk, :], rhs=wb[:, kk, :], start=(kk == 0), stop=False))
            k.op(pe, [one1, bb], [p], lambda e: e.matmul(p[:], lhsT=one1[:], rhs=bb[:, cg * 512:(cg + 1) * 512], start=False, stop=True))
            k.op(act, [p], [r], lambda e: e.copy(out=r[:], in_=p[:]))
            k.dma(sp, modv[:, cg * 512:(cg + 1) * 512], r[:], [r], [modv])
        k.pop()

    def mod_tiles(l, which, ng):
        base = 0 if which == 1 else 3
        gB = k.sb([128, D], F32)
        bcast_load(gB, ng[l:l + 1, :], ng)
        outt = []
        for r in range(2):
            A = k.sb([128, D], F32)
            sh = k.sb([128, D], F32)
            bcast_load(A, modv[r:r + 1, (base + 1) * D:(base + 2) * D], modv)
            bcast_load(sh, modv[r:r + 1, base * D:(base + 1) * D], modv)
            k.op(dve, [A, gB], [A], lambda e: e.scalar_tensor_tensor(out=A[:], in0=A[:], scalar=1.0, in1=gB[:], op0=ALU.add, op1=ALU.mult))
            outt += [A, sh]
        return outt

    def rms_mod(xt, A, sh, junk, st, hout):
        k.op(act, [xt], [junk, st], lambda e: e.activation(out=junk[:], in_=xt[:], func=AF.Square, accum_out=st[:, 0:1]))
        k.op(dve, [st], [st], lambda e: e.tensor_scalar(out=st[:, 1:2], in0=st[:, 0:1], scalar1=1.0 / D, scalar2=EPS, op0=ALU.mult, op1=ALU.add))
        k.op(act, [st], [st], lambda e: e.activation(out=st[:, 2:3], in_=st[:, 1:2], func=AF.Sqrt))
        k.op(dve, [st], [st], lambda e: e.reciprocal(out=st[:, 3:4], in_=st[:, 2:3]))
        k.op(dve, [xt, st, A], [junk], lambda e: e.scalar_tensor_tensor(out=junk[:], in0=xt[:], scalar=st[:, 3:4], in1=A[:], op0=ALU.mult, op1=ALU.mult))
        k.op(dve, [junk, sh], [hout], lambda e: e.tensor_add(out=hout[:], in0=junk[:], in1=sh[:]))

    def transpose8(src, dstT, ptr, idt, evac):
        for c in range(8):
            k.op(pe, [src, idt], [ptr], lambda e: e.transpose(ptr[:, c * 128:(c + 1) * 128], src[:, c * 128:(c + 1) * 128], idt[:]))
        if evac is act:
            k.op(act, [ptr], [dstT], lambda e: e.copy(out=dstT[:].rearrange("p k t -> p (k t)"), in_=ptr[:]))
        else:
            k.op(dve, [ptr], [dstT], lambda e: e.tensor_copy(out=dstT[:].rearrange("p k t -> p (k t)"), in_=ptr[:]))

    def stage_norm1(l, xcur):
        k.push()
        A_l, sh_l, A_c, sh_c = mod_tiles(l, 1, norm1_g)
        xt = [k.sb([128, D], F32) for _ in range(2)]
        junk = k.sb([128, D], F32)
        st = k.sb([128, 4], F32)
        hb = k.sb([128, D], BF16)
        hTt = [k.sb([128, 8, 128], BF16) for _ in range(2)]
        ptr = k.ps([128, 1024], BF16)
        for t in range(NT):
            x_ = xt[t % 2]
            k.dma(sp, x_[:], xcur[t * 128:(t + 1) * 128, :], [xcur], [x_])
            A, sh = (A_c, sh_c) if t < NC_ else (A_l, sh_l)
            rms_mod(x_, A, sh, junk, st, hb)
            h_ = hTt[t % 2]
            transpose8(hb, h_, ptr, identb, act)
            k.dma(pool, hT[t], h_[:], [h_], [hT])
        k.pop()

    def stage_proj(l):
        k.push()
        PW = 2048
        passes = [(c0, min(c0 + PW, NIN)) for c0 in range(0, NIN, PW)]
        wt = [k.sb([128, 8, PW], BF16) for _ in range(2)]
        ht = [k.sb([128, 8, 128], BF16) for _ in range(2)]
        ot = [k.sb([128, PW], BF16) for _ in range(2)]
        lrt = k.sb([128, 32], F32)
        pb = [k.ps([128, 512]) for _ in range(6)]
        ev = 0
        it = 0
        for pi, (c0, c1) in enumerate(passes):
            w_ = wt[pi % 2]
            ncol = c1 - c0
            for kk in range(8):
                k.dma(pool, w_[:, kk, 0:ncol], w_in[l, kk * 128:(kk + 1) * 128, c0:c1], [w_in], [w_])
            for t in range(NT):
                h_ = ht[it % 2]
                o_ = ot[it % 2]
                it += 1
                k.dma(sp, h_[:], hT[t], [hT], [h_])
                for n0 in range(0, ncol, 512):
                    n1 = min(n0 + 512, ncol)
                    p = pb[ev % 6]
                    for kk in range(8):
                        k.op(pe, [h_, w_], [p], lambda e: e.matmul(p[:, 0:n1 - n0], lhsT=h_[:, kk, :], rhs=w_[:, kk, n0:n1], start=(kk == 0), stop=(kk == 7)))
                    if ev % 2 == 0:
                        k.op(act, [p], [o_], lambda e: e.copy(out=o_[:, n0:n1], in_=p[:, 0:n1 - n0]))
                    else:
                        k.op(dve, [p], [o_], lambda e: e.tensor_copy(out=o_[:, n0:n1], in_=p[:, 0:n1 - n0]))
                    if c0 + n0 <= C_LR < c0 + n1:
                        off = C_LR - c0 - n0
                        k.op(dve, [p], [lrt, p], lambda e: e.tensor_copy(out=lrt[:], in_=p[:, off:off + 32]))
                        k.dma(sp, lrp[t * 128:(t + 1) * 128, :], lrt[:], [lrt], [lrp])
                    ev += 1
                k.dma(pool, proj[t * 128:(t + 1) * 128, c0:c1], o_[:, 0:ncol], [o_], [proj])
        k.pop()


    def rope_apply(dst, src, rp, nh, blk, tmp):
        G = 128 // (2 * blk)
        sv = src[:].rearrange("p (h g t d) -> p (h g) t d", h=nh, g=G, t=2)
        tv = tmp[:].rearrange("p (h g t d) -> p (h g) t d", h=nh, g=G, t=2)
        cosb = rp[:, 0, :].rearrange("p (o d) -> p o d", o=1).to_broadcast([128, nh, 128])
        sinv = rp[:, 1, :].rearrange("p (o g t d) -> p (o g) t d", o=1, g=G, t=2)
        k.op(dve, [src, rp], [dst], lambda e: e.tensor_mul(out=dst[:].rearrange("p (h d) -> p h d", h=nh), in0=src[:].rearrange("p (h d) -> p h d", h=nh), in1=cosb))
        if G == 1:
            for tt in range(2):
                k.op(dve, [src, rp], [tmp], lambda e: e.tensor_mul(out=tv[:, :, tt, :], in0=sv[:, :, 1 - tt, :], in1=sinv[:, :, tt, :].to_broadcast([128, nh, blk])))
        else:
            s4 = src[:].rearrange("p (h g t d) -> p h g t d", h=nh, g=G, t=2)
            t4 = tmp[:].rearrange("p (h g t d) -> p h g t d", h=nh, g=G, t=2)
            r4 = rp[:, 1, :].rearrange("p (g t d) -> p g t d", g=G, t=2)
            for g in range(G):
                for tt in range(2):
                    k.op(dve, [src, rp], [tmp], lambda e: e.tensor_mul(out=t4[:, :, g, tt, :], in0=s4[:, :, g, 1 - tt, :], in1=r4[:, g, tt, :].rearrange("p (o d) -> p o d", o=1).to_broadcast([128, nh, blk])))
        k.op(dve, [dst, tmp], [dst], lambda e: e.tensor_add(out=dst[:], in0=dst[:], in1=tmp[:]))

    def stage_recur(l, kind):
        k.push()
        is_gla = kind == 0
        cq, cv, cg = (C_GQ, C_GV, C_GR) if is_gla else (C_RQ, C_RV, C_RG)
        ng = gla_norm_g if is_gla else ret_norm_g
        scale = 128.0 ** -0.5
        masks = k.sb([128, 2, 512], F32)
        tri = k.sb([128, 4, 128], F32)
        n16 = k.sb([128, 1], F32)
        gainB = k.sb([128, D], F32)
        k.dma(sp, masks[:], c_masks[0:2].rearrange("m j c -> j m c"), [c_masks], [masks])
        k.dma(sp, tri[:], c_tri[:].rearrange("m j c -> j m c"), [c_tri], [tri])
        k.op(dve, [], [n16], lambda e: e.memset(n16[:], -1.0 / 16.0))
        bcast_load(gainB, ng[l:l + 1, :], ng)
        sp_p = [k.sb([128, 512], F32) for _ in range(2 if is_gla else 1)]
        if is_gla:
            WA = k.sb([33, 1024], F32)
            lrT_p = [k.sb([33, 128], F32) for _ in range(2)]
            k.dma(sp, WA[:], wa_aug[l], [wa_aug], [WA])
            for lrT in lrT_p:
                k.op(dve, [], [lrT], lambda e: e.memset(lrT[:], 1.0))
        else:
            k.dma(sp, sp_p[0][:], c_spret[:], [c_spret], [sp_p[0]])
        S = k.sb([128, D], F32)
        Sbf = k.sb([128, D], BF16)
        qk = [k.sb([128, 1024], BF16) for _ in range(2)]
        vt = [k.sb([128, 1024], BF16) for _ in range(2)]
        lrt = [k.sb([128, 32], F32) for _ in range(2)]
        gt = [k.sb([128, 1024], BF16) for _ in range(2)]
        oft = [k.sb([128, 1024], F32) for _ in range(2)]
        rpt = [k.sb([128, 2, 128], F32) for _ in range(2)]
        def pair(shape, dt):
            return [k.sb(shape, dt) for _ in range(2)]
        nd = 2 if is_gla else 1
        eq_p = [k.sb([128, 512], F32) for _ in range(nd)]
        ekin_p = [k.sb([128, 512], F32) for _ in range(nd)]
        ekout_p = [k.sb([128, 512], F32) for _ in range(nd)]
        dec_p = [k.sb([128, 4], F32) for _ in range(nd)]
        qkr_p = pair([128, 1024], F32)
        t2_p = pair([128, 1024], F32)
        qkin_p = pair([128, 1024], BF16)
        kout_p = pair([128, 512], BF16)
        qkT_p = pair([128, 8, 128], BF16)
        PT_p = pair([128, 512], BF16)
        osb_p = pair([128, 1024], F32)
        junk_p = pair([128, 1024], F32)
        yn_p = pair([128, 1024], F32)
        sg_p = pair([128, 1024], F32)
        yb_p = pair([128, 1024], BF16)
        yTt_p = pair([128, 8, 128], BF16)
        st_p = pair([128, 32], F32)
        bA = k.ps([128, 512])
        bB = k.ps([128, 512])
        bC = k.ps([128, 512])
        ptr = k.ps([128, 1024], BF16)
        ob = k.ps([128, 1024])
        dsb = k.ps([128, 1024])

        it = 0
        for dirn in range(2):
            order = list(range(NT)) if dirn == 0 else (list(range(NC_ - 1, -1, -1)) + list(range(NT - 1, NC_ - 1, -1)))
            k.op(dve, [], [S], lambda e: e.memset(S[:], 0.0))
            k.op(dve, [], [Sbf], lambda e: e.memset(Sbf[:], 0.0))
            decay_done = False
            for t in order:
                pr = it % 2
                it += 1
                rows = slice(t * 128, (t + 1) * 128)
                qk_, v_, lr_, g_, of_, rp_ = qk[pr], vt[pr], lrt[pr], gt[pr], oft[pr], rpt[pr]
                pd = pr if is_gla else 0
                sp_t, eq, ekin, ekout, dec = sp_p[pd], eq_p[pd], ekin_p[pd], ekout_p[pd], dec_p[pd]
                if is_gla:
                    lrT = lrT_p[pr]
                qkr, t2, qkin, kout, qkT, PT = qkr_p[pr], t2_p[pr], qkin_p[pr], kout_p[pr], qkT_p[pr], PT_p[pr]
                osb, junk, yn, sg, yb, yTt, st = osb_p[pr], junk_p[pr], yn_p[pr], sg_p[pr], yb_p[pr], yTt_p[pr], st_p[pr]
                k.dma(sp, qk_[:], proj[rows, cq:cq + 1024], [proj], [qk_])
                k.dma(sp, v_[:], proj[rows, cv:cv + 1024], [proj], [v_])
                if is_gla:
                    k.dma(sp, lr_[:], lrp[rows, :], [lrp], [lr_])
                else:
                    k.dma(sp, rp_[:], c_rope_r[:, rows, :].rearrange("c t d -> t c d"), [c_rope_r], [rp_])
                if dirn == 1:
                    k.dma(sp, g_[:], proj[rows, cg:cg + 1024], [proj], [g_])
                    k.dma(sp, of_[:], o_part[rows, :], [o_part], [of_])
                if is_gla:
                    k.op(pe, [lr_, ident], [bC], lambda e: e.transpose(bC[0:32, 0:128], lr_[:], ident[:]))
                    k.op(act, [bC], [lrT], lambda e: e.copy(out=lrT[0:32, :], in_=bC[0:32, 0:128]))
                    k.op(pe, [lrT, WA], [bA], lambda e: e.matmul(bA[:], lhsT=lrT[:], rhs=WA[:, dirn * 512:(dirn + 1) * 512], start=True, stop=True))
                    k.op(act, [bA], [sp_t], lambda e: e.activation(out=sp_t[:], in_=bA[:], func=AF.Exp, scale=-1.0))
                    k.op(act, [sp_t], [sp_t], lambda e: e.activation(out=sp_t[:], in_=sp_t[:], func=AF.Ln, bias=1.0))
                if is_gla or not decay_done:
                    decay_done = True
                    k.op(pe, [tri, sp_t], [bB], lambda e: e.matmul(bB[:], lhsT=tri[:, 2 * dirn, :], rhs=sp_t[:], start=True, stop=True))
                    k.op(pe, [tri, sp_t], [bA], lambda e: e.matmul(bA[:], lhsT=tri[:, 2 * dirn + 1, :], rhs=sp_t[:], start=True, stop=True))
                    for h in range(4):
                        k.op(pe, [sp_t, n16], [bC], lambda e: e.matmul(bC[:, 128 + h:129 + h], lhsT=sp_t[:, h * 128:(h + 1) * 128], rhs=n16[:], start=True, stop=True))
                    k.op(act, [bB], [eq], lambda e: e.activation(out=eq[:], in_=bB[:], func=AF.Exp))
                    k.op(act, [bB], [ekin], lambda e: e.activation(out=ekin[:], in_=bB[:], func=AF.Exp, scale=-1.0))
                    k.op(act, [bA], [ekout], lambda e: e.activation(out=ekout[:], in_=bA[:], func=AF.Exp))
                    k.op(act, [bC], [dec], lambda e: e.activation(out=dec[:], in_=bC[:, 128:132], func=AF.Exp))
                if is_gla:
                    src = qk_
                else:
                    rope_apply(qkr, qk_, rp_, 8, 64, t2)
                    src = qkr
                k.op(dve, [src, eq], [qkin], lambda e: e.scalar_tensor_tensor(out=qkin[:, 0:512], in0=src[:, 0:512], scalar=scale, in1=eq[:], op0=ALU.mult, op1=ALU.mult))
                k.op(dve, [src, ekin], [qkin], lambda e: e.tensor_mul(out=qkin[:, 512:1024], in0=src[:, 512:1024], in1=ekin[:]))
                k.op(dve, [src, ekout], [kout], lambda e: e.tensor_mul(out=kout[:], in0=src[:, 512:1024], in1=ekout[:]))
                transpose8(qkin, qkT, ptr, identb, act)
                for h in range(4):
                    k.op(pe, [qkT], [bC], lambda e: e.matmul(bC[:, h * 128:(h + 1) * 128], lhsT=qkT[:, 4 + h, :], rhs=qkT[:, h, :], start=True, stop=True))
                k.op(dve, [bC, masks], [PT], lambda e: e.tensor_mul(out=PT[:], in0=bC[:], in1=masks[:, dirn, :]))
                for h in range(4):
                    hs = slice(h * 256, (h + 1) * 256)
                    k.op(pe, [PT, v_], [ob], lambda e: e.matmul(ob[:, hs], lhsT=PT[:, h * 128:(h + 1) * 128], rhs=v_[:, hs], start=True, stop=False))
                    k.op(pe, [qkT, Sbf], [ob], lambda e: e.matmul(ob[:, hs], lhsT=qkT[:, h, :], rhs=Sbf[:, hs], start=False, stop=True))
                for h in range(4):
                    hs = slice(h * 256, (h + 1) * 256)
                    k.op(pe, [kout, v_], [dsb], lambda e: e.matmul(dsb[:, hs], lhsT=kout[:, h * 128:(h + 1) * 128], rhs=v_[:, hs], start=True, stop=True))
                for h in range(4):
                    hs = slice(h * 256, (h + 1) * 256)
                    k.op(dve, [S, dec, dsb], [S], lambda e: e.scalar_tensor_tensor(out=S[:, hs], in0=S[:, hs], scalar=dec[:, h:h + 1], in1=dsb[:, hs], op0=ALU.mult, op1=ALU.add))
                k.op(act, [S], [Sbf], lambda e: e.copy(out=Sbf[:], in_=S[:]))
                if dirn == 0:
                    k.op(act, [ob], [osb], lambda e: e.copy(out=osb[:], in_=ob[:]))
                    k.dma(pool, o_part[rows, :], osb[:], [osb], [o_part])
                else:
                    k.op(dve, [ob, of_], [osb], lambda e: e.tensor_add(out=osb[:], in0=ob[:], in1=of_[:]))
                    k.op(dve, [osb], [st], lambda e: e.reduce_sum(out=st[:, 0:4], in_=osb[:].rearrange("p (h d) -> p h d", h=4), axis=AX.X))
                    k.op(act, [osb], [junk], lambda e: e.activation(out=junk[:], in_=osb[:], func=AF.Square))
                    k.op(dve, [junk], [st], lambda e: e.reduce_sum(out=st[:, 4:8], in_=junk[:].rearrange("p (h d) -> p h d", h=4), axis=AX.X))
                    k.op(dve, [st], [st], lambda e: e.tensor_scalar(out=st[:, 8:16], in0=st[:, 0:8], scalar1=1.0 / 256.0, scalar2=None, op0=ALU.mult))
                    k.op(dve, [st], [st], lambda e: e.tensor_mul(out=st[:, 16:20], in0=st[:, 8:12], in1=st[:, 8:12]))
                    k.op(dve, [st], [st], lambda e: e.tensor_sub(out=st[:, 20:24], in0=st[:, 12:16], in1=st[:, 16:20]))
                    k.op(dve, [st], [st], lambda e: e.tensor_scalar(out=st[:, 20:24], in0=st[:, 20:24], scalar1=EPS, scalar2=None, op0=ALU.add))
                    k.op(act, [st], [st], lambda e: e.activation(out=st[:, 24:28], in_=st[:, 20:24], func=AF.Sqrt))
                    k.op(dve, [st], [st], lambda e: e.reciprocal(out=st[:, 28:32], in_=st[:, 24:28]))
                    for h in range(4):
                        hs = slice(h * 256, (h + 1) * 256)
                        k.op(dve, [osb, st], [yn], lambda e: e.tensor_scalar(out=yn[:, hs], in0=osb[:, hs], scalar1=st[:, 8 + h:9 + h], scalar2=st[:, 28 + h:29 + h], op0=ALU.subtract, op1=ALU.mult))
                    k.op(act, [g_], [sg], lambda e: e.activation(out=sg[:], in_=g_[:], func=AF.Silu))
                    k.op(dve, [yn, gainB], [yn], lambda e: e.tensor_mul(out=yn[:], in0=yn[:], in1=gainB[:]))
                    k.op(dve, [yn, sg], [yb], lambda e: e.tensor_mul(out=yb[:], in0=yn[:], in1=sg[:]))
                    transpose8(yb, yTt, ptr, identb, act)
                    k.dma(pool, yT[kind][t], yTt[:], [yTt], [yT[kind]])
        k.pop()


    def stage_attn(l, with_ctx):
        k.push()
        scale = 128.0 ** -0.5
        kT_all = k.sb([128, 2, T], BF16)
        v_all = k.sb([128, NT, 256], BF16)
        masks = k.sb([128, 3, 512], F32)
        k.dma(sp, masks[:], c_masks[:].rearrange("m j c -> j m c"), [c_masks], [masks])
        sE = k.sb([128, 8], F32)
        onesf = k.sb([128, 128], F32)
        sinkE = k.sb([128, 8, 128], F32)
        bcast_load(sE, attn_sink[l:l + 1, :], attn_sink)
        k.op(act, [sE], [sE], lambda e: e.activation(out=sE[:], in_=sE[:], func=AF.Exp))
        k.op(dve, [], [onesf], lambda e: e.memset(onesf[:], 1.0))
        for h in range(8):
            k.op(dve, [onesf, sE], [sinkE], lambda e: e.tensor_scalar(out=sinkE[:, h, :], in0=onesf[:], scalar1=sE[:, h:h + 1], scalar2=None, op0=ALU.mult))
        for t0 in range(0, NT, 16):
            t1 = min(NT, t0 + 16)
            k.dma(sp, v_all[:, t0:t1, :], proj[t0 * 128:t1 * 128, C_AV:C_AV + 256].rearrange("(t p) c -> p t c", p=128), [proj], [v_all])
        kt = [k.sb([128, 256], BF16) for _ in range(2)]
        rpt = [k.sb([128, 2, 128], F32) for _ in range(2)]
        kr = k.sb([128, 256], F32)
        tmpk = k.sb([128, 256], F32)
        kb16 = k.sb([128, 256], BF16)
        ptr = k.ps([128, 1024], BF16)
        for t in range(NT):
            k_ = kt[t % 2]
            rp_ = rpt[t % 2]
            rows = slice(t * 128, (t + 1) * 128)
            k.dma(sp, k_[:], proj[rows, C_AK:C_AK + 256], [proj], [k_])
            if t >= NC_:
                n = t - NC_
                k.dma(sp, rp_[:], c_rope_a[:, n * 128:(n + 1) * 128, :].rearrange("c t d -> t c d"), [c_rope_a], [rp_])
                rope_apply(kr, k_, rp_, 2, 32, tmpk)
                k.op(act, [kr], [kb16], lambda e: e.copy(out=kb16[:], in_=kr[:]))
                src = kb16
            else:
                src = k_
            for h in range(2):
                k.op(pe, [src, identb], [ptr], lambda e: e.transpose(ptr[:, h * 128:(h + 1) * 128], src[:, h * 128:(h + 1) * 128], identb[:]))
            k.op(act, [ptr], [kT_all], lambda e: e.copy(out=kT_all[:, :, rows], in_=ptr[:, 0:256].rearrange("p (h t) -> p h t", h=2)))
        qt = [k.sb([128, 1024], BF16) for _ in range(2)]
        qr = k.sb([128, 1024], F32)
        tmpq = k.sb([128, 1024], F32)
        qs = k.sb([128, 1024], BF16)
        qT = k.sb([128, 8, 128], BF16)
        Pt = [k.sb([128, 512], BF16) for _ in range(3)]
        rden = k.sb([128, 512], F32)
        yTt = [k.sb([128, 8, 128], BF16) for _ in range(2)]
        scb = [k.ps([128, 512]) for _ in range(3)]
        outb = [k.ps([128, 512]) for _ in range(2)]
        denb = [k.ps([128, 512]) for _ in range(2)]
        qtiles = list(range(0 if with_ctx else NC_, NT))
        for qi, t in enumerate(qtiles):
            q_ = qt[qi % 2]
            rp_ = rpt[qi % 2]
            y_ = yTt[qi % 2]
            rows = slice(t * 128, (t + 1) * 128)
            k.dma(sp, q_[:], proj[rows, C_AQ:C_AQ + 1024], [proj], [q_])
            if t >= NC_:
                n = t - NC_
                k.dma(sp, rp_[:], c_rope_a[:, n * 128:(n + 1) * 128, :].rearrange("c t d -> t c d"), [c_rope_a], [rp_])
                rope_apply(qr, q_, rp_, 8, 32, tmpq)
                k.op(dve, [qr], [qs], lambda e: e.tensor_scalar(out=qs[:], in0=qr[:], scalar1=scale, scalar2=None, op0=ALU.mult))
            else:
                k.op(dve, [q_], [qs], lambda e: e.tensor_scalar(out=qs[:], in0=q_[:], scalar1=scale, scalar2=None, op0=ALU.mult))
            transpose8(qs, qT, ptr, identb, act)
            kbs = [(c, None) for c in range(NC_)]
            if t >= NC_:
                n = t - NC_
                if n >= 1:
                    kbs.append((t - 1, 2))
                kbs.append((t, None))
                if t + 1 < NT:
                    kbs.append((t + 1, 0))
            items = [(g, kb, mi, j == 0, j == len(kbs) - 1) for g in range(2) for j, (kb, mi) in enumerate(kbs)]

            def emit_sc(ii):
                g, kb, mi, first, last = items[ii]
                sc = scb[ii % 3]
                k.op(pe, [kT_all, qT], [sc], lambda e: e.matmul(sc[:], lhsT=kT_all[:, g, kb * 128:(kb + 1) * 128], rhs=qT[:, 4 * g:4 * g + 4, :].rearrange("p h t -> p (h t)"), start=True, stop=True))

            def emit_pv(ii):
                g, kb, mi, first, last = items[ii]
                sc = scb[ii % 3]
                P = Pt[ii % 3]
                k.op(act, [sc], [P], lambda e: e.activation(out=P[:], in_=sc[:], func=AF.Exp))
                if mi is not None:
                    k.op(dve, [P, masks], [P], lambda e: e.tensor_mul(out=P[:], in0=P[:], in1=masks[:, mi, :]))
                k.op(pe, [v_all, P], [outb[g]], lambda e: e.matmul(outb[g][:], lhsT=v_all[:, kb, g * 128:(g + 1) * 128], rhs=P[:], start=first, stop=last))
                k.op(pe, [onesb, P], [denb[g]], lambda e: e.matmul(denb[g][:], lhsT=onesb[:], rhs=P[:], start=first, stop=last))
                if last:
                    k.op(dve, [denb[g], sinkE], [rden], lambda e: e.tensor_add(out=rden[:], in0=denb[g][:], in1=sinkE[:, 4 * g:4 * g + 4, :].rearrange("p h t -> p (h t)")))
                    k.op(dve, [rden], [rden], lambda e: e.reciprocal(out=rden[:], in_=rden[:]))
                    k.op(dve, [outb[g], rden], [y_], lambda e: e.tensor_mul(out=y_[:, 4 * g:4 * g + 4, :].rearrange("p h t -> p (h t)"), in0=outb[g][:], in1=rden[:]))

            emit_sc(0)
            for ii in range(len(items)):
                if ii + 1 < len(items):
                    emit_sc(ii + 1)
                emit_pv(ii)
            k.dma(pool, yT[1][t], y_[:], [y_], [yT[1]])
        k.pop()


    def load_w_bf16(dst, src2d):
        for kk in range(8):
            k.dma(pool, dst[:, kk, :], src2d[kk * 128:(kk + 1) * 128, :], [w_out], [dst])

    def stage_merge(l, xcur, with_ctx):
        k.push()
        wbr = [k.sb([128, 8, 1024], BF16) for _ in range(3)]
        wo = k.sb([128, 8, 1024], BF16)
        for b in range(3):
            load_w_bf16(wbr[b], w_br[b][l])
        load_w_bf16(wo, w_out[l])
        g1B = [k.sb([128, D], F32) for _ in range(2)]
        for r in range(2):
            bcast_load(g1B[r], modv[r:r + 1, 2 * D:3 * D], modv)
        yTb = [[k.sb([128, 8, 128], BF16) for _ in range(3)] for _ in range(2)]
        gat = [k.sb([128, 3072], BF16) for _ in range(2)]
        xt = [k.sb([128, D], F32) for _ in range(2)]
        sig = k.sb([128, 3072], F32)
        merged = k.sb([128, D], F32)
        tmp = k.sb([128, D], F32)
        mb = k.sb([128, D], BF16)
        mT = k.sb([128, 8, 128], BF16)
        xo = [k.sb([128, D], F32) for _ in range(2)]
        brb = [k.ps([128, 1024]) for _ in range(2)]
        ob = k.ps([128, 1024])
        ptr = k.ps([128, 1024], BF16)
        tiles = list(range(0 if with_ctx else NC_, NT))
        bi = 0
        for i, t in enumerate(tiles):
            pr = i % 2
            rows = slice(t * 128, (t + 1) * 128)
            for b in range(3):
                k.dma(sp, yTb[pr][b][:], yT[b][t], [yT[b]], [yTb[pr][b]])
            k.dma(sp, gat[pr][:], proj[rows, C_GATES:C_GATES + 3072], [proj], [gat[pr]])
            k.dma(sp, xt[pr][:], xcur[rows, :], [xcur], [xt[pr]])
            k.op(act, [gat[pr]], [sig], lambda e: e.activation(out=sig[:], in_=gat[pr][:], func=AF.Sigmoid))
            for b in range(3):
                pb_ = brb[bi % 2]
                bi += 1
                y_ = yTb[pr][b]
                for half in range(2):
                    for kk in range(8):
                        k.op(pe, [y_, wbr[b]], [pb_], lambda e: e.matmul(pb_[:, half * 512:(half + 1) * 512], lhsT=y_[:, kk, :], rhs=wbr[b][:, kk, half * 512:(half + 1) * 512], start=(kk == 0), stop=(kk == 7)))
                if b == 0:
                    k.op(dve, [pb_, sig], [merged], lambda e: e.tensor_mul(out=merged[:], in0=pb_[:], in1=sig[:, 0:1024]))
                else:
                    k.op(dve, [pb_, sig], [tmp], lambda e: e.tensor_mul(out=tmp[:], in0=pb_[:], in1=sig[:, b * 1024:(b + 1) * 1024]))
                    k.op(dve, [merged, tmp], [merged], lambda e: e.tensor_add(out=merged[:], in0=merged[:], in1=tmp[:]))
            k.op(act, [merged], [mb], lambda e: e.copy(out=mb[:], in_=merged[:]))
            transpose8(mb, mT, ptr, identb, act)
            for half in range(2):
                for kk in range(8):
                    k.op(pe, [mT, wo], [ob], lambda e: e.matmul(ob[:, half * 512:(half + 1) * 512], lhsT=mT[:, kk, :], rhs=wo[:, kk, half * 512:(half + 1) * 512], start=(kk == 0), stop=(kk == 7)))
            gB = g1B[1] if t < NC_ else g1B[0]
            k.op(dve, [ob, gB], [tmp], lambda e: e.tensor_mul(out=tmp[:], in0=ob[:], in1=gB[:]))
            k.op(dve, [tmp, xt[pr]], [xo[pr]], lambda e: e.tensor_add(out=xo[pr][:], in0=tmp[:], in1=xt[pr][:]))
            k.dma(pool, xm[rows, :], xo[pr][:], [xo[pr]], [xm])
        k.pop()


    def stage_moe(l, xnext, last):
        k.push()
        GS = 12
        A_l, sh_l, A_c, sh_c = mod_tiles(l, 2, norm2_g)
        g2B = [k.sb([128, D], F32) for _ in range(2)]
        for r in range(2):
            bcast_load(g2B[r], modv[r:r + 1, 5 * D:6 * D], modv)
        if last:
            fgB = k.sb([128, D], F32)
            bcast_load(fgB, final_g[0:1, :], final_g)
        wr = k.sb([128, 8, 36], F32)
        brB = k.sb([128, 36], F32)
        k.dma(sp, wr[:], w_r[l].rearrange("(k p) n -> p k n", p=128), [w_r], [wr])
        bcast_load(brB, b_r[l:l + 1, :], b_r)
        acc = k.sb([128, GS, D], F32)
        hTb = k.sb([128, 8, GS * 128], BF16)
        Gd = k.sb([128, GS, 32], F32)
        W = [k.sb([128, 8, 1024], BF16) for _ in range(3)]
        uT = k.sb([128, 8, 512], BF16)
        sgl = [k.sb([128, 512], F32) for _ in range(2)]
        xt = [k.sb([128, D], F32) for _ in range(2)]
        junk = k.sb([128, D], F32)
        h2 = k.sb([128, D], F32)
        h2T = k.sb([128, 8, 128], F32)
        st = k.sb([128, 4], F32)
        rt = k.sb([128, 64], F32)
        lgs = k.sb([128, 36], F32)
        em = k.sb([128, 32], F32)
        em2 = k.sb([128, 32], F32)
        oh1 = k.sb([128, 32], F32)
        oh2 = k.sb([128, 32], F32)
        ptrf = k.ps([128, 1024])
        gvb = [k.ps([128, 512]) for _ in range(4)]
        yb = k.ps([128, 1024])
        tiles = list(range(NC_ if last else 0, NT))
        groups = [tiles[i:i + GS] for i in range(0, len(tiles), GS)]
        for grp in groups:
            for sl, t in enumerate(grp):
                x_ = xt[sl % 2]
                rows = slice(t * 128, (t + 1) * 128)
                k.dma(sp, x_[:], xm[rows, :], [xm], [x_])
                A, sh = (A_c, sh_c) if t < NC_ else (A_l, sh_l)
                rms_mod(x_, A, sh, junk, st, h2)
                transpose8(h2, h2T, ptrf, ident, act)
                k.op(dve, [h2T], [hTb], lambda e: e.tensor_copy(out=hTb[:, :, sl * 128:(sl + 1) * 128], in_=h2T[:]))
                for kk in range(8):
                    k.op(pe, [h2T, wr], [ptrf], lambda e: e.matmul(ptrf[:, 0:36], lhsT=h2T[:, kk, :], rhs=wr[:, kk, :], start=(kk == 0), stop=(kk == 7)))
                k.op(dve, [ptrf, brB], [lgs], lambda e: e.tensor_add(out=lgs[:], in0=ptrf[:, 0:36], in1=brB[:]))
                k.op(dve, [lgs], [rt], lambda e: e.reduce_max(out=rt[:, 0:1], in_=lgs[:, 0:4], axis=AX.X))
                k.op(dve, [lgs, rt], [rt], lambda e: e.tensor_scalar(out=rt[:, 4:8], in0=lgs[:, 0:4], scalar1=rt[:, 0:1], scalar2=None, op0=ALU.is_equal))
                k.op(dve, [rt], [rt], lambda e: e.tensor_scalar(out=rt[:, 1:2], in0=rt[:, 0:1], scalar1=-1.0, scalar2=None, op0=ALU.mult))
                k.op(act, [lgs, rt], [rt], lambda e: e.activation(out=rt[:, 8:12], in_=lgs[:, 0:4], func=AF.Exp, bias=rt[:, 1:2], scale=1.0, accum_out=rt[:, 2:3]))
                k.op(dve, [rt], [rt], lambda e: e.reciprocal(out=rt[:, 3:4], in_=rt[:, 2:3]))
                k.op(dve, [rt], [rt], lambda e: e.tensor_scalar(out=rt[:, 12:16], in0=rt[:, 4:8], scalar1=1.0, scalar2=BIG, op0=ALU.subtract, op1=ALU.mult))
                k.op(dve, [lgs, rt], [em], lambda e: e.tensor_tensor(out=em[:].rearrange("p (g e) -> p g e", g=4), in0=lgs[:, 4:36].rearrange("p (g e) -> p g e", g=4), in1=rt[:, 12:16].rearrange("p (g o) -> p g o", o=1).to_broadcast([128, 4, 8]), op=ALU.add))
                k.op(dve, [em], [rt], lambda e: e.reduce_max(out=rt[:, 16:17], in_=em[:], axis=AX.X))
                k.op(dve, [em, rt], [oh1], lambda e: e.tensor_scalar(out=oh1[:], in0=em[:], scalar1=rt[:, 16:17], scalar2=None, op0=ALU.is_equal))
                k.op(dve, [oh1, em], [em2], lambda e: e.scalar_tensor_tensor(out=em2[:], in0=oh1[:], scalar=-BIG, in1=em[:], op0=ALU.mult, op1=ALU.add))
                k.op(dve, [em2], [rt], lambda e: e.reduce_max(out=rt[:, 17:18], in_=em2[:], axis=AX.X))
                k.op(dve, [em2, rt], [oh2], lambda e: e.tensor_scalar(out=oh2[:], in0=em2[:], scalar1=rt[:, 17:18], scalar2=None, op0=ALU.is_equal))
                k.op(dve, [rt], [rt], lambda e: e.tensor_sub(out=rt[:, 18:19], in0=rt[:, 17:18], in1=rt[:, 16:17]))
                k.op(act, [rt], [rt], lambda e: e.activation(out=rt[:, 19:20], in_=rt[:, 18:19], func=AF.Exp))
                k.op(dve, [rt], [rt], lambda e: e.tensor_scalar(out=rt[:, 20:21], in0=rt[:, 19:20], scalar1=1.0, scalar2=None, op0=ALU.add))
                k.op(dve, [rt], [rt], lambda e: e.reciprocal(out=rt[:, 21:22], in_=rt[:, 20:21]))
                k.op(dve, [rt], [rt], lambda e: e.tensor_mul(out=rt[:, 22:23], in0=rt[:, 19:20], in1=rt[:, 21:22]))
                k.op(dve, [rt], [rt], lambda e: e.tensor_mul(out=rt[:, 23:24], in0=rt[:, 21:22], in1=rt[:, 3:4]))
                k.op(dve, [rt], [rt], lambda e: e.tensor_mul(out=rt[:, 24:25], in0=rt[:, 22:23], in1=rt[:, 3:4]))
                k.op(dve, [oh1, rt], [Gd], lambda e: e.tensor_scalar(out=Gd[:, sl, :], in0=oh1[:], scalar1=rt[:, 23:24], scalar2=None, op0=ALU.mult))
                k.op(dve, [oh2, rt, Gd], [Gd], lambda e: e.scalar_tensor_tensor(out=Gd[:, sl, :], in0=oh2[:], scalar=rt[:, 24:25], in1=Gd[:, sl, :], op0=ALU.mult, op1=ALU.add))
            ng_ = len(grp)
            blocks = [(b0, min(b0 + 4, ng_)) for b0 in range(0, ng_, 4)]
            gi = 0
            for ex in range(NEXP):
                load_w_bf16(W[0], moe_w1[(l * 32 + ex) * D:(l * 32 + ex + 1) * D, :])
                load_w_bf16(W[1], moe_w3[(l * 32 + ex) * D:(l * 32 + ex + 1) * D, :])
                load_w_bf16(W[2], moe_w2[(l * 32 + ex) * D:(l * 32 + ex + 1) * D, :])
                for (b0, b1) in blocks:
                    N = (b1 - b0) * 128
                    cols = slice(b0 * 128, b1 * 128)
                    for hc in range(8):
                        gb = gvb[gi % 4]
                        vb = gvb[(gi + 1) % 4]
                        sg_ = sgl[(gi // 2) % 2]
                        gi += 2
                        for kk in range(8):
                            k.op(pe, [W[0], hTb], [gb], lambda e: e.matmul(gb[:, 0:N], lhsT=W[0][:, kk, hc * 128:(hc + 1) * 128], rhs=hTb[:, kk, cols], start=(kk == 0), stop=(kk == 7)))
                        for kk in range(8):
                            k.op(pe, [W[1], hTb], [vb], lambda e: e.matmul(vb[:, 0:N], lhsT=W[1][:, kk, hc * 128:(hc + 1) * 128], rhs=hTb[:, kk, cols], start=(kk == 0), stop=(kk == 7)))
                        k.op(act, [gb], [sg_], lambda e: e.activation(out=sg_[:, 0:N], in_=gb[:, 0:N], func=AF.Silu))
                        k.op(dve, [sg_, vb], [uT], lambda e: e.tensor_mul(out=uT[:, hc, 0:N], in0=sg_[:, 0:N], in1=vb[:, 0:N]))
                    for sl in range(b0, b1):
                        tc_ = slice((sl - b0) * 128, (sl - b0 + 1) * 128)
                        for half in range(2):
                            for hc in range(8):
                                k.op(pe, [uT, W[2]], [yb], lambda e: e.matmul(yb[:, half * 512:(half + 1) * 512], lhsT=uT[:, hc, tc_], rhs=W[2][:, hc, half * 512:(half + 1) * 512], start=(hc == 0), stop=(hc == 7)))
                        if ex == 0:
                            k.op(dve, [yb, Gd], [acc], lambda e: e.tensor_scalar(out=acc[:, sl, :], in0=yb[:], scalar1=Gd[:, sl, ex:ex + 1], scalar2=None, op0=ALU.mult))
                        else:
                            k.op(dve, [yb, Gd, acc], [acc], lambda e: e.scalar_tensor_tensor(out=acc[:, sl, :], in0=yb[:], scalar=Gd[:, sl, ex:ex + 1], in1=acc[:, sl, :], op0=ALU.mult, op1=ALU.add))
            for sl, t in enumerate(grp):
                x_ = xt[sl % 2]
                rows = slice(t * 128, (t + 1) * 128)
                k.dma(sp, x_[:], xm[rows, :], [xm], [x_])
                gB = g2B[1] if t < NC_ else g2B[0]
                k.op(dve, [acc, gB], [junk], lambda e: e.tensor_mul(out=junk[:], in0=acc[:, sl, :], in1=gB[:]))
                k.op(dve, [junk, x_], [h2], lambda e: e.tensor_add(out=h2[:], in0=junk[:], in1=x_[:]))
                if not last:
                    k.dma(pool, xnext[rows, :], h2[:], [h2], [xnext])
                else:
                    k.op(act, [h2], [junk, st], lambda e: e.activation(out=junk[:], in_=h2[:], func=AF.Square, accum_out=st[:, 0:1]))
                    k.op(dve, [st], [st], lambda e: e.tensor_scalar(out=st[:, 1:2], in0=st[:, 0:1], scalar1=1.0 / D, scalar2=EPS, op0=ALU.mult, op1=ALU.add))
                    k.op(act, [st], [st], lambda e: e.activation(out=st[:, 2:3], in_=st[:, 1:2], func=AF.Sqrt))
                    k.op(dve, [st], [st], lambda e: e.reciprocal(out=st[:, 3:4], in_=st[:, 2:3]))
                    k.op(dve, [h2, st, fgB], [junk], lambda e: e.scalar_tensor_tensor(out=junk[:], in0=h2[:], scalar=st[:, 3:4], in1=fgB[:], op0=ALU.mult, op1=ALU.mult))
                    k.dma(pool, out[(t - NC_) * 128:(t - NC_ + 1) * 128, :], junk[:], [junk], [out])
        k.pop()


    def stage_moe2(l, xnext, last):
        tiles = list(range(NC_ if last else 0, NT))
        ntl = len(tiles)
        NBLK = (2 * ntl * 128 + 32 * (BLK - 1) + BLK - 1) // BLK
        NSLOT = NBLK * BLK
        k.push()
        OH = [k.sb([128, ntl, 32], F32) for _ in range(2)]
        RANK = k.sb([128, ntl, 32], F32)
        GW = k.sb([128, ntl, 2], F32)
        DESTI = k.sb([128, ntl, 2], I32)
        PS = k.sb([128, 32], F32)
        BE = k.sb([128, 80], F32)
        wbase = k.sb([128, 8], F32)
        k.dma(sp, wbase[:], c_wbase[:], [c_wbase], [wbase])
        k.push()
        A_l, sh_l, A_c, sh_c = mod_tiles(l, 2, norm2_g)
        wr = k.sb([128, 8, 36], F32)
        brB = k.sb([128, 36], F32)
        k.dma(sp, wr[:], w_r[l].rearrange("(k p) n -> p k n", p=128), [w_r], [wr])
        bcast_load(brB, b_r[l:l + 1, :], b_r)
        LT = k.sb([128, 128], F32)
        onesf = k.sb([128, 128], F32)
        k.dma(sp, LT[:], c_tri[3], [c_tri], [LT])
        k.op(dve, [LT], [LT], lambda e: e.tensor_scalar(out=LT[:], in0=LT[:], scalar1=-16.0, scalar2=None, op0=ALU.mult))
        k.op(dve, [], [onesf], lambda e: e.memset(onesf[:], 1.0))
        Rsum = k.sb([128, 32], F32)
        Mt = k.sb([128, 32], F32)
        k.op(dve, [], [Rsum], lambda e: e.memset(Rsum[:], 0.0))
        xt = [k.sb([128, D], F32) for _ in range(2)]
        junk = k.sb([128, D], F32)
        h2 = k.sb([128, D], F32)
        h2b = [k.sb([128, D], BF16) for _ in range(2)]
        h2T = k.sb([128, 8, 128], F32)
        st = k.sb([128, 4], F32)
        rt = k.sb([128, 64], F32)
        lgs = k.sb([128, 36], F32)
        em = k.sb([128, 32], F32)
        em2 = k.sb([128, 32], F32)
        ptrf = k.ps([128, 1024])
        rkp = k.ps([128, 32])
        for sl, t in enumerate(tiles):
            x_ = xt[sl % 2]
            hb_ = h2b[sl % 2]
            rows = slice(t * 128, (t + 1) * 128)
            oh1 = OH[0][:, sl, :]
            oh2 = OH[1][:, sl, :]
            k.dma(sp, x_[:], xm[rows, :], [xm], [x_])
            A, sh = (A_c, sh_c) if t < NC_ else (A_l, sh_l)
            rms_mod(x_, A, sh, junk, st, h2)
            k.op(act, [h2], [hb_], lambda e: e.copy(out=hb_[:], in_=h2[:]))
            k.dma(sp, h2d[rows, :], hb_[:], [hb_], [h2d])
            transpose8(h2, h2T, ptrf, ident, act)
            for kk in range(8):
                k.op(pe, [h2T, wr], [ptrf], lambda e: e.matmul(ptrf[:, 0:36], lhsT=h2T[:, kk, :], rhs=wr[:, kk, :], start=(kk == 0), stop=(kk == 7)))
            k.op(dve, [ptrf, brB], [lgs], lambda e: e.tensor_add(out=lgs[:], in0=ptrf[:, 0:36], in1=brB[:]))
            k.op(dve, [lgs], [rt], lambda e: e.reduce_max(out=rt[:, 0:1], in_=lgs[:, 0:4], axis=AX.X))
            k.op(dve, [lgs, rt], [rt], lambda e: e.tensor_scalar(out=rt[:, 4:8], in0=lgs[:, 0:4], scalar1=rt[:, 0:1], scalar2=None, op0=ALU.is_equal))
            k.op(dve, [rt], [rt], lambda e: e.tensor_scalar(out=rt[:, 1:2], in0=rt[:, 0:1], scalar1=-1.0, scalar2=None, op0=ALU.mult))
            k.op(act, [lgs, rt], [rt], lambda e: e.activation(out=rt[:, 8:12], in_=lgs[:, 0:4], func=AF.Exp, bias=rt[:, 1:2], scale=1.0, accum_out=rt[:, 2:3]))
            k.op(dve, [rt], [rt], lambda e: e.reciprocal(out=rt[:, 3:4], in_=rt[:, 2:3]))
            k.op(dve, [rt], [rt], lambda e: e.tensor_scalar(out=rt[:, 12:16], in0=rt[:, 4:8], scalar1=1.0, scalar2=BIG, op0=ALU.subtract, op1=ALU.mult))
            k.op(dve, [lgs, rt], [em], lambda e: e.tensor_tensor(out=em[:].rearrange("p (g e) -> p g e", g=4), in0=lgs[:, 4:36].rearrange("p (g e) -> p g e", g=4), in1=rt[:, 12:16].rearrange("p (g o) -> p g o", o=1).to_broadcast([128, 4, 8]), op=ALU.add))
            k.op(dve, [em], [rt], lambda e: e.reduce_max(out=rt[:, 16:17], in_=em[:], axis=AX.X))
            k.op(dve, [em, rt], [OH[0]], lambda e: e.tensor_scalar(out=oh1, in0=em[:], scalar1=rt[:, 16:17], scalar2=None, op0=ALU.is_equal))
            k.op(dve, [OH[0], em], [em2], lambda e: e.scalar_tensor_tensor(out=em2[:], in0=oh1, scalar=-BIG, in1=em[:], op0=ALU.mult, op1=ALU.add))
            k.op(dve, [em2], [rt], lambda e: e.reduce_max(out=rt[:, 17:18], in_=em2[:], axis=AX.X))
            k.op(dve, [em2, rt], [OH[1]], lambda e: e.tensor_scalar(out=oh2, in0=em2[:], scalar1=rt[:, 17:18], scalar2=None, op0=ALU.is_equal))
            k.op(dve, [rt], [rt], lambda e: e.tensor_sub(out=rt[:, 18:19], in0=rt[:, 17:18], in1=rt[:, 16:17]))
            k.op(act, [rt], [rt], lambda e: e.activation(out=rt[:, 19:20], in_=rt[:, 18:19], func=AF.Exp))
            k.op(dve, [rt], [rt], lambda e: e.tensor_scalar(out=rt[:, 20:21], in0=rt[:, 19:20], scalar1=1.0, scalar2=None, op0=ALU.add))
            k.op(dve, [rt], [rt], lambda e: e.reciprocal(out=rt[:, 21:22], in_=rt[:, 20:21]))
            k.op(dve, [rt], [rt], lambda e: e.tensor_mul(out=rt[:, 22:23], in0=rt[:, 19:20], in1=rt[:, 21:22]))
            k.op(dve, [rt], [GW], lambda e: e.tensor_mul(out=GW[:, sl, 0:1], in0=rt[:, 21:22], in1=rt[:, 3:4]))
            k.op(dve, [rt], [GW], lambda e: e.tensor_mul(out=GW[:, sl, 1:2], in0=rt[:, 22:23], in1=rt[:, 3:4]))
            k.op(dve, [OH[0], OH[1]], [Mt], lambda e: e.tensor_add(out=Mt[:], in0=oh1, in1=oh2))
            k.op(pe, [LT, Mt], [rkp], lambda e: e.matmul(rkp[:], lhsT=LT[:], rhs=Mt[:], start=True, stop=False))
            k.op(pe, [onesf, Rsum], [rkp], lambda e: e.matmul(rkp[:], lhsT=onesf[:], rhs=Rsum[:], start=False, stop=True))
            k.op(act, [rkp], [RANK], lambda e: e.copy(out=RANK[:, sl, :], in_=rkp[:]))
            k.op(dve, [Rsum, Mt], [Rsum], lambda e: e.tensor_add(out=Rsum[:], in0=Rsum[:], in1=Mt[:]))
        cnt = k.sb([128, 32], F32)
        pad = k.sb([128, 32], F32)
        pend = k.sb([128, 32], F32)
        bst = k.sb([128, 80], F32)
        tmp32 = k.sb([128, 32], F32)
        dstf = k.sb([128, 2], F32)
        bcast_load(bst, c_bstart[0:1, :], c_bstart)
        k.op(pe, [onesf, Rsum], [rkp], lambda e: e.matmul(rkp[:], lhsT=onesf[:], rhs=Rsum[:], start=True, stop=True))
        k.op(dve, [rkp], [cnt], lambda e: e.tensor_copy(out=cnt[:], in_=rkp[:]))
        k.op(dve, [], [pad], lambda e: e.memset(pad[:], 0.0))
        for j in range((2 * ntl * 128) // BLK + 1):
            k.op(dve, [cnt, pad], [pad], lambda e: e.scalar_tensor_tensor(out=pad[:], in0=cnt[:], scalar=float(j * BLK), in1=pad[:], op0=ALU.is_gt, op1=ALU.add))
        k.op(dve, [pad], [pad], lambda e: e.tensor_scalar(out=pad[:], in0=pad[:], scalar1=float(BLK), scalar2=None, op0=ALU.mult))
        k.op(dve, [], [PS], lambda e: e.memset(PS[:], 0.0))
        for ex in range(1, 32):
            k.op(dve, [PS, pad], [PS], lambda e: e.tensor_add(out=PS[:, ex:ex + 1], in0=PS[:, ex - 1:ex], in1=pad[:, ex - 1:ex]))
        k.op(dve, [PS, pad], [pend], lambda e: e.tensor_add(out=pend[:], in0=PS[:], in1=pad[:]))
        k.op(dve, [], [BE], lambda e: e.memset(BE[:], 0.0))
        for ex in range(32):
            k.op(dve, [bst, pend, BE], [BE], lambda e: e.scalar_tensor_tensor(out=BE[:], in0=bst[:], scalar=pend[:, ex:ex + 1], in1=BE[:], op0=ALU.is_ge, op1=ALU.add))
        k.op(dve, [BE], [BE], lambda e: e.tensor_scalar(out=BE[:], in0=BE[:], scalar1=31.0, scalar2=1024.0, op0=ALU.min, op1=ALU.mult))
        for sl in range(ntl):
            k.op(dve, [RANK, PS], [tmp32], lambda e: e.tensor_add(out=tmp32[:], in0=RANK[:, sl, :], in1=PS[:]))
            for kq in range(2):
                k.op(dve, [tmp32, OH[kq]], [junk], lambda e: e.tensor_mul(out=junk[:, 0:32], in0=tmp32[:], in1=OH[kq][:, sl, :]))
                k.op(dve, [junk], [dstf], lambda e: e.reduce_sum(out=dstf[:, kq:kq + 1], in_=junk[:, 0:32], axis=AX.X))
            k.op(dve, [dstf], [DESTI], lambda e: e.tensor_copy(out=DESTI[:, sl, :], in_=dstf[:]))
        k.pop()
        k.push()
        zt = k.sb([128, 4, D], BF16)
        k.op(dve, [], [zt], lambda e: e.memset(zt[:], 0.0))
        xsv = xs[:].rearrange("(n p) d -> p n d", p=128)
        for b in range(NBLK):
            k.dma(sp, xsv[:, 4 * b:4 * b + 4, :], zt[:], [zt], [xs])
        hb2 = [k.sb([128, D], BF16) for _ in range(2)]
        for sl, t in enumerate(tiles):
            hb_ = hb2[sl % 2]
            k.dma(sp, hb_[:], h2d[t * 128:(t + 1) * 128, :], [h2d], [hb_])
            for kq in range(2):
                k.idma(xs[:, :], hb_[:], [hb_, DESTI], [xs], hb_, out_off=bass.IndirectOffsetOnAxis(ap=DESTI[:, sl, kq:kq + 1], axis=0), bound=NSLOT - 1)
        k.pop()
        k.push()
        stg = [k.sb([128, 8 * D], F32) for _ in range(2)]
        W = [k.sb([128, 8, D], BF16) for _ in range(3)]
        idxf = k.sb([128, 8], F32)
        idxi = [k.sb([128, 8], I32) for _ in range(2)]
        xst = [k.sb([128, D], BF16) for _ in range(4)]
        xT = k.sb([128, 8, 512], BF16)
        uT = k.sb([128, 8, 512], BF16)
        sgl = [k.sb([128, 512], F32) for _ in range(2)]
        ysb = [k.sb([128, D], F32) for _ in range(2)]
        ptr = k.ps([128, 1024], BF16)
        gvb = [k.ps([128, 512]) for _ in range(4)]
        yb = k.ps([128, 1024])
        wsrc = [w[:, :] for w in (moe_w1, moe_w3, moe_w2)]
        si = 0
        gi = 0
        yi = 0
        for b in range(NBLK):
            ix = idxi[b % 2]
            k.op(dve, [BE], [idxf], lambda e: e.tensor_scalar(out=idxf[:, 1:2], in0=BE[:, b:b + 1], scalar1=0.125, scalar2=float(l * 32 * 128), op0=ALU.mult, op1=ALU.add))
            k.op(dve, [wbase, idxf], [idxf], lambda e: e.tensor_add(out=idxf[:, 0:1], in0=wbase[:, 0:1], in1=idxf[:, 1:2]))
            k.op(dve, [idxf], [ix], lambda e: e.tensor_copy(out=ix[:], in_=idxf[:]))
            for i in range(4):
                k.dma(sp, xst[i][:], xs[b * BLK + i * 128:b * BLK + (i + 1) * 128, :], [xs], [xst[i]])
            for m in range(3):
                sg_ = stg[si % 2]
                si += 1
                k.idma(sg_[:, :], wsrc[m], [moe_w1, ix], [sg_], sg_, in_off=bass.IndirectOffsetOnAxis(ap=ix[:, 0:1], axis=0), bound=2 * 32 * 128 - 1)
                k.op(act, [sg_], [W[m]], lambda e: e.copy(out=W[m][:].rearrange("p k n -> p (k n)"), in_=sg_[:]))
            for i in range(4):
                for c in range(8):
                    k.op(pe, [xst[i], identb], [ptr], lambda e: e.transpose(ptr[:, c * 128:(c + 1) * 128], xst[i][:, c * 128:(c + 1) * 128], identb[:]))
                k.op(dve, [ptr], [xT], lambda e: e.tensor_copy(out=xT[:, :, i * 128:(i + 1) * 128], in_=ptr[:].rearrange("p (k t) -> p k t", k=8)))
            for hc in range(8):
                gb = gvb[gi % 4]
                vb = gvb[(gi + 1) % 4]
                sl_ = sgl[(gi // 2) % 2]
                gi += 2
                for kk in range(8):
                    k.op(pe, [W[0], xT], [gb], lambda e: e.matmul(gb[:], lhsT=W[0][:, kk, hc * 128:(hc + 1) * 128], rhs=xT[:, kk, :], start=(kk == 0), stop=(kk == 7)))
                for kk in range(8):
                    k.op(pe, [W[1], xT], [vb], lambda e: e.matmul(vb[:], lhsT=W[1][:, kk, hc * 128:(hc + 1) * 128], rhs=xT[:, kk, :], start=(kk == 0), stop=(kk == 7)))
                k.op(act, [gb], [sl_], lambda e: e.activation(out=sl_[:], in_=gb[:], func=AF.Silu))
                k.op(dve, [sl_, vb], [uT], lambda e: e.tensor_mul(out=uT[:, hc, :], in0=sl_[:], in1=vb[:]))
            for i in range(4):
                tc_ = slice(i * 128, (i + 1) * 128)
                for half in range(2):
                    for hc in range(8):
                        k.op(pe, [uT, W[2]], [yb], lambda e: e.matmul(yb[:, half * 512:(half + 1) * 512], lhsT=uT[:, hc, tc_], rhs=W[2][:, hc, half * 512:(half + 1) * 512], start=(hc == 0), stop=(hc == 7)))
                y_ = ysb[yi % 2]
                yi += 1
                k.op(dve, [yb], [y_], lambda e: e.tensor_copy(out=y_[:], in_=yb[:]))
                k.dma(sp, ys[b * BLK + i * 128:b * BLK + (i + 1) * 128, :], y_[:], [y_], [ys])
        k.pop()
        k.push()
        g2B = [k.sb([128, D], F32) for _ in range(2)]
        for r in range(2):
            bcast_load(g2B[r], modv[r:r + 1, 5 * D:6 * D], modv)
        if last:
            fgB = k.sb([128, D], F32)
            bcast_load(fgB, final_g[0:1, :], final_g)
        xt = [k.sb([128, D], F32) for _ in range(2)]
        y0 = [k.sb([128, D], F32) for _ in range(2)]
        y1 = [k.sb([128, D], F32) for _ in range(2)]
        f_ = k.sb([128, D], F32)
        xo = [k.sb([128, D], F32) for _ in range(2)]
        junk = k.sb([128, D], F32)
        st = k.sb([128, 4], F32)
        for sl, t in enumerate(tiles):
            pr = sl % 2
            rows = slice(t * 128, (t + 1) * 128)
            k.dma(sp, xt[pr][:], xm[rows, :], [xm], [xt[pr]])
            k.idma(y0[pr][:], ys[:, :], [ys, DESTI], [y0[pr]], y0[pr], in_off=bass.IndirectOffsetOnAxis(ap=DESTI[:, sl, 0:1], axis=0), bound=NSLOT - 1)
            k.idma(y1[pr][:], ys[:, :], [ys, DESTI], [y1[pr]], y1[pr], in_off=bass.IndirectOffsetOnAxis(ap=DESTI[:, sl, 1:2], axis=0), bound=NSLOT - 1)
            gB = g2B[1] if t < NC_ else g2B[0]
            k.op(dve, [y0[pr], GW], [f_], lambda e: e.tensor_scalar(out=f_[:], in0=y0[pr][:], scalar1=GW[:, sl, 0:1], scalar2=None, op0=ALU.mult))
            k.op(dve, [y1[pr], GW, f_], [f_], lambda e: e.scalar_tensor_tensor(out=f_[:], in0=y1[pr][:], scalar=GW[:, sl, 1:2], in1=f_[:], op0=ALU.mult, op1=ALU.add))
            k.op(dve, [f_, gB], [f_], lambda e: e.tensor_mul(out=f_[:], in0=f_[:], in1=gB[:]))
            k.op(dve, [f_, xt[pr]], [xo[pr]], lambda e: e.tensor_add(out=xo[pr][:], in0=f_[:], in1=xt[pr][:]))
            if not last:
                k.dma(sp, xnext[rows, :], xo[pr][:], [xo[pr]], [xnext])
            else:
                k.op(act, [xo[pr]], [junk, st], lambda e: e.activation(out=junk[:], in_=xo[pr][:], func=AF.Square, accum_out=st[:, 0:1]))
                k.op(dve, [st], [st], lambda e: e.tensor_scalar(out=st[:, 1:2], in0=st[:, 0:1], scalar1=1.0 / D, scalar2=EPS, op0=ALU.mult, op1=ALU.add))
                k.op(act, [st], [st], lambda e: e.activation(out=st[:, 2:3], in_=st[:, 1:2], func=AF.Sqrt))
                k.op(dve, [st], [st], lambda e: e.reciprocal(out=st[:, 3:4], in_=st[:, 2:3]))
                k.op(dve, [xo[pr], st, fgB], [junk], lambda e: e.scalar_tensor_tensor(out=junk[:], in0=xo[pr][:], scalar=st[:, 3:4], in1=fgB[:], op0=ALU.mult, op1=ALU.mult))
                k.dma(sp, out[(t - NC_) * 128:(t - NC_ + 1) * 128, :], junk[:], [junk], [out])
        k.pop()
        k.pop()

    layers = list(range(n_layers))
    xcur = xin
    for l in layers:
        last = l == n_layers - 1
        stage_ada(l)
        if stop >= 2:
            stage_norm1(l, xcur)
        if stop >= 3:
            stage_proj(l)
        if stop >= 4:
            stage_recur(l, 0)
        if stop >= 5:
            stage_recur(l, 2)
        if stop >= 6:
            stage_attn(l, not last)
        if stop >= 7:
            stage_merge(l, xcur, not last)
        if stop >= 8:
            (stage_moe if dense_moe else stage_moe2)(l, xn, last)
        xcur = xn
        if stop < 99:
            break
    if stop < 99:
        k.push()
        tmp = k.sb([128, D], F32)
        for t in range(L // 128):
            k.dma(sp, tmp[:], xin[CTX + t * 128:CTX + (t + 1) * 128, :], [xin], [tmp])
            k.dma(sp, out[t * 128:(t + 1) * 128, :], tmp[:], [tmp], [out])
        k.pop()
    k.barrier()
    return k, dbg_out


def _const_tables(L):
    T = CTX + L
    j = np.arange(128)[:, None]
    i = np.arange(128)[None, :]
    le = (j <= i).astype(np.float32)
    gt = (j > i).astype(np.float32)
    ge = (j >= i).astype(np.float32)
    lt = (j < i).astype(np.float32)
    masks = np.stack([np.tile(m, (1, 4)) for m in (le, gt, ge)]).astype(np.float32)
    tri = (np.stack([le, gt, ge, lt]) * (-1.0 / 16.0)).astype(np.float32)
    ld = np.log(1.0 - np.exp2(-5.0 - np.arange(4, dtype=np.float32))).astype(np.float32)
    spret = np.repeat((-16.0 * ld)[None, :], 128, axis=1).reshape(1, 512)
    spret = np.broadcast_to(np.repeat(-16.0 * ld, 128)[None, :], (128, 512)).astype(np.float32)

    def tab(pos, half):
        inv = (10000.0 ** (-np.arange(half, dtype=np.float32) / half)).astype(np.float32)
        ang = pos.astype(np.float32)[:, None] * inv[None, :]
        return np.cos(ang).astype(np.float32), np.sin(ang).astype(np.float32)

    rows = np.arange(L) // 64
    cols = np.arange(L) % 64
    cr, sr = tab(rows, 32)
    cc, sc = tab(cols, 32)
    rope_a = np.stack([np.concatenate([cr, cr, cc, cc], 1), np.concatenate([-sr, sr, -sc, sc], 1)]).astype(np.float32)
    c2, s2 = tab(np.arange(T), 64)
    rope_r = np.stack([np.concatenate([c2, c2], 1), np.concatenate([-s2, s2], 1)]).astype(np.float32)
    return dict(c_ident=np.eye(128, dtype=np.float32), c_masks=masks, c_tri=tri, c_spret=spret,
                c_rope_a=rope_a, c_rope_r=rope_r,
                c_bstart=(np.arange(80, dtype=np.float32) * 512.0).reshape(1, 80),
                c_wbase=(np.arange(8, dtype=np.float32)[None, :] * 128.0 + np.arange(128, dtype=np.float32)[:, None]))


def prep_inputs(inp, b, L):
    f = lambda a: np.ascontiguousarray(np.asarray(a, dtype=np.float32))
    m = {}
    m["xin"] = f(np.concatenate([inp["ctx"][b], inp["x"][b][:L]], axis=0))
    cc = np.stack([np.asarray(inp["c"][b]), np.asarray(inp["c_ctx"])])
    m["ccT"] = f(cc.reshape(2, 8, 128).transpose(2, 1, 0).reshape(128, 16))
    for n in ("w_ada", "b_ada", "norm1_g", "norm2_g", "w_in", "gla_norm_g", "ret_norm_g", "attn_sink",
              "w_br_gla", "w_br_attn", "w_br_ret", "w_out", "moe_w1", "moe_w3", "moe_w2"):
        if n in inp:
            m[n] = f(inp[n])
            if n.startswith("moe_w"):
                m[n] = np.ascontiguousarray(m[n].reshape(2, 32, 8, 128, D).transpose(0, 1, 3, 2, 4)).reshape(2 * 32 * 128, 8 * D)
    wa = np.zeros((2, 33, 1024), np.float32)
    wa[:, 0:16, 0:512] = inp["gla_wa2"][:, 0]
    wa[:, 16:32, 512:1024] = inp["gla_wa2"][:, 1]
    wa[:, 32, 0:512] = inp["gla_ba"][:, 0]
    wa[:, 32, 512:1024] = inp["gla_ba"][:, 1]
    m["wa_aug"] = wa
    m["w_r"] = f(np.concatenate([inp["moe_w_grp"], inp["moe_w_exp"]], axis=-1))
    m["b_r"] = f(np.concatenate([inp["moe_b_grp"], inp["moe_b_exp"]], axis=-1))
    m["final_g"] = f(np.asarray(inp["final_g"]).reshape(1, D))
    return m


_CACHE = {}


def kernel(**inputs):
    L = 8192
    B = 8
    if "prog" not in _CACHE:
        _CACHE["prog"] = build(L)
        _CACHE["const"] = _const_tables(L)
    k, _ = _CACHE["prog"]
    inp = {n: np.asarray(v) for n, v in inputs.items()}
    shared = None
    in_maps = []
    for b in range(B):
        m = prep_inputs(inp, b, L)
        if shared is None:
            shared = {n: v for n, v in m.items() if n not in ("xin", "ccT")}
        else:
            for n in shared:
                m[n] = shared[n]
        m.update(_CACHE["const"])
        in_maps.append(m)
    res = run_bass_kernel_spmd(k.nc, in_maps, core_ids=list(range(B)))
    return np.stack([np.asarray(r["out"]) for r in res.results]).astype(np.float32)
```
